# Optimizing a Trainium2 kernel written in Bass

```python
import jax, jax.numpy as jnp
from jax import lax
import numpy as np

D_MODEL = 1024
BATCH = 32
SEQ = 2048
DEPTH = 4

HEAD_DIM = 64
DIL_PATTERNS = ((128, 1), (512, 4), (2048, 16))
DIL_HEADS = 4
N_DIL_SUB = DIL_HEADS * len(DIL_PATTERNS)
DIL_QKV = N_DIL_SUB * HEAD_DIM
DIL_OUT = DIL_HEADS * HEAD_DIM
DIL_BLOCK = 64
WIN_HALF = 128
WIN_BLOCK = 128
WIN_Q = D_MODEL
WIN_Q_HEADS = WIN_Q // HEAD_DIM
WIN_KV_HEADS = 4
WIN_KV = WIN_KV_HEADS * HEAD_DIM
N_BRANCH = 2
SPLITS = (DIL_QKV, 2 * DIL_QKV, 3 * DIL_QKV,
          3 * DIL_QKV + WIN_Q, 3 * DIL_QKV + WIN_Q + WIN_KV, 3 * DIL_QKV + WIN_Q + 2 * WIN_KV)
IN_COLS = 3 * DIL_QKV + WIN_Q + 2 * WIN_KV + N_BRANCH * D_MODEL
N_EXPERTS = 16
CAPACITY_FACTOR = 2
D_EXPERT = 1024
RMS_EPS = 1e-6
NEG_INF = -1e30

kernel_name = "hybrid_dilated_window_ec_moe_encoder"


def rmsnorm(x, g):
    xf = x.astype(jnp.float32)
    y = xf * lax.rsqrt(jnp.mean(xf * xf, axis=-1, keepdims=True) + RMS_EPS)
    return (y * g.astype(jnp.float32)).astype(x.dtype)


def alibi_slopes(n):
    return 2.0 ** (-8.0 * jnp.arange(1, n + 1, dtype=jnp.float32) / n)


def banded_attention(q, k, v, half, blk, slopes, pos_scale, sink=None):
    N, L, H, dh = q.shape
    Hk = k.shape[2]
    G = H // Hk
    nb = -(-L // blk)
    Lp = nb * blk
    pad = Lp - L
    qb = jnp.pad(q, ((0, 0), (0, pad), (0, 0), (0, 0))).reshape(N, nb, blk, Hk, G, dh)

    def key_blocks(t):
        tp = jnp.pad(t, ((0, 0), (blk, pad + blk), (0, 0), (0, 0)))
        return jnp.concatenate([
            tp[:, :Lp].reshape(N, nb, blk, Hk, dh),
            tp[:, blk:blk + Lp].reshape(N, nb, blk, Hk, dh),
            tp[:, 2 * blk:].reshape(N, nb, blk, Hk, dh)], axis=2)

    kb = key_blocks(k)
    vb = key_blocks(v)
    a = jnp.arange(blk)[:, None]
    b = jnp.arange(3 * blk)[None, :]
    rel = b - blk - a
    kpos = (jnp.arange(nb)[:, None, None] - 1) * blk + b[None]
    mask = (jnp.abs(rel) <= half)[None] & (kpos >= 0) & (kpos < L)
    bias = -slopes.astype(jnp.float32).reshape(Hk, G)[:, :, None, None] * \
        (pos_scale * jnp.abs(rel)).astype(jnp.float32)[None, None]
    s = jnp.einsum('nibkgd,nijkd->nikgbj', qb, kb).astype(jnp.float32) * (dh ** -0.5) + bias
    s = jnp.where(mask[None, :, None, None], s, NEG_INF)
    m = jnp.max(s, axis=-1)
    if sink is not None:
        sk = sink.astype(jnp.float32).reshape(Hk, G)[:, :, None]
        m = jnp.maximum(m, sk)
    p = jnp.exp(s - m[..., None])
    denom = jnp.sum(p, axis=-1)
    if sink is not None:
        denom = denom + jnp.exp(sk - m)
    p = p / denom[..., None]
    o = jnp.einsum('nikgbj,nijkd->nibkgd', p.astype(v.dtype), vb)
    o = o.reshape(N, Lp, H, dh)[:, :L]
    lse = jnp.moveaxis(m + jnp.log(denom), -1, 2).reshape(N, Lp, H)[:, :L]
    return o, lse


def dilated_mixture(q, k, v):
    B, S = q.shape[:2]
    slopes = alibi_slopes(N_DIL_SUB)
    outs, lses = [], []
    for g, (w, r) in enumerate(DIL_PATTERNS):
        sl = slice(g * DIL_HEADS, (g + 1) * DIL_HEADS)

        def strided(t):
            return t[:, :, sl].reshape(B, S // r, r, DIL_HEADS, HEAD_DIM).transpose(0, 2, 1, 3, 4) \
                .reshape(B * r, S // r, DIL_HEADS, HEAD_DIM)

        o, lse = banded_attention(strided(q), strided(k), strided(v), w // (2 * r), DIL_BLOCK,
                                  slopes[sl], r)
        outs.append(o.reshape(B, r, S // r, DIL_HEADS, HEAD_DIM).transpose(0, 2, 1, 3, 4)
                    .reshape(B, S, DIL_HEADS, HEAD_DIM))
        lses.append(lse.reshape(B, r, S // r, DIL_HEADS).transpose(0, 2, 1, 3).reshape(B, S, DIL_HEADS))
    wts = jax.nn.softmax(jnp.stack(lses, axis=0), axis=0)
    return jnp.sum(wts[..., None].astype(q.dtype) * jnp.stack(outs, axis=0), axis=0)


def hybrid_mixer(h, w_in, w_branch_a, w_branch_b, b_gate, sink_logit, w_out):
    B, S, D = h.shape
    proj = jnp.einsum('bsd,dc->bsc', h, w_in)
    qa, ka, va, qw, kw, vw, gates = jnp.split(proj, SPLITS, axis=-1)
    oa = dilated_mixture(qa.reshape(B, S, N_DIL_SUB, HEAD_DIM),
                         ka.reshape(B, S, N_DIL_SUB, HEAD_DIM),
                         va.reshape(B, S, N_DIL_SUB, HEAD_DIM))
    ya = jnp.einsum('bsc,cd->bsd', oa.reshape(B, S, DIL_OUT), w_branch_a)
    ob, _ = banded_attention(qw.reshape(B, S, WIN_Q_HEADS, HEAD_DIM),
                             kw.reshape(B, S, WIN_KV_HEADS, HEAD_DIM),
                             vw.reshape(B, S, WIN_KV_HEADS, HEAD_DIM),
                             WIN_HALF, WIN_BLOCK, alibi_slopes(WIN_Q_HEADS), 1, sink_logit)
    yb = jnp.einsum('bsc,cd->bsd', ob.reshape(B, S, WIN_Q), w_branch_b)
    g = jax.nn.sigmoid((gates + b_gate).astype(jnp.float32)).astype(h.dtype).reshape(B, S, N_BRANCH, D)
    merged = g[:, :, 0] * ya + g[:, :, 1] * yb
    return jnp.einsum('bsd,de->bse', merged, w_out)


def expert_choice_ffn(h, w_router, w_gate, w_up, w_down):
    B, S, D = h.shape
    cap = CAPACITY_FACTOR * S // N_EXPERTS
    aff = jax.nn.softmax(jnp.einsum('bsd,de->bse', h, w_router).astype(jnp.float32), axis=-1)
    gate, idx = lax.top_k(jnp.swapaxes(aff, 1, 2), cap)
    bidx = jnp.arange(B)[:, None, None]
    xe = h[bidx, idx]
    a = jnp.einsum('becd,edf->becf', xe, w_gate)
    u = jnp.einsum('becd,edf->becf', xe, w_up)
    ye = jnp.einsum('becf,efd->becd', jax.nn.silu(a) * u, w_down)
    return jnp.zeros_like(h).at[bidx, idx].add(gate[..., None].astype(h.dtype) * ye)


def setup_inputs(seed: int = 0) -> dict:
    key = jax.random.key(seed)
    ks = jax.random.split(key, 14)
    f = jnp.float32
    return {
        "x": jax.random.normal(ks[0], (BATCH, SEQ, D_MODEL), f),
        "norm_mix": 1.0 + 0.02 * jax.random.normal(ks[1], (DEPTH, D_MODEL), f),
        "w_in": jax.random.normal(ks[2], (DEPTH, D_MODEL, IN_COLS), f) * D_MODEL ** -0.5,
        "w_branch_a": jax.random.normal(ks[3], (DEPTH, DIL_OUT, D_MODEL), f) * DIL_OUT ** -0.5,
        "w_branch_b": jax.random.normal(ks[4], (DEPTH, WIN_Q, D_MODEL), f) * WIN_Q ** -0.5,
        "b_gate": 0.1 * jax.random.normal(ks[5], (DEPTH, N_BRANCH * D_MODEL), f),
        "sink_logit": 0.5 * jax.random.normal(ks[6], (DEPTH, WIN_Q_HEADS), f),
        "w_out": jax.random.normal(ks[7], (DEPTH, D_MODEL, D_MODEL), f) * D_MODEL ** -0.5,
        "norm_ffn": 1.0 + 0.02 * jax.random.normal(ks[8], (DEPTH, D_MODEL), f),
        "w_router": jax.random.normal(ks[9], (DEPTH, D_MODEL, N_EXPERTS), f) * D_MODEL ** -0.5,
        "w_expert_gate": jax.random.normal(ks[10], (DEPTH, N_EXPERTS, D_MODEL, D_EXPERT), f) * D_MODEL ** -0.5,
        "w_expert_up": jax.random.normal(ks[11], (DEPTH, N_EXPERTS, D_MODEL, D_EXPERT), f) * D_MODEL ** -0.5,
        "w_expert_down": jax.random.normal(ks[12], (DEPTH, N_EXPERTS, D_EXPERT, D_MODEL), f) * D_EXPERT ** -0.5,
        "norm_final": 1.0 + 0.02 * jax.random.normal(ks[13], (D_MODEL,), f),
    }


def reference(x, norm_mix, w_in, w_branch_a, w_branch_b, b_gate, sink_logit, w_out,
              norm_ffn, w_router, w_expert_gate, w_expert_up, w_expert_down, norm_final):
    h = x
    for l in range(DEPTH):
        h = h + hybrid_mixer(rmsnorm(h, norm_mix[l]), w_in[l], w_branch_a[l], w_branch_b[l],
                             b_gate[l], sink_logit[l], w_out[l])
        h = h + expert_choice_ffn(rmsnorm(h, norm_ffn[l]), w_router[l], w_expert_gate[l],
                                  w_expert_up[l], w_expert_down[l])
    return rmsnorm(h, norm_final)
```

```python
import numpy as np
from contextlib import ExitStack
import concourse.bass as bass
import concourse.mybir as mybir
from concourse.bass_utils import run_bass_kernel_spmd

F32 = mybir.dt.float32
BF16 = mybir.dt.bfloat16
AF = mybir.ActivationFunctionType
ALU = mybir.AluOpType
AX = mybir.AxisListType

D = 1024
S = 2048
DEPTH = 4
NCORES = 8
NSEQ = 4
KC = 8
NE = 16
CAP = 256
EPS = 1e-6
DIL_R = (1, 4, 16)


def _slopes(n):
    return [float(2.0 ** (-8.0 * i / n)) for i in range(1, n + 1)]


SL_DIL = _slopes(12)
SL_WIN = _slopes(16)


class Ev:
    __slots__ = ("sem", "val", "key")

    def __init__(self, sem, val, key):
        self.sem, self.val, self.key = sem, val, key


class Buf:
    __slots__ = ("name", "w", "r", "excl")

    def __init__(self, name, excl=False):
        self.name = name
        self.w = None
        self.r = {}
        self.excl = excl


class Eng:
    def __init__(self, name, h, sem, is_pe=False):
        self.name, self.h, self.sem, self.is_pe = name, h, sem, is_pe
        self.cnt = 0
        self.waited = {}


class DSem:
    def __init__(self, sem, key):
        self.sem, self.key, self.val = sem, key, 0


class Prog:
    def __init__(self, nc, es):
        self.nc = nc
        self.es = es
        mk = lambda n: es.enter_context(nc.semaphore(n))
        self.pe = Eng("pe", nc.tensor, mk("s_pe"), True)
        self.act = Eng("act", nc.scalar, mk("s_act"))
        self.dve = Eng("dve", nc.vector, mk("s_dve"))
        self.pool = Eng("pool", nc.gpsimd, mk("s_pool"))
        self.sp = Eng("sp", nc.sync, mk("s_sp"))
        self.engs = [self.pe, self.act, self.dve, self.pool, self.sp]
        self.dsems = []
        self.dtags = {}
        self.nds = 0

    def dsem(self, tag=None):
        if tag is not None and tag in self.dtags:
            return self.dtags[tag]
        self.nds += 1
        d = DSem(self.es.enter_context(self.nc.semaphore("s_dma%d" % self.nds)), "dma%d" % self.nds)
        self.dsems.append(d)
        if tag is not None:
            self.dtags[tag] = d
        return d

    def need(self, eng, ev, raw):
        if ev is None:
            return
        if ev.key == eng.name and eng.is_pe:
            return
        if eng.waited.get(ev.key, 0) >= ev.val:
            return
        eng.h.wait_ge(ev.sem, ev.val)
        eng.waited[ev.key] = ev.val

    def deps(self, eng, reads, writes):
        for b in reads:
            self.need(eng, b.w, True)
            if b.excl:
                for ev in b.r.values():
                    if ev.key != eng.name:
                        self.need(eng, ev, False)
        for b in writes:
            self.need(eng, b.w, False)
            for ev in b.r.values():
                self.need(eng, ev, False)

    def mark(self, ev, reads, writes):
        for b in writes:
            b.w = ev
            b.r = {}
        for b in reads:
            o = b.r.get(ev.key)
            if o is None or o.val < ev.val:
                b.r[ev.key] = ev

    def emit(self, eng, fn, reads=(), writes=(), signal=True):
        self.deps(eng, reads, writes)
        ins = fn()
        if signal:
            eng.cnt += 1
            ins.then_inc(eng.sem, 1)
            ev = Ev(eng.sem, eng.cnt, eng.name)
        else:
            ev = Ev(eng.sem, eng.cnt + 1, eng.name)
        self.mark(ev, reads, writes)
        return ins

    def dma(self, q, ds, out_ap, in_ap, reads=(), writes=()):
        self.deps(q, reads, writes)
        if not isinstance(out_ap, list):
            out_ap, in_ap = [out_ap], [in_ap]
        for o, i in zip(out_ap, in_ap):
            ds.val += 16
            q.h.dma_start(out=o, in_=i).then_inc(ds.sem, 16)
        ev = Ev(ds.sem, ds.val, ds.key)
        self.mark(ev, reads, writes)

    def barrier(self):
        for e in self.engs:
            for f in self.engs:
                if f is e or f.cnt == 0:
                    continue
                if e.waited.get(f.name, 0) < f.cnt:
                    e.h.wait_ge(f.sem, f.cnt)
                    e.waited[f.name] = f.cnt
            for d in self.dsems:
                if d.val and e.waited.get(d.key, 0) < d.val:
                    e.h.wait_ge(d.sem, d.val)
                    e.waited[d.key] = d.val


class _Stop(Exception):
    pass


class Ring:
    def __init__(self, items):
        self.items = items
        self.i = -1

    def next(self):
        self.i = (self.i + 1) % len(self.items)
        return self.items[self.i]


def build_program(L=DEPTH, NS=NSEQ, stop_after=None):
    nc = bass.Bass("TRN2", target_bir_lowering=False)
    es = ExitStack()
    P = Prog(nc, es)
    pe, act, dve, pool, sp = P.pe, P.act, P.dve, P.pool, P.sp

    def din(name, shape):
        return nc.dram_tensor(name, list(shape), F32, kind="ExternalInput").ap()

    NV = L * 8 + L * 8 + L * 16 + 8 + L * 8
    V_MIX, V_FFN, V_BG, V_FIN, V_SINK = 0, L * 8, L * 16, L * 32, L * 32 + 8
    xT = din("xT", [NS, D, S])
    wa_d = din("wa", [L, 6, D, 384])
    wqw_d = din("wqw", [L, 8, D, 128])
    wkvw_d = din("wkvw", [L, 4, D, 192])
    wtail_d = din("wtail", [L, 8, D, 256])
    wba_d = din("wba", [L, 8, 256, 128])
    wbb_d = din("wbb", [L, 8, D, 128])
    wout_d = din("wout", [L, D, D])
    wr_d = din("wr", [L, D, NE])
    weg_d = din("weg", [L, NE, D, D])
    weu_d = din("weu", [L, NE, D, D])
    wed_d = din("wed", [L, NE, D, D])
    vecs_d = din("vecs", [128, NV])
    tabs_d = din("tabs", [128, 2 * 3 * 128])
    cst2_d = din("cst2", [128, 384])
    outT = nc.dram_tensor("outT", [NS, D, S], F32, kind="ExternalOutput").ap()

    def sb(name, shape, dt):
        return es.enter_context(nc.sbuf_tensor(name, list(shape), dt))

    uctr = [0]

    def uname():
        uctr[0] += 1
        return "t%d" % uctr[0]

    hT = sb("hT", [128, KC, S], F32)
    hnT = sb("hnT", [128, KC, S], BF16)
    vec = sb("vec", [128, NV], F32)
    esink = sb("esink", [128, L * 8], F32)
    tab = sb("tab", [128, 2, 3, 128], F32)
    ones16 = sb("ones16", [NE, NE], F32)
    onesF = sb("onesF", [128, 128], BF16)
    onesA0 = sb("onesA0", [128, 128], BF16)
    ones0B = sb("ones0B", [128, 128], BF16)
    wr_sb = sb("wr_sb", [128, L, KC, NE], BF16)
    epsb = sb("epsb", [128, 1], F32)
    B_hT, B_hnT, B_const = Buf("hT"), Buf("hnT"), Buf("const")
    B_wr = Buf("wr")
    identF = sb("identF", [128, 128], F32)
    iota1 = sb("iota1", [128, 256], F32)
    identB = sb("identB", [128, 128], BF16)
    B_c2 = Buf("c2")

    PSB = []
    for i in range(8):
        t = es.enter_context(nc.psum_tensor("ps%d" % i, [128, 512], F32))
        PSB.append((t, Buf("ps%d" % i, excl=True)))
    ps_ring = Ring(PSB)
    fresh = {}

    def next_ps():
        t, b = ps_ring.next()
        fresh[b.name] = True
        return t, b

    def mm(psb, out_ap, lhsT, rhs, reads, last=False, sig=False, start=None, stop=None):
        b = psb[1]
        st = fresh[b.name] if start is None else start
        fresh[b.name] = False
        sp_ = last if stop is None else (stop or last)
        P.emit(pe, lambda: nc.tensor.matmul(out_ap, lhsT, rhs, start=st, stop=sp_),
               reads=reads, writes=[b], signal=(last or sig))

    ds_c = P.dsem()
    P.dma(sp, ds_c, [vec[:, :], tab[:].rearrange("p a b c -> p (a b c)")],
          [vecs_d[:, :], tabs_d[:, :]], writes=[B_const])
    ds_c2 = P.dsem()
    P.dma(sp, ds_c2, [identF[:, :], iota1[:, :]], [cst2_d[:, 0:128], cst2_d[:, 128:384]], writes=[B_c2])
    ds_c3 = P.dsem()
    P.dma(pool, ds_c3, identB[:, :], cst2_d[:, 0:128], writes=[B_c2])
    ds_wr = P.dsem()
    P.dma(pool, ds_wr, [wr_sb[:, l, :, :] for l in range(L)],
          [wr_d[l].rearrange("(k p) e -> p k e", p=128) for l in range(L)], writes=[B_wr])
    B_ones = Buf("ones")
    P.emit(dve, lambda: nc.vector.memset(onesF[:], 1.0), writes=[B_ones])
    P.emit(dve, lambda: nc.vector.memset(onesA0[:], 0.0), writes=[B_ones])
    P.emit(dve, lambda: nc.vector.memset(ones0B[:], 0.0), writes=[B_ones])
    P.emit(dve, lambda: nc.vector.memset(onesA0[:, 0:64], 1.0), writes=[B_ones])
    P.emit(dve, lambda: nc.vector.memset(ones0B[:, 64:128], 1.0), writes=[B_ones])
    P.emit(dve, lambda: nc.vector.memset(ones16[:], 1.0), writes=[B_ones])
    P.emit(dve, lambda: nc.vector.memset(epsb[:], EPS), writes=[B_ones])
    B_esink = Buf("esink")
    P.emit(act, lambda: nc.scalar.activation(out=esink[:], in_=vec[:, V_SINK:V_SINK + L * 8], func=AF.Exp),
           reads=[B_const], writes=[B_esink])

    ds_x = P.dsem("x")
    ds_out = P.dsem("dbg")

    def rmsnorm(gcol, sq_ring, rs_ring, final_seq=None, stage_ring=None):
        for tt in range(4):
            tsl = slice(tt * 512, (tt + 1) * 512)
            ps = next_ps()
            for c in range(KC):
                sq, bsq = sq_ring.next()
                P.emit(act, lambda: nc.scalar.activation(out=sq[:], in_=hT[:, c, tsl], func=AF.Square),
                       reads=[B_hT], writes=[bsq])
                mm(ps, ps[0][:, :], onesF[:], sq[:], [bsq, B_ones], last=(c == KC - 1), sig=True)
            rs, brs = rs_ring.next()
            P.emit(act, lambda: nc.scalar.activation(out=rs[:], in_=ps[0][:, :], func=AF.Sqrt,
                                                     scale=1.0 / D, bias=epsb[:, 0:1]),
                   reads=[ps[1], B_ones], writes=[brs])
            P.emit(dve, lambda: nc.vector.reciprocal(rs[:], rs[:]), reads=[brs], writes=[brs])
            for c in range(KC):
                if final_seq is None:
                    P.emit(dve, lambda: nc.vector.scalar_tensor_tensor(
                        out=hnT[:, c, tsl], in0=hT[:, c, tsl], scalar=vec[:, gcol + c:gcol + c + 1],
                        in1=rs[:], op0=ALU.mult, op1=ALU.mult),
                        reads=[B_hT, brs, B_const], writes=[B_hnT])
                else:
                    st, bst, dso = stage_ring.next()
                    P.emit(dve, lambda: nc.vector.scalar_tensor_tensor(
                        out=st[:], in0=hT[:, c, tsl], scalar=vec[:, gcol + c:gcol + c + 1],
                        in1=rs[:], op0=ALU.mult, op1=ALU.mult),
                        reads=[B_hT, brs, B_const], writes=[bst])
                    P.dma(sp, dso, outT[final_seq, c * 128:(c + 1) * 128, tsl], st[:], reads=[bst])

    def perm512(c, ut, r):
        if r == 1:
            return hnT[:, c, ut * 512:(ut + 1) * 512]
        if r == 4:
            return hnT[:, c, ut:S:4]
        return hnT[:, c, :].rearrange("p (a q) -> p q a", q=16)[:, 4 * ut:4 * ut + 4, :]

    def perm128(c, uc, r):
        if r == 1:
            return hnT[:, c, uc * 128:(uc + 1) * 128]
        if r == 4:
            sub, a0 = uc // 4, (uc % 4) * 128
            return hnT[:, c, sub + 4 * a0:sub + 4 * (a0 + 127) + 1:4]
        return hnT[:, c, uc:S:16]

    def nat128(qt, r):
        if r == 1:
            return slice(qt * 128, (qt + 1) * 128)
        if r == 4:
            sub, a0 = qt // 4, (qt % 4) * 128
            return slice(sub + 4 * a0, sub + 4 * (a0 + 127) + 1, 4)
        return slice(qt, S, 16)

    def ps_view(ps, r):
        if r == 16:
            return ps[0][:, :].rearrange("p (q a) -> p q a", q=4)
        return ps[0][:, :]

    cp_flip = [0]

    def copy_ps(out_ap, in_ap, reads, writes, eng=None):
        if eng is None:
            cp_flip[0] ^= 1
            eng = act if cp_flip[0] else dve
        if eng is act:
            P.emit(act, lambda: nc.scalar.copy(out=out_ap, in_=in_ap), reads=reads, writes=writes)
        else:
            P.emit(dve, lambda: nc.vector.tensor_copy(out=out_ap, in_=in_ap), reads=reads, writes=writes)

    def ck(name):
        if stop_after == name:
            raise _Stop()

    try:
      for s in range(NS):
          P.dma(sp, ds_x, [hT[:, c, :] for c in range(KC)], [xT[s, c * 128:(c + 1) * 128, :] for c in range(KC)],
                writes=[B_hT])
          for l in range(L):
              with ExitStack() as ph:
                  def sbp(name, shape, dt):
                      return ph.enter_context(nc.sbuf_tensor(uname(), list(shape), dt))

                  sq_ring = Ring([(sbp("sq", [128, 512], BF16), Buf("sq%d" % i)) for i in range(2)])
                  rs_ring = Ring([(sbp("rs", [128, 512], F32), Buf("rs%d" % i)) for i in range(2)])
                  OT = sbp("OT", [128, 10, S], BF16)
                  B_OT = [Buf("OT%d" % i) for i in range(10)]
                  ck("load")
                  rmsnorm(V_MIX + l * 8, sq_ring, rs_ring)
                  ck("norm")

                  with ExitStack() as ph2:
                      def sb2(shape, dt):
                          return ph2.enter_context(nc.sbuf_tensor(uname(), list(shape), dt))

                      QA0, Q0B, K2 = sb2([128, S], BF16), sb2([128, S], BF16), sb2([128, S], BF16)
                      VA0, V0B = sb2([128, 16, 128], BF16), sb2([128, 16, 128], BF16)
                      acc = sb2([128, 2, S], F32)
                      B_QA, B_QB, B_K, B_VA, B_VB, B_acc = (Buf(n) for n in ("QA", "QB", "K", "VA", "VB", "acc"))
                      wa_ring = Ring([(sb2([128, KC, 384], BF16), Buf("wa%d" % i), P.dsem("wa%d" % i)) for i in range(1)])
                      wq_ring = Ring([(sb2([128, KC, 128], BF16), Buf("wq%d" % i), P.dsem("wq%d" % i)) for i in range(1)])
                      wkv_ring = Ring([(sb2([128, KC, 192], BF16), Buf("wkv%d" % i), P.dsem("wkv%d" % i)) for i in range(1)])
                      tS_ring = Ring([(sb2([128, 384], F32), Buf("tS%d" % i)) for i in range(2)])
                      PT_ring = Ring([(sb2([128, 384], BF16), Buf("PT%d" % i)) for i in range(4)])
                      dn_ring = Ring([(sb2([128, 128], F32), Buf("dn%d" % i)) for i in range(2)])
                      P.emit(dve, lambda: nc.vector.memset(QA0[64:128, :], 0.0), writes=[B_QA])
                      P.emit(dve, lambda: nc.vector.memset(Q0B[0:64, :], 0.0), writes=[B_QB])
                      P.emit(dve, lambda: nc.vector.memset(VA0[:, :, 64:128], 0.0), writes=[B_VA])
                      P.emit(dve, lambda: nc.vector.memset(V0B[:, :, 0:64], 0.0), writes=[B_VB])
                      ck("ph2alloc")

                      def proj_q(wt, bw, col0, r):
                          for ut in range(4):
                              ps = next_ps()
                              for c in range(KC):
                                  mm(ps, ps_view(ps, r), wt[:, c, col0:col0 + 128], perm512(c, ut, r),
                                     [bw, B_hnT], last=(c == KC - 1))
                              usl = slice(ut * 512, (ut + 1) * 512)
                              copy_ps(QA0[0:64, usl], ps[0][0:64, :], [ps[1]], [B_QA], act)
                              copy_ps(Q0B[64:128, usl], ps[0][64:128, :], [ps[1]], [B_QB], dve)

                      def proj_k(wt, bw, col0, r):
                          for ut in range(4):
                              ps = next_ps()
                              for c in range(KC):
                                  mm(ps, ps_view(ps, r), wt[:, c, col0:col0 + 128], perm512(c, ut, r),
                                     [bw, B_hnT], last=(c == KC - 1))
                              copy_ps(K2[:, ut * 512:(ut + 1) * 512], ps[0][:, :], [ps[1]], [B_K])

                      def attention(r, tabi, scA, scB, epilogue):
                          tps = (S // r) // 128
                          for qt in range(16):
                              jj = qt % tps
                              chunks = []
                              if jj > 0:
                                  chunks.append((qt - 1, 0))
                              chunks.append((qt, 1))
                              if jj < tps - 1:
                                  chunks.append((qt + 1, 2))
                              lo, hi = chunks[0][1] * 128, (chunks[-1][1] + 1) * 128
                              qsl = slice(qt * 128, (qt + 1) * 128)
                              pts = []
                              for (QX, BQ, sc) in ((QA0, B_QA, scA), (Q0B, B_QB, scB)):
                                  ps = next_ps()
                                  for i, (kc, ti) in enumerate(chunks):
                                      mm(ps, ps[0][:, ti * 128:(ti + 1) * 128], K2[:, kc * 128:(kc + 1) * 128],
                                         QX[:, qsl], [B_K, BQ], last=(i == len(chunks) - 1))
                                  tS, btS = tS_ring.next()
                                  P.emit(dve, lambda: nc.vector.scalar_tensor_tensor(
                                      out=tS[:, lo:hi], in0=tab[:, tabi].rearrange("p a b -> p (a b)")[:, lo:hi],
                                      scalar=sc, in1=ps[0][:, lo:hi], op0=ALU.mult, op1=ALU.add),
                                      reads=[ps[1], B_const], writes=[btS])
                                  PT, bPT = PT_ring.next()
                                  P.emit(act, lambda: nc.scalar.activation(out=PT[:, lo:hi], in_=tS[:, lo:hi],
                                                                           func=AF.Exp, scale=0.125),
                                         reads=[btS], writes=[bPT])
                                  pts.append((PT, bPT))
                              pso = next_ps()
                              n = 0
                              tot = 4 * len(chunks)
                              for (PT, bPT), VX, BV, oX in ((pts[0], VA0, B_VA, onesA0), (pts[1], V0B, B_VB, ones0B)):
                                  for (kc, ti) in chunks:
                                      n += 1
                                      mm(pso, pso[0][:, 0:128], VX[:, kc, :], PT[:, ti * 128:(ti + 1) * 128],
                                         [BV, bPT], last=False)
                                      n += 1
                                      mm(pso, pso[0][:, 128:256], oX[:], PT[:, ti * 128:(ti + 1) * 128],
                                         [B_ones, bPT], last=(n == tot))
                              epilogue(pso, qt)

                      for jp in range(2):
                          for g in range(3):
                              r = DIL_R[g]
                              wt, bw, dsw = wa_ring.next()
                              P.dma(pool, dsw, wt[:], wa_d[l, jp * 3 + g].rearrange("(k p) c -> p k c", p=128),
                                    writes=[bw])
                              ck("wadma")
                              proj_q(wt, bw, 0, r)
                              ck("projq")
                              proj_k(wt, bw, 128, r)
                              ck("proj%d" % g)
                              for u0 in range(0, 16, 4):
                                  ps = next_ps()
                                  for uu in range(4):
                                      for c in range(KC):
                                          mm(ps, ps[0][:, uu * 128:(uu + 1) * 128], perm128(c, u0 + uu, r),
                                             wt[:, c, 256:384], [bw, B_hnT], last=(uu == 3 and c == KC - 1),
                                             start=(c == 0), stop=(c == KC - 1))
                                  ck("vmm")
                                  pv = ps[0][:, :].rearrange("p (n c) -> p n c", c=128)
                                  copy_ps(VA0[:, u0:u0 + 4, 0:64], pv[:, :, 0:64], [ps[1]], [B_VA], act)
                                  ck("vcpa")
                                  copy_ps(V0B[:, u0:u0 + 4, 64:128], pv[:, :, 64:128], [ps[1]], [B_VB], dve)
                              sA = -8.0 * SL_DIL[g * 4 + 2 * jp] * r
                              sB = -8.0 * SL_DIL[g * 4 + 2 * jp + 1] * r

                              def epi_dil(pso, qt, g=g, r=r):
                                  nat = nat128(qt, r)
                                  pv = pso[0][:, 0:256].rearrange("p (t q) -> p t q", t=2)
                                  if g == 0:
                                      copy_ps(acc[:, :, nat], pv, [pso[1]], [B_acc], act)
                                  else:
                                      P.emit(dve, lambda: nc.vector.tensor_tensor(out=acc[:, :, nat], in0=pv,
                                                                                  in1=acc[:, :, nat], op=ALU.add),
                                             reads=[pso[1], B_acc], writes=[B_acc])

                              ck("vproj%d" % g)
                              attention(r, 1, sA, sB, epi_dil)
                              ck("att%d" % g)
                          P.emit(dve, lambda: nc.vector.reciprocal(acc[:, 1, :], acc[:, 1, :]), reads=[B_acc],
                                 writes=[B_acc])
                          P.emit(dve, lambda: nc.vector.tensor_tensor(out=OT[:, jp, :], in0=acc[:, 0, :],
                                                                      in1=acc[:, 1, :], op=ALU.mult),
                                 reads=[B_acc], writes=[B_OT[jp]])

                      for m in range(8):
                          g = m // 2
                          if m % 2 == 0:
                              wkv, bkv, dskv = wkv_ring.next()
                              P.dma(pool, dskv, wkv[:], wkvw_d[l, g].rearrange("(k p) c -> p k c", p=128),
                                    writes=[bkv])
                              proj_k(wkv, bkv, 0, 1)
                              for u0 in range(0, 16, 8):
                                  ps = next_ps()
                                  for uu in range(8):
                                      for c in range(KC):
                                          mm(ps, ps[0][:, uu * 64:(uu + 1) * 64], perm128(c, u0 + uu, 1),
                                             wkv[:, c, 128:192], [bkv, B_hnT], last=(uu == 7 and c == KC - 1),
                                             start=(c == 0), stop=(c == KC - 1))
                                  pv = ps[0][:, :].rearrange("p (n c) -> p n c", c=64)
                                  copy_ps(VA0[:, u0:u0 + 8, 0:64], pv, [ps[1]], [B_VA], act)
                                  copy_ps(V0B[:, u0:u0 + 8, 64:128], pv, [ps[1]], [B_VB], dve)
                          wq, bq, dsq = wq_ring.next()
                          P.dma(pool, dsq, wq[:], wqw_d[l, m].rearrange("(k p) c -> p k c", p=128), writes=[bq])
                          proj_q(wq, bq, 0, 1)

                          def epi_win(pso, qt, m=m):
                              dn, bdn = dn_ring.next()
                              P.emit(dve, lambda: nc.vector.tensor_scalar(
                                  out=dn[:], in0=pso[0][:, 128:256], scalar1=esink[:, l * 8 + m:l * 8 + m + 1],
                                  scalar2=None, op0=ALU.add), reads=[pso[1], B_esink], writes=[bdn])
                              P.emit(dve, lambda: nc.vector.reciprocal(dn[:], dn[:]), reads=[bdn], writes=[bdn])
                              P.emit(dve, lambda: nc.vector.tensor_tensor(
                                  out=OT[:, 2 + m, qt * 128:(qt + 1) * 128], in0=pso[0][:, 0:128], in1=dn[:],
                                  op=ALU.mult), reads=[pso[1], bdn], writes=[B_OT[2 + m]])

                          attention(1, 0, -8.0 * SL_WIN[2 * m], -8.0 * SL_WIN[2 * m + 1], epi_win)
                          ck("win%d" % m)
                  P.barrier()

                  with ExitStack() as ph3:
                      def sb3(shape, dt):
                          return ph3.enter_context(nc.sbuf_tensor(uname(), list(shape), dt))

                      wo = sb3([128, KC, D], BF16)
                      B_wo, ds_wo = Buf("wo"), P.dsem("wo")
                      P.dma(pool, ds_wo, wo[:], wout_d[l].rearrange("(k p) c -> p k c", p=128), writes=[B_wo])
                      tw_ring = Ring([(sb3([128, KC, 256], BF16), sb3([128, KC, 128], BF16), sb3([128, 2, 128], BF16),
                                       Buf("tw%d" % i), P.dsem("tw%d" % i)) for i in range(2)])
                      mg_ring = Ring([(sb3([128, KC, 512], BF16), Buf("mg%d" % i)) for i in range(1)])
                      g_ring = Ring([(sb3([128, 512], F32), Buf("g%d" % i)) for i in range(4)])
                      for tt in range(4):
                          tsl = slice(tt * 512, (tt + 1) * 512)
                          mg, bmg = mg_ring.next()
                          for j in range(8):
                              wtj, wbbj, wbaj, btw, dstw = tw_ring.next()
                              P.dma(pool, dstw, [wtj[:], wbbj[:], wbaj[:]],
                                    [wtail_d[l, j].rearrange("(k p) c -> p k c", p=128),
                                     wbb_d[l, j].rearrange("(k p) c -> p k c", p=128),
                                     wba_d[l, j].rearrange("(k p) c -> p k c", p=128)], writes=[btw])
                              gts = []
                              for b in range(2):
                                  ps = next_ps()
                                  for c in range(KC):
                                      mm(ps, ps[0][:, :], wtj[:, c, b * 128:(b + 1) * 128], hnT[:, c, tsl],
                                         [btw, B_hnT], last=(c == KC - 1))
                                  gt, bgt = g_ring.next()
                                  col = V_BG + l * 16 + b * 8 + j
                                  P.emit(act, lambda: nc.scalar.activation(out=gt[:], in_=ps[0][:, :], func=AF.Sigmoid,
                                                                           bias=vec[:, col:col + 1], scale=1.0),
                                         reads=[ps[1], B_const], writes=[bgt])
                                  gts.append((gt, bgt))
                              psa = next_ps()
                              for p_ in range(2):
                                  mm(psa, psa[0][:, :], wbaj[:, p_, :], OT[:, p_, tsl], [btw, B_OT[p_]], last=(p_ == 1))
                              P.emit(dve, lambda: nc.vector.tensor_tensor(out=gts[0][0][:], in0=psa[0][:, :],
                                                                          in1=gts[0][0][:], op=ALU.mult),
                                     reads=[psa[1], gts[0][1]], writes=[gts[0][1]])
                              psb = next_ps()
                              for mm_ in range(8):
                                  mm(psb, psb[0][:, :], wbbj[:, mm_, :], OT[:, 2 + mm_, tsl], [btw, B_OT[2 + mm_]],
                                     last=(mm_ == 7))
                              P.emit(dve, lambda: nc.vector.tensor_tensor(out=gts[1][0][:], in0=psb[0][:, :],
                                                                          in1=gts[1][0][:], op=ALU.mult),
                                     reads=[psb[1], gts[1][1]], writes=[gts[1][1]])
                              P.emit(dve, lambda: nc.vector.tensor_tensor(out=mg[:, j, :], in0=gts[0][0][:],
                                                                          in1=gts[1][0][:], op=ALU.add),
                                     reads=[gts[0][1], gts[1][1]], writes=[bmg])
                          for jo in range(8):
                              ps = next_ps()
                              for c in range(KC):
                                  mm(ps, ps[0][:, :], wo[:, c, jo * 128:(jo + 1) * 128], mg[:, c, :], [B_wo, bmg],
                                     last=(c == KC - 1))
                              P.emit(dve, lambda: nc.vector.tensor_tensor(out=hT[:, jo, tsl], in0=ps[0][:, :],
                                                                          in1=hT[:, jo, tsl], op=ALU.add),
                                     reads=[ps[1], B_hT], writes=[B_hT])
                  P.barrier()
              ck("mixer")

              with ExitStack() as ph:
                  def sbm(shape, dt):
                      return ph.enter_context(nc.sbuf_tensor(uname(), list(shape), dt))

                  hn2 = sbm([128, 16, D], BF16)
                  posmT = sbm([128, 256], F32)
                  gmT = sbm([128, 256], F32)
                  gmHL = sbm([128, 2, 256], BF16)
                  B_hn2, B_posmT, B_gmT, B_gmHL = Buf("hn2"), Buf("posmT"), Buf("gmT"), Buf("gmHL")
                  with ExitStack() as ph1:
                      def sb1(shape, dt):
                          return ph1.enter_context(nc.sbuf_tensor(uname(), list(shape), dt))

                      sq_ring = Ring([(sb1([128, 512], BF16), Buf("sq%d" % i)) for i in range(2)])
                      rs_ring = Ring([(sb1([128, 512], F32), Buf("rs%d" % i)) for i in range(2)])
                      rmsnorm(V_FFN + l * 8, sq_ring, rs_ring)
                      affT = sb1([NE, S], F32)
                      work = sb1([NE, S], F32)
                      t3 = sb1([NE, S], F32)
                      mx8 = sb1([NE, 8], F32)
                      B_aff, B_work, B_t3, B_mx = Buf("aff"), Buf("work"), Buf("t3"), Buf("mx8")
                      ex_ring = Ring([(sb1([NE, 512], F32), Buf("ex%d" % i)) for i in range(2)])
                      for tt in range(4):
                          tsl = slice(tt * 512, (tt + 1) * 512)
                          ps = next_ps()
                          for c in range(KC):
                              mm(ps, ps[0][0:NE, :], wr_sb[:, l, c, :], hnT[:, c, tsl], [B_wr, B_hnT],
                                 last=(c == KC - 1))
                          ex, bex = ex_ring.next()
                          P.emit(act, lambda: nc.scalar.activation(out=ex[:], in_=ps[0][0:NE, :], func=AF.Exp),
                                 reads=[ps[1]], writes=[bex])
                          ps2 = next_ps()
                          mm(ps2, ps2[0][0:NE, :], ones16[:], ex[:], [bex, B_ones], last=True)
                          P.emit(dve, lambda: nc.vector.reciprocal(affT[:, tsl], ps2[0][0:NE, :]), reads=[ps2[1]],
                                 writes=[B_aff])
                          P.emit(dve, lambda: nc.vector.tensor_tensor(out=affT[:, tsl], in0=ex[:], in1=affT[:, tsl],
                                                                      op=ALU.mult), reads=[bex, B_aff], writes=[B_aff])
                      for tc in range(16):
                          for half in range(2):
                              ps = next_ps()
                              for j in range(4):
                                  c = half * 4 + j
                                  mm(ps, ps[0][:, j * 128:(j + 1) * 128], hnT[:, c, tc * 128:(tc + 1) * 128], identB[:],
                                     [B_hnT, B_c2], last=(j == 3), start=True, stop=True)
                              copy_ps(hn2[:, tc, half * 512:(half + 1) * 512], ps[0][:, :], [ps[1]], [B_hn2], act)
                      src = affT
                      bsrc = B_aff
                      for it in range(CAP // 8):
                          P.emit(dve, lambda: nc.vector.max(out=mx8[:], in_=src[:]), reads=[bsrc], writes=[B_mx])
                          P.emit(dve, lambda: nc.vector.match_replace(out=work[:], in_to_replace=mx8[:],
                                                                      in_values=src[:], imm_value=0.0),
                                 reads=[B_mx, bsrc], writes=[B_work])
                          src, bsrc = work, B_work
                      P.emit(dve, lambda: nc.vector.tensor_tensor(out=work[:], in0=affT[:], in1=work[:],
                                                                  op=ALU.subtract),
                             reads=[B_aff, B_work], writes=[B_work])
                      P.emit(dve, lambda: nc.vector.tensor_single_scalar(out=affT[:], in_=work[:], scalar=0.0,
                                                                         op=ALU.is_gt),
                             reads=[B_work], writes=[B_aff])
                      P.emit(dve, lambda: nc.vector.tensor_tensor_scan(out=t3[:], data0=affT[:], data1=affT[:],
                                                                       initial=0.0, op0=ALU.add, op1=ALU.max),
                             reads=[B_aff], writes=[B_t3])
                      P.emit(dve, lambda: nc.vector.tensor_tensor(out=t3[:], in0=t3[:], in1=affT[:], op=ALU.mult),
                             reads=[B_t3, B_aff], writes=[B_t3])
                      for (srcT, bsrcT, dstT, bdstT) in ((t3, B_t3, posmT, B_posmT), (work, B_work, gmT, B_gmT)):
                          ps = next_ps()
                          for tc in range(16):
                              mm(ps, ps[0][:, tc * 16:(tc + 1) * 16], srcT[:, tc * 128:(tc + 1) * 128],
                                 identF[0:NE, 0:NE], [bsrcT, B_c2], last=(tc == 15), start=True, stop=True)
                          copy_ps(dstT[:, :], ps[0][:, 0:256], [ps[1]], [bdstT], act)
                      P.emit(dve, lambda: nc.vector.tensor_copy(out=gmHL[:, 0, :], in_=gmT[:, :]), reads=[B_gmT],
                             writes=[B_gmHL])
                      P.emit(dve, lambda: nc.vector.tensor_tensor(out=gmT[:, :], in0=gmT[:, :], in1=gmHL[:, 0, :],
                                                                  op=ALU.subtract),
                             reads=[B_gmT, B_gmHL], writes=[B_gmT])
                      P.emit(dve, lambda: nc.vector.tensor_copy(out=gmHL[:, 1, :], in_=gmT[:, :]), reads=[B_gmT],
                             writes=[B_gmHL])
                  P.barrier()
                  ck("route")
                  PTg = hnT[:, 0:2, :]
                  Pm = hnT[:, 2:4, :].rearrange("p a (b c) -> p (a b) c", c=256)
                  xeT = hnT[:, 4, :].rearrange("p (a c) -> p a c", c=256)
                  hm = hnT[:, 5, :].rearrange("p (a c) -> p a c", c=256)
                  yeb = hnT[:, 6, :].rearrange("p (k d) -> p k d", k=2)
                  B_PT, B_P, B_xe, B_hm, B_ye = Buf("PT"), Buf("P"), Buf("xe"), Buf("hm"), Buf("ye")
                  we_ring = Ring([(sbm([128, KC, D], BF16), Buf("we%d" % i), P.dsem("we%d" % i)) for i in range(3)])
                  s_ring = Ring([(sbm([128, 256], F32), Buf("s%d" % i)) for i in range(2)])
                  gc_ring = Ring([(sbm([128, 2], F32), Buf("gc%d" % i)) for i in range(2)])
                  for e in range(NE):
                      ws = []
                      for wd_ in (weg_d, weu_d, wed_d):
                          wt, bw, dsw = we_ring.next()
                          P.dma(pool, dsw, [wt[:, 0:4, :], wt[:, 4:8, :]],
                                [wd_[l, e, 0:512, :].rearrange("(k p) c -> p k c", p=128),
                                 wd_[l, e, 512:1024, :].rearrange("(k p) c -> p k c", p=128)], writes=[bw])
                          ws.append((wt, bw))
                      (wg, bwg), (wu, bwu), (wdn, bwd) = ws
                      for tc in range(16):
                          P.emit(dve, lambda: nc.vector.tensor_scalar(
                              out=Pm[:, tc, :], in0=iota1[:, :], scalar1=posmT[:, tc * 16 + e:tc * 16 + e + 1],
                              scalar2=None, op0=ALU.is_equal), reads=[B_posmT, B_c2], writes=[B_P])
                      for k in range(2):
                          for tt in range(4):
                              ps = next_ps()
                              for j in range(4):
                                  tc = tt * 4 + j
                                  mm(ps, ps[0][:, j * 128:(j + 1) * 128], Pm[:, tc, k * 128:(k + 1) * 128], identB[:],
                                     [B_P, B_c2], last=(j == 3), start=True, stop=True)
                              copy_ps(PTg[:, k, tt * 512:(tt + 1) * 512], ps[0][:, :], [ps[1]], [B_PT])
                      psg = next_ps()
                      for k in range(2):
                          for tc in range(16):
                              mm(psg, psg[0][:, 2 * k:2 * k + 2], Pm[:, tc, k * 128:(k + 1) * 128],
                                 gmHL[:, :, tc * 16 + e], [B_P, B_gmHL], last=(k == 1 and tc == 15), start=(tc == 0), stop=(tc == 15))
                      gc, bgc = gc_ring.next()
                      P.emit(dve, lambda: nc.vector.reduce_sum(
                          out=gc[:, :], in_=psg[0][:, 0:4].rearrange("p (k h) -> p k h", h=2), axis=AX.X),
                          reads=[psg[1]], writes=[bgc])
                      for half in range(4):
                          ps = next_ps()
                          for j in range(2):
                              c = half * 2 + j
                              for tc in range(16):
                                  mm(ps, ps[0][:, j * 256:(j + 1) * 256], hn2[:, tc, c * 128:(c + 1) * 128], Pm[:, tc, :],
                                     [B_hn2, B_P], last=(j == 1 and tc == 15), start=(tc == 0), stop=(tc == 15))
                          copy_ps(xeT[:, half * 2:half * 2 + 2, :], ps[0][:, :].rearrange("p (a c) -> p a c", c=256),
                                  [ps[1]], [B_xe], act)
                      for f in range(8):
                          ps = next_ps()
                          for c in range(KC):
                              mm(ps, ps[0][:, 0:256], wg[:, c, f * 128:(f + 1) * 128], xeT[:, c, :], [bwg, B_xe],
                                 start=(c == 0), stop=(c == KC - 1))
                          for c in range(KC):
                              mm(ps, ps[0][:, 256:512], wu[:, c, f * 128:(f + 1) * 128], xeT[:, c, :], [bwu, B_xe],
                                 last=(c == KC - 1), start=(c == 0))
                          st, bst = s_ring.next()
                          P.emit(act, lambda: nc.scalar.activation(out=st[:], in_=ps[0][:, 0:256], func=AF.Silu),
                                 reads=[ps[1]], writes=[bst])
                          P.emit(dve, lambda: nc.vector.tensor_tensor(out=hm[:, f, :], in0=ps[0][:, 256:512], in1=st[:],
                                                                      op=ALU.mult),
                                 reads=[ps[1], bst], writes=[B_hm])
                      for k in range(2):
                          for dh in range(2):
                              ps = next_ps()
                              for f in range(8):
                                  mm(ps, ps[0][:, :], hm[:, f, k * 128:(k + 1) * 128], wdn[:, f, dh * 512:(dh + 1) * 512],
                                     [bwd, B_hm], last=(f == 7))
                              P.emit(act, lambda: nc.scalar.activation(out=yeb[:, k, dh * 512:(dh + 1) * 512],
                                                                       in_=ps[0][:, :], func=AF.Copy,
                                                                       scale=gc[:, k:k + 1]),
                                     reads=[ps[1], bgc], writes=[B_ye])
                      for jo in range(8):
                          for tt in range(4):
                              tsl = slice(tt * 512, (tt + 1) * 512)
                              ps = next_ps()
                              for k in range(2):
                                  mm(ps, ps[0][:, :], yeb[:, k, jo * 128:(jo + 1) * 128], PTg[:, k, tsl], [B_ye, B_PT],
                                     last=(k == 1))
                              P.emit(dve, lambda: nc.vector.tensor_tensor(out=hT[:, jo, tsl], in0=ps[0][:, :],
                                                                          in1=hT[:, jo, tsl], op=ALU.add),
                                     reads=[ps[1], B_hT], writes=[B_hT])
                  P.barrier()

          with ExitStack() as ph:
              def sbf(shape, dt):
                  return ph.enter_context(nc.sbuf_tensor(uname(), list(shape), dt))

              sq_ring = Ring([(sbf([128, 512], BF16), Buf("sq%d" % i)) for i in range(2)])
              rs_ring = Ring([(sbf([128, 512], F32), Buf("rs%d" % i)) for i in range(2)])
              stage_ring = Ring([(sbf([128, 512], F32), Buf("stg%d" % i), P.dsem("out%d" % i)) for i in range(3)])
              rmsnorm(V_FIN, sq_ring, rs_ring, final_seq=s, stage_ring=stage_ring)
              P.barrier()

    except _Stop:
        P.barrier()
        for c in range(KC):
            P.dma(sp, ds_out, outT[0, c * 128:(c + 1) * 128, :], hT[:, c, :], reads=[B_hT])

    for d in P.dsems:
        if d.val and sp.waited.get(d.key, 0) < d.val:
            sp.h.wait_ge(d.sem, d.val)
    nc._keep_es = es if False else None; globals().setdefault("_KEEP", []).append(es)
    return nc


def _tables():
    kk = np.arange(128)[:, None]
    qq = np.arange(128)[None, :]
    tabs = np.zeros((128, 2, 3, 128), np.float32)
    for ti in range(3):
        rel = np.abs(kk + (ti - 1) * 128 - qq).astype(np.float32)
        tabs[:, 0, ti, :] = np.where(rel <= 128, rel, 1e6)
        tabs[:, 1, ti, :] = np.where(rel <= 64, rel, 1e6)
    cst2 = np.zeros((128, 384), np.float32)
    cst2[:, 0:128] = np.eye(128, dtype=np.float32)
    cst2[:, 128:384] = np.arange(1, 257, dtype=np.float32)[None, :]
    return tabs.reshape(128, -1), cst2


def prep_weights(norm_mix, w_in, w_branch_a, w_branch_b, b_gate, sink_logit, w_out, norm_ffn, w_router,
                 w_expert_gate, w_expert_up, w_expert_down, norm_final, L=DEPTH):
    f = np.float32
    w_in = np.asarray(w_in, f)
    wa = np.empty((L, 6, D, 384), f)
    for jp in range(2):
        for g in range(3):
            c0 = g * 256 + jp * 128
            u = jp * 3 + g
            wa[:, u, :, 0:128] = w_in[:L, :, c0:c0 + 128]
            wa[:, u, :, 128:256] = w_in[:L, :, 768 + c0:768 + c0 + 128]
            wa[:, u, :, 256:384] = w_in[:L, :, 1536 + c0:1536 + c0 + 128]
    q0, k0, v0, g0 = 2304, 3328, 3584, 3840
    wqw = np.ascontiguousarray(w_in[:L, :, q0:q0 + 1024].reshape(L, D, 8, 128).transpose(0, 2, 1, 3))
    wkvw = np.empty((L, 4, D, 192), f)
    for g in range(4):
        wkvw[:, g, :, 0:64] = w_in[:L, :, k0 + g * 64:k0 + (g + 1) * 64]
        wkvw[:, g, :, 64:128] = w_in[:L, :, k0 + g * 64:k0 + (g + 1) * 64]
        wkvw[:, g, :, 128:192] = w_in[:L, :, v0 + g * 64:v0 + (g + 1) * 64]
    wtail = np.empty((L, 8, D, 256), f)
    for j in range(8):
        wtail[:, j, :, 0:128] = w_in[:L, :, g0 + j * 128:g0 + (j + 1) * 128]
        wtail[:, j, :, 128:256] = w_in[:L, :, g0 + 1024 + j * 128:g0 + 1024 + (j + 1) * 128]
    wba = np.ascontiguousarray(np.asarray(w_branch_a, f)[:L].reshape(L, 256, 8, 128).transpose(0, 2, 1, 3))
    wbb = np.ascontiguousarray(np.asarray(w_branch_b, f)[:L].reshape(L, D, 8, 128).transpose(0, 2, 1, 3))

    def pm(v):
        v = np.asarray(v, f)
        return v.reshape(v.shape[:-1] + (8, 128))

    NV = L * 40 + 8
    vecs = np.zeros((128, NV), f)
    vecs[:, 0:L * 8] = pm(norm_mix)[:L].transpose(2, 0, 1).reshape(128, L * 8)
    vecs[:, L * 8:L * 16] = pm(norm_ffn)[:L].transpose(2, 0, 1).reshape(128, L * 8)
    bg = np.asarray(b_gate, f)[:L].reshape(L, 2, 8, 128)
    vecs[:, L * 16:L * 32] = bg.transpose(3, 0, 1, 2).reshape(128, L * 16)
    vecs[:, L * 32:L * 32 + 8] = pm(norm_final).transpose(1, 0)
    sk = np.asarray(sink_logit, f)[:L].reshape(L, 8, 2)
    sp = np.repeat(sk.transpose(2, 0, 1)[:, None], 64, axis=1).reshape(128, L * 8)
    vecs[:, L * 32 + 8:] = sp
    tabs, cst2 = _tables()
    return {
        "wa": wa, "wqw": wqw, "wkvw": wkvw, "wtail": wtail, "wba": wba, "wbb": wbb,
        "wout": np.ascontiguousarray(np.asarray(w_out, f)[:L]),
        "wr": np.ascontiguousarray(np.asarray(w_router, f)[:L]),
        "weg": np.ascontiguousarray(np.asarray(w_expert_gate, f)[:L]),
        "weu": np.ascontiguousarray(np.asarray(w_expert_up, f)[:L]),
        "wed": np.ascontiguousarray(np.asarray(w_expert_down, f)[:L]),
        "vecs": vecs, "tabs": tabs, "cst2": cst2,
    }


def kernel(x, norm_mix, w_in, w_branch_a, w_branch_b, b_gate, sink_logit, w_out, norm_ffn, w_router,
           w_expert_gate, w_expert_up, w_expert_down, norm_final):
    x = np.asarray(x, np.float32)
    wts = prep_weights(norm_mix, w_in, w_branch_a, w_branch_b, b_gate, sink_logit, w_out, norm_ffn, w_router,
                       w_expert_gate, w_expert_up, w_expert_down, norm_final)
    nc = build_program(DEPTH, NSEQ)
    in_maps = []
    for c in range(NCORES):
        m = dict(wts)
        m["xT"] = np.ascontiguousarray(x[c * NSEQ:(c + 1) * NSEQ].transpose(0, 2, 1))
        in_maps.append(m)
    res = run_bass_kernel_spmd(nc, in_maps, core_ids=list(range(NCORES)))
    out = np.empty((NCORES * NSEQ, S, D), np.float32)
    for c in range(NCORES):
        out[c * NSEQ:(c + 1) * NSEQ] = res.results[c]["outT"].transpose(0, 2, 1)
    return out
```

```python
import numpy as np
from contextlib import ExitStack
import concourse.bass as bass
import concourse.mybir as mybir
from concourse.bass_utils import run_bass_kernel_spmd

F32 = mybir.dt.float32
BF16 = mybir.dt.bfloat16
AF = mybir.ActivationFunctionType
ALU = mybir.AluOpType
AX = mybir.AxisListType

D = 1024
S = 2048
DEPTH = 4
NCORES = 8
NSEQ = 4
KC = 8
NE = 16
CAP = 256
EPS = 1e-6
DIL_R = (1, 4, 16)


def _slopes(n):
    return [float(2.0 ** (-8.0 * i / n)) for i in range(1, n + 1)]


SL_DIL = _slopes(12)
SL_WIN = _slopes(16)


class Ev:
    __slots__ = ("sem", "val", "key")

    def __init__(self, sem, val, key):
        self.sem, self.val, self.key = sem, val, key


class Buf:
    __slots__ = ("name", "w", "r", "excl")

    def __init__(self, name, excl=False):
        self.name = name
        self.w = None
        self.r = {}
        self.excl = excl


class Eng:
    def __init__(self, name, h, sem, is_pe=False):
        self.name, self.h, self.sem, self.is_pe = name, h, sem, is_pe
        self.cnt = 0
        self.waited = {}


class DSem:
    def __init__(self, sem, key):
        self.sem, self.key, self.val = sem, key, 0


class Prog:
    def __init__(self, nc, es):
        self.nc = nc
        self.es = es
        mk = lambda n: es.enter_context(nc.semaphore(n))
        self.pe = Eng("pe", nc.tensor, mk("s_pe"), True)
        self.act = Eng("act", nc.scalar, mk("s_act"))
        self.dve = Eng("dve", nc.vector, mk("s_dve"))
        self.pool = Eng("pool", nc.gpsimd, mk("s_pool"))
        self.sp = Eng("sp", nc.sync, mk("s_sp"))
        self.engs = [self.pe, self.act, self.dve, self.pool, self.sp]
        self.dsems = []
        self.dtags = {}
        self.nds = 0

    def dsem(self, tag=None):
        if tag is not None and tag in self.dtags:
            return self.dtags[tag]
        self.nds += 1
        d = DSem(self.es.enter_context(self.nc.semaphore("s_dma%d" % self.nds)), "dma%d" % self.nds)
        self.dsems.append(d)
        if tag is not None:
            self.dtags[tag] = d
        return d

    def need(self, eng, ev, raw):
        if ev is None:
            return
        if ev.key == eng.name and eng.is_pe:
            return
        if eng.waited.get(ev.key, 0) >= ev.val:
            return
        eng.h.wait_ge(ev.sem, ev.val)
        eng.waited[ev.key] = ev.val

    def deps(self, eng, reads, writes):
        for b in reads:
            self.need(eng, b.w, True)
            if b.excl:
                for ev in b.r.values():
                    if ev.key != eng.name:
                        self.need(eng, ev, False)
        for b in writes:
            self.need(eng, b.w, False)
            for ev in b.r.values():
                self.need(eng, ev, False)

    def mark(self, ev, reads, writes):
        for b in writes:
            b.w = ev
            b.r = {}
        for b in reads:
            o = b.r.get(ev.key)
            if o is None or o.val < ev.val:
                b.r[ev.key] = ev

    def emit(self, eng, fn, reads=(), writes=(), signal=True):
        self.deps(eng, reads, writes)
        ins = fn()
        if signal:
            eng.cnt += 1
            ins.then_inc(eng.sem, 1)
            ev = Ev(eng.sem, eng.cnt, eng.name)
        else:
            ev = Ev(eng.sem, eng.cnt + 1, eng.name)
        self.mark(ev, reads, writes)
        return ins

    def dma(self, q, ds, out_ap, in_ap, reads=(), writes=()):
        self.deps(q, reads, writes)
        if not isinstance(out_ap, list):
            out_ap, in_ap = [out_ap], [in_ap]
        for o, i in zip(out_ap, in_ap):
            ds.val += 16
            q.h.dma_start(out=o, in_=i).then_inc(ds.sem, 16)
        ev = Ev(ds.sem, ds.val, ds.key)
        self.mark(ev, reads, writes)

    def barrier(self):
        for e in self.engs:
            for f in self.engs:
                if f is e or f.cnt == 0:
                    continue
                if e.waited.get(f.name, 0) < f.cnt:
                    e.h.wait_ge(f.sem, f.cnt)
                    e.waited[f.name] = f.cnt
            for d in self.dsems:
                if d.val and e.waited.get(d.key, 0) < d.val:
                    e.h.wait_ge(d.sem, d.val)
                    e.waited[d.key] = d.val


class _Stop(Exception):
    pass


class Ring:
    def __init__(self, items):
        self.items = items
        self.i = -1

    def next(self):
        self.i = (self.i + 1) % len(self.items)
        return self.items[self.i]


def build_program(L=DEPTH, NS=NSEQ, stop_after=None):
    nc = bass.Bass("TRN2", target_bir_lowering=False)
    es = ExitStack()
    P = Prog(nc, es)
    pe, act, dve, pool, sp = P.pe, P.act, P.dve, P.pool, P.sp

    def din(name, shape):
        return nc.dram_tensor(name, list(shape), F32, kind="ExternalInput").ap()

    NV = L * 8 + L * 8 + L * 16 + 8 + L * 8
    V_MIX, V_FFN, V_BG, V_FIN, V_SINK = 0, L * 8, L * 16, L * 32, L * 32 + 8
    xT = din("xT", [NS, D, S])
    wa_d = din("wa", [L, 6, D, 384])
    wqw_d = din("wqw", [L, 8, D, 128])
    wkvw_d = din("wkvw", [L, 4, D, 192])
    wtail_d = din("wtail", [L, 8, D, 256])
    wba_d = din("wba", [L, 8, 256, 128])
    wbb_d = din("wbb", [L, 8, D, 128])
    wout_d = din("wout", [L, D, D])
    wr_d = din("wr", [L, D, NE])
    weg_d = din("weg", [L, NE, D, D])
    weu_d = din("weu", [L, NE, D, D])
    wed_d = din("wed", [L, NE, D, D])
    vecs_d = din("vecs", [128, NV])
    tabs_d = din("tabs", [128, 2 * 3 * 128])
    cst2_d = din("cst2", [128, 384])
    outT = nc.dram_tensor("outT", [NS, D, S], F32, kind="ExternalOutput").ap()

    def sb(name, shape, dt):
        return es.enter_context(nc.sbuf_tensor(name, list(shape), dt))

    uctr = [0]

    def uname():
        uctr[0] += 1
        return "t%d" % uctr[0]

    hT = sb("hT", [128, KC, S], F32)
    hnT = sb("hnT", [128, KC, S], BF16)
    vec = sb("vec", [128, NV], F32)
    esink = sb("esink", [128, L * 8], F32)
    tab = sb("tab", [128, 2, 3, 128], F32)
    ones16 = sb("ones16", [NE, NE], F32)
    onesF = sb("onesF", [128, 128], BF16)
    onesA0 = sb("onesA0", [128, 128], BF16)
    ones0B = sb("ones0B", [128, 128], BF16)
    wr_sb = sb("wr_sb", [128, L, KC, NE], BF16)
    epsb = sb("epsb", [128, 1], F32)
    B_hT, B_hnT, B_const = Buf("hT"), Buf("hnT"), Buf("const")
    B_wr = Buf("wr")
    identF = sb("identF", [128, 128], F32)
    iota1 = sb("iota1", [128, 256], F32)
    identB = sb("identB", [128, 128], BF16)
    B_c2 = Buf("c2")

    PSB = []
    for i in range(8):
        t = es.enter_context(nc.psum_tensor("ps%d" % i, [128, 512], F32))
        PSB.append((t, Buf("ps%d" % i, excl=True)))
    ps_ring = Ring(PSB)
    fresh = {}

    def next_ps():
        t, b = ps_ring.next()
        fresh[b.name] = True
        return t, b

    def mm(psb, out_ap, lhsT, rhs, reads, last=False, sig=False, start=None, stop=None):
        b = psb[1]
        st = fresh[b.name] if start is None else start
        fresh[b.name] = False
        sp_ = last if stop is None else (stop or last)
        P.emit(pe, lambda: nc.tensor.matmul(out_ap, lhsT, rhs, start=st, stop=sp_),
               reads=reads, writes=[b], signal=(last or sig))

    ds_c = P.dsem()
    P.dma(sp, ds_c, [vec[:, :], tab[:].rearrange("p a b c -> p (a b c)")],
          [vecs_d[:, :], tabs_d[:, :]], writes=[B_const])
    ds_c2 = P.dsem()
    P.dma(sp, ds_c2, [identF[:, :], iota1[:, :]], [cst2_d[:, 0:128], cst2_d[:, 128:384]], writes=[B_c2])
    ds_c3 = P.dsem()
    P.dma(pool, ds_c3, identB[:, :], cst2_d[:, 0:128], writes=[B_c2])
    ds_wr = P.dsem()
    P.dma(pool, ds_wr, [wr_sb[:, l, :, :] for l in range(L)],
          [wr_d[l].rearrange("(k p) e -> p k e", p=128) for l in range(L)], writes=[B_wr])
    B_ones = Buf("ones")
    P.emit(dve, lambda: nc.vector.memset(onesF[:], 1.0), writes=[B_ones])
    P.emit(dve, lambda: nc.vector.memset(onesA0[:], 0.0), writes=[B_ones])
    P.emit(dve, lambda: nc.vector.memset(ones0B[:], 0.0), writes=[B_ones])
    P.emit(dve, lambda: nc.vector.memset(onesA0[:, 0:64], 1.0), writes=[B_ones])
    P.emit(dve, lambda: nc.vector.memset(ones0B[:, 64:128], 1.0), writes=[B_ones])
    P.emit(dve, lambda: nc.vector.memset(ones16[:], 1.0), writes=[B_ones])
    P.emit(dve, lambda: nc.vector.memset(epsb[:], EPS), writes=[B_ones])
    B_esink = Buf("esink")
    P.emit(act, lambda: nc.scalar.activation(out=esink[:], in_=vec[:, V_SINK:V_SINK + L * 8], func=AF.Exp),
           reads=[B_const], writes=[B_esink])

    ds_x = P.dsem("x")
    ds_out = P.dsem("dbg")

    def rmsnorm(gcol, sq_ring, rs_ring, final_seq=None, stage_ring=None):
        for tt in range(4):
            tsl = slice(tt * 512, (tt + 1) * 512)
            ps = next_ps()
            for c in range(KC):
                sq, bsq = sq_ring.next()
                P.emit(act, lambda: nc.scalar.activation(out=sq[:], in_=hT[:, c, tsl], func=AF.Square),
                       reads=[B_hT], writes=[bsq])
                mm(ps, ps[0][:, :], onesF[:], sq[:], [bsq, B_ones], last=(c == KC - 1), sig=True)
            rs, brs = rs_ring.next()
            P.emit(act, lambda: nc.scalar.activation(out=rs[:], in_=ps[0][:, :], func=AF.Sqrt,
                                                     scale=1.0 / D, bias=epsb[:, 0:1]),
                   reads=[ps[1], B_ones], writes=[brs])
            P.emit(dve, lambda: nc.vector.reciprocal(rs[:], rs[:]), reads=[brs], writes=[brs])
            for c in range(KC):
                if final_seq is None:
                    P.emit(dve, lambda: nc.vector.scalar_tensor_tensor(
                        out=hnT[:, c, tsl], in0=hT[:, c, tsl], scalar=vec[:, gcol + c:gcol + c + 1],
                        in1=rs[:], op0=ALU.mult, op1=ALU.mult),
                        reads=[B_hT, brs, B_const], writes=[B_hnT])
                else:
                    st, bst, dso = stage_ring.next()
                    P.emit(dve, lambda: nc.vector.scalar_tensor_tensor(
                        out=st[:], in0=hT[:, c, tsl], scalar=vec[:, gcol + c:gcol + c + 1],
                        in1=rs[:], op0=ALU.mult, op1=ALU.mult),
                        reads=[B_hT, brs, B_const], writes=[bst])
                    P.dma(sp, dso, outT[final_seq, c * 128:(c + 1) * 128, tsl], st[:], reads=[bst])

    def perm512(c, ut, r):
        if r == 1:
            return hnT[:, c, ut * 512:(ut + 1) * 512]
        if r == 4:
            return hnT[:, c, ut:S:4]
        return hnT[:, c, :].rearrange("p (a q) -> p q a", q=16)[:, 4 * ut:4 * ut + 4, :]

    def perm128(c, uc, r):
        if r == 1:
            return hnT[:, c, uc * 128:(uc + 1) * 128]
        if r == 4:
            sub, a0 = uc // 4, (uc % 4) * 128
            return hnT[:, c, sub + 4 * a0:sub + 4 * (a0 + 127) + 1:4]
        return hnT[:, c, uc:S:16]

    def nat128(qt, r):
        if r == 1:
            return slice(qt * 128, (qt + 1) * 128)
        if r == 4:
            sub, a0 = qt // 4, (qt % 4) * 128
            return slice(sub + 4 * a0, sub + 4 * (a0 + 127) + 1, 4)
        return slice(qt, S, 16)

    def ps_view(ps, r):
        if r == 16:
            return ps[0][:, :].rearrange("p (q a) -> p q a", q=4)
        return ps[0][:, :]

    cp_flip = [0]

    def copy_ps(out_ap, in_ap, reads, writes, eng=None):
        if eng is None:
            cp_flip[0] ^= 1
            eng = act if cp_flip[0] else dve
        if eng is act:
            P.emit(act, lambda: nc.scalar.copy(out=out_ap, in_=in_ap), reads=reads, writes=writes)
        else:
            P.emit(dve, lambda: nc.vector.tensor_copy(out=out_ap, in_=in_ap), reads=reads, writes=writes)

    def ck(name):
        if stop_after == name:
            raise _Stop()

    try:
      for s in range(NS):
          P.dma(sp, ds_x, [hT[:, c, :] for c in range(KC)], [xT[s, c * 128:(c + 1) * 128, :] for c in range(KC)],
                writes=[B_hT])
          for l in range(L):
              with ExitStack() as ph:
                  def sbp(name, shape, dt):
                      return ph.enter_context(nc.sbuf_tensor(uname(), list(shape), dt))

                  sq_ring = Ring([(sbp("sq", [128, 512], BF16), Buf("sq%d" % i)) for i in range(2)])
                  rs_ring = Ring([(sbp("rs", [128, 512], F32), Buf("rs%d" % i)) for i in range(2)])
                  OT = sbp("OT", [128, 10, S], BF16)
                  B_OT = [Buf("OT%d" % i) for i in range(10)]
                  ck("load")
                  rmsnorm(V_MIX + l * 8, sq_ring, rs_ring)
                  ck("norm")

                  with ExitStack() as ph2:
                      def sb2(shape, dt):
                          return ph2.enter_context(nc.sbuf_tensor(uname(), list(shape), dt))

                      QA0, Q0B, K2 = sb2([128, S], BF16), sb2([128, S], BF16), sb2([128, S], BF16)
                      VA0, V0B = sb2([128, 16, 128], BF16), sb2([128, 16, 128], BF16)
                      acc = sb2([128, 2, S], F32)
                      B_QA, B_QB, B_K, B_VA, B_VB, B_acc = (Buf(n) for n in ("QA", "QB", "K", "VA", "VB", "acc"))
                      wa_ring = Ring([(sb2([128, KC, 384], BF16), Buf("wa%d" % i), P.dsem("wa%d" % i)) for i in range(1)])
                      wq_ring = Ring([(sb2([128, KC, 128], BF16), Buf("wq%d" % i), P.dsem("wq%d" % i)) for i in range(1)])
                      wkv_ring = Ring([(sb2([128, KC, 192], BF16), Buf("wkv%d" % i), P.dsem("wkv%d" % i)) for i in range(1)])
                      tS_ring = Ring([(sb2([128, 384], F32), Buf("tS%d" % i)) for i in range(2)])
                      PT_ring = Ring([(sb2([128, 384], BF16), Buf("PT%d" % i)) for i in range(4)])
                      dn_ring = Ring([(sb2([128, 128], F32), Buf("dn%d" % i)) for i in range(2)])
                      P.emit(dve, lambda: nc.vector.memset(QA0[64:128, :], 0.0), writes=[B_QA])
                      P.emit(dve, lambda: nc.vector.memset(Q0B[0:64, :], 0.0), writes=[B_QB])
                      P.emit(dve, lambda: nc.vector.memset(VA0[:, :, 64:128], 0.0), writes=[B_VA])
                      P.emit(dve, lambda: nc.vector.memset(V0B[:, :, 0:64], 0.0), writes=[B_VB])
                      ck("ph2alloc")

                      def proj_q(wt, bw, col0, r):
                          for ut in range(4):
                              ps = next_ps()
                              for c in range(KC):
                                  mm(ps, ps_view(ps, r), wt[:, c, col0:col0 + 128], perm512(c, ut, r),
                                     [bw, B_hnT], last=(c == KC - 1))
                              usl = slice(ut * 512, (ut + 1) * 512)
                              copy_ps(QA0[0:64, usl], ps[0][0:64, :], [ps[1]], [B_QA], act)
                              copy_ps(Q0B[64:128, usl], ps[0][64:128, :], [ps[1]], [B_QB], dve)

                      def proj_k(wt, bw, col0, r):
                          for ut in range(4):
                              ps = next_ps()
                              for c in range(KC):
                                  mm(ps, ps_view(ps, r), wt[:, c, col0:col0 + 128], perm512(c, ut, r),
                                     [bw, B_hnT], last=(c == KC - 1))
                              copy_ps(K2[:, ut * 512:(ut + 1) * 512], ps[0][:, :], [ps[1]], [B_K])

                      def attention(r, tabi, scA, scB, epilogue):
                          tps = (S // r) // 128

                          def stage1(qt):
                              jj = qt % tps
                              chunks = []
                              if jj > 0:
                                  chunks.append((qt - 1, 0))
                              chunks.append((qt, 1))
                              if jj < tps - 1:
                                  chunks.append((qt + 1, 2))
                              lo, hi = chunks[0][1] * 128, (chunks[-1][1] + 1) * 128
                              qsl = slice(qt * 128, (qt + 1) * 128)
                              pts = []
                              for (QX, BQ, sc) in ((QA0, B_QA, scA), (Q0B, B_QB, scB)):
                                  ps = next_ps()
                                  for i, (kc, ti) in enumerate(chunks):
                                      mm(ps, ps[0][:, ti * 128:(ti + 1) * 128], K2[:, kc * 128:(kc + 1) * 128],
                                         QX[:, qsl], [B_K, BQ], last=(i == len(chunks) - 1))
                                  tS, btS = tS_ring.next()
                                  P.emit(dve, lambda: nc.vector.scalar_tensor_tensor(
                                      out=tS[:, lo:hi], in0=tab[:, tabi].rearrange("p a b -> p (a b)")[:, lo:hi],
                                      scalar=sc, in1=ps[0][:, lo:hi], op0=ALU.mult, op1=ALU.add),
                                      reads=[ps[1], B_const], writes=[btS])
                                  PT, bPT = PT_ring.next()
                                  P.emit(act, lambda: nc.scalar.activation(out=PT[:, lo:hi], in_=tS[:, lo:hi],
                                                                           func=AF.Exp, scale=0.125),
                                         reads=[btS], writes=[bPT])
                                  pts.append((PT, bPT))
                              return chunks, pts

                          def stage2(qt, chunks, pts):
                              pso = next_ps()
                              n = 0
                              tot = 4 * len(chunks)
                              for (PT, bPT), VX, BV, oX in ((pts[0], VA0, B_VA, onesA0), (pts[1], V0B, B_VB, ones0B)):
                                  for (kc, ti) in chunks:
                                      n += 1
                                      mm(pso, pso[0][:, 0:128], VX[:, kc, :], PT[:, ti * 128:(ti + 1) * 128],
                                         [BV, bPT], last=False)
                                      n += 1
                                      mm(pso, pso[0][:, 128:256], oX[:], PT[:, ti * 128:(ti + 1) * 128],
                                         [B_ones, bPT], last=(n == tot))
                              epilogue(pso, qt)

                          cur = stage1(0)
                          for qt in range(16):
                              nxt = stage1(qt + 1) if qt + 1 < 16 else None
                              stage2(qt, *cur)
                              cur = nxt

                      for jp in range(2):
                          for g in range(3):
                              r = DIL_R[g]
                              wt, bw, dsw = wa_ring.next()
                              P.dma(pool, dsw, wt[:], wa_d[l, jp * 3 + g].rearrange("(k p) c -> p k c", p=128),
                                    writes=[bw])
                              ck("wadma")
                              proj_q(wt, bw, 0, r)
                              ck("projq")
                              proj_k(wt, bw, 128, r)
                              ck("proj%d" % g)
                              for u0 in range(0, 16, 4):
                                  ps = next_ps()
                                  for uu in range(4):
                                      for c in range(KC):
                                          mm(ps, ps[0][:, uu * 128:(uu + 1) * 128], perm128(c, u0 + uu, r),
                                             wt[:, c, 256:384], [bw, B_hnT], last=(uu == 3 and c == KC - 1),
                                             start=(c == 0), stop=(c == KC - 1))
                                  ck("vmm")
                                  pv = ps[0][:, :].rearrange("p (n c) -> p n c", c=128)
                                  copy_ps(VA0[:, u0:u0 + 4, 0:64], pv[:, :, 0:64], [ps[1]], [B_VA], act)
                                  ck("vcpa")
                                  copy_ps(V0B[:, u0:u0 + 4, 64:128], pv[:, :, 64:128], [ps[1]], [B_VB], dve)
                              sA = -8.0 * SL_DIL[g * 4 + 2 * jp] * r
                              sB = -8.0 * SL_DIL[g * 4 + 2 * jp + 1] * r

                              def epi_dil(pso, qt, g=g, r=r):
                                  nat = nat128(qt, r)
                                  pv = pso[0][:, 0:256].rearrange("p (t q) -> p t q", t=2)
                                  if g == 0:
                                      copy_ps(acc[:, :, nat], pv, [pso[1]], [B_acc], act)
                                  else:
                                      P.emit(dve, lambda: nc.vector.tensor_tensor(out=acc[:, :, nat], in0=pv,
                                                                                  in1=acc[:, :, nat], op=ALU.add),
                                             reads=[pso[1], B_acc], writes=[B_acc])

                              ck("vproj%d" % g)
                              attention(r, 1, sA, sB, epi_dil)
                              ck("att%d" % g)
                          P.emit(dve, lambda: nc.vector.reciprocal(acc[:, 1, :], acc[:, 1, :]), reads=[B_acc],
                                 writes=[B_acc])
                          P.emit(dve, lambda: nc.vector.tensor_tensor(out=OT[:, jp, :], in0=acc[:, 0, :],
                                                                      in1=acc[:, 1, :], op=ALU.mult),
                                 reads=[B_acc], writes=[B_OT[jp]])

                      for m in range(8):
                          g = m // 2
                          if m % 2 == 0:
                              wkv, bkv, dskv = wkv_ring.next()
                              P.dma(pool, dskv, wkv[:], wkvw_d[l, g].rearrange("(k p) c -> p k c", p=128),
                                    writes=[bkv])
                              proj_k(wkv, bkv, 0, 1)
                              for u0 in range(0, 16, 8):
                                  ps = next_ps()
                                  for uu in range(8):
                                      for c in range(KC):
                                          mm(ps, ps[0][:, uu * 64:(uu + 1) * 64], perm128(c, u0 + uu, 1),
                                             wkv[:, c, 128:192], [bkv, B_hnT], last=(uu == 7 and c == KC - 1),
                                             start=(c == 0), stop=(c == KC - 1))
                                  pv = ps[0][:, :].rearrange("p (n c) -> p n c", c=64)
                                  copy_ps(VA0[:, u0:u0 + 8, 0:64], pv, [ps[1]], [B_VA], act)
                                  copy_ps(V0B[:, u0:u0 + 8, 64:128], pv, [ps[1]], [B_VB], dve)
                          wq, bq, dsq = wq_ring.next()
                          P.dma(pool, dsq, wq[:], wqw_d[l, m].rearrange("(k p) c -> p k c", p=128), writes=[bq])
                          proj_q(wq, bq, 0, 1)

                          def epi_win(pso, qt, m=m):
                              dn, bdn = dn_ring.next()
                              P.emit(dve, lambda: nc.vector.tensor_scalar(
                                  out=dn[:], in0=pso[0][:, 128:256], scalar1=esink[:, l * 8 + m:l * 8 + m + 1],
                                  scalar2=None, op0=ALU.add), reads=[pso[1], B_esink], writes=[bdn])
                              P.emit(dve, lambda: nc.vector.reciprocal(dn[:], dn[:]), reads=[bdn], writes=[bdn])
                              P.emit(dve, lambda: nc.vector.tensor_tensor(
                                  out=OT[:, 2 + m, qt * 128:(qt + 1) * 128], in0=pso[0][:, 0:128], in1=dn[:],
                                  op=ALU.mult), reads=[pso[1], bdn], writes=[B_OT[2 + m]])

                          attention(1, 0, -8.0 * SL_WIN[2 * m], -8.0 * SL_WIN[2 * m + 1], epi_win)
                          ck("win%d" % m)
                  P.barrier()

                  with ExitStack() as ph3:
                      def sb3(shape, dt):
                          return ph3.enter_context(nc.sbuf_tensor(uname(), list(shape), dt))

                      wo = sb3([128, KC, D], BF16)
                      B_wo, ds_wo = Buf("wo"), P.dsem("wo")
                      P.dma(pool, ds_wo, wo[:], wout_d[l].rearrange("(k p) c -> p k c", p=128), writes=[B_wo])
                      tw_ring = Ring([(sb3([128, KC, 256], BF16), sb3([128, KC, 128], BF16), sb3([128, 2, 128], BF16),
                                       Buf("tw%d" % i), P.dsem("tw%d" % i)) for i in range(2)])
                      mg_ring = Ring([(sb3([128, KC, 512], BF16), Buf("mg%d" % i)) for i in range(1)])
                      g_ring = Ring([(sb3([128, 512], F32), Buf("g%d" % i)) for i in range(4)])
                      for tt in range(4):
                          tsl = slice(tt * 512, (tt + 1) * 512)
                          mg, bmg = mg_ring.next()
                          for j in range(8):
                              wtj, wbbj, wbaj, btw, dstw = tw_ring.next()
                              P.dma(pool, dstw, [wtj[:], wbbj[:], wbaj[:]],
                                    [wtail_d[l, j].rearrange("(k p) c -> p k c", p=128),
                                     wbb_d[l, j].rearrange("(k p) c -> p k c", p=128),
                                     wba_d[l, j].rearrange("(k p) c -> p k c", p=128)], writes=[btw])
                              gts = []
                              for b in range(2):
                                  ps = next_ps()
                                  for c in range(KC):
                                      mm(ps, ps[0][:, :], wtj[:, c, b * 128:(b + 1) * 128], hnT[:, c, tsl],
                                         [btw, B_hnT], last=(c == KC - 1))
                                  gt, bgt = g_ring.next()
                                  col = V_BG + l * 16 + b * 8 + j
                                  P.emit(act, lambda: nc.scalar.activation(out=gt[:], in_=ps[0][:, :], func=AF.Sigmoid,
                                                                           bias=vec[:, col:col + 1], scale=1.0),
                                         reads=[ps[1], B_const], writes=[bgt])
                                  gts.append((gt, bgt))
                              psa = next_ps()
                              for p_ in range(2):
                                  mm(psa, psa[0][:, :], wbaj[:, p_, :], OT[:, p_, tsl], [btw, B_OT[p_]], last=(p_ == 1))
                              P.emit(dve, lambda: nc.vector.tensor_tensor(out=gts[0][0][:], in0=psa[0][:, :],
                                                                          in1=gts[0][0][:], op=ALU.mult),
                                     reads=[psa[1], gts[0][1]], writes=[gts[0][1]])
                              psb = next_ps()
                              for mm_ in range(8):
                                  mm(psb, psb[0][:, :], wbbj[:, mm_, :], OT[:, 2 + mm_, tsl], [btw, B_OT[2 + mm_]],
                                     last=(mm_ == 7))
                              P.emit(dve, lambda: nc.vector.tensor_tensor(out=gts[1][0][:], in0=psb[0][:, :],
                                                                          in1=gts[1][0][:], op=ALU.mult),
                                     reads=[psb[1], gts[1][1]], writes=[gts[1][1]])
                              P.emit(dve, lambda: nc.vector.tensor_tensor(out=mg[:, j, :], in0=gts[0][0][:],
                                                                          in1=gts[1][0][:], op=ALU.add),
                                     reads=[gts[0][1], gts[1][1]], writes=[bmg])
                          for jo in range(8):
                              ps = next_ps()
                              for c in range(KC):
                                  mm(ps, ps[0][:, :], wo[:, c, jo * 128:(jo + 1) * 128], mg[:, c, :], [B_wo, bmg],
                                     last=(c == KC - 1))
                              P.emit(dve, lambda: nc.vector.tensor_tensor(out=hT[:, jo, tsl], in0=ps[0][:, :],
                                                                          in1=hT[:, jo, tsl], op=ALU.add),
                                     reads=[ps[1], B_hT], writes=[B_hT])
                  P.barrier()
              ck("mixer")

              with ExitStack() as ph:
                  def sbm(shape, dt):
                      return ph.enter_context(nc.sbuf_tensor(uname(), list(shape), dt))

                  hn2 = sbm([128, 16, D], BF16)
                  posmT = sbm([128, 256], F32)
                  gmT = sbm([128, 256], F32)
                  gmHL = sbm([128, 2, 256], BF16)
                  B_hn2, B_posmT, B_gmT, B_gmHL = Buf("hn2"), Buf("posmT"), Buf("gmT"), Buf("gmHL")
                  with ExitStack() as ph1:
                      def sb1(shape, dt):
                          return ph1.enter_context(nc.sbuf_tensor(uname(), list(shape), dt))

                      sq_ring = Ring([(sb1([128, 512], BF16), Buf("sq%d" % i)) for i in range(2)])
                      rs_ring = Ring([(sb1([128, 512], F32), Buf("rs%d" % i)) for i in range(2)])
                      rmsnorm(V_FFN + l * 8, sq_ring, rs_ring)
                      affT = sb1([NE, S], F32)
                      work = sb1([NE, S], F32)
                      t3 = sb1([NE, S], F32)
                      mx8 = sb1([NE, 8], F32)
                      B_aff, B_work, B_t3, B_mx = Buf("aff"), Buf("work"), Buf("t3"), Buf("mx8")
                      ex_ring = Ring([(sb1([NE, 512], F32), Buf("ex%d" % i)) for i in range(2)])
                      for tt in range(4):
                          tsl = slice(tt * 512, (tt + 1) * 512)
                          ps = next_ps()
                          for c in range(KC):
                              mm(ps, ps[0][0:NE, :], wr_sb[:, l, c, :], hnT[:, c, tsl], [B_wr, B_hnT],
                                 last=(c == KC - 1))
                          ex, bex = ex_ring.next()
                          P.emit(act, lambda: nc.scalar.activation(out=ex[:], in_=ps[0][0:NE, :], func=AF.Exp),
                                 reads=[ps[1]], writes=[bex])
                          ps2 = next_ps()
                          mm(ps2, ps2[0][0:NE, :], ones16[:], ex[:], [bex, B_ones], last=True)
                          P.emit(dve, lambda: nc.vector.reciprocal(affT[:, tsl], ps2[0][0:NE, :]), reads=[ps2[1]],
                                 writes=[B_aff])
                          P.emit(dve, lambda: nc.vector.tensor_tensor(out=affT[:, tsl], in0=ex[:], in1=affT[:, tsl],
                                                                      op=ALU.mult), reads=[bex, B_aff], writes=[B_aff])
                      for tc in range(16):
                          for half in range(2):
                              ps = next_ps()
                              for j in range(4):
                                  c = half * 4 + j
                                  mm(ps, ps[0][:, j * 128:(j + 1) * 128], hnT[:, c, tc * 128:(tc + 1) * 128], identB[:],
                                     [B_hnT, B_c2], last=(j == 3), start=True, stop=True)
                              copy_ps(hn2[:, tc, half * 512:(half + 1) * 512], ps[0][:, :], [ps[1]], [B_hn2], act)
                      src = affT
                      bsrc = B_aff
                      for it in range(CAP // 8):
                          P.emit(dve, lambda: nc.vector.max(out=mx8[:], in_=src[:]), reads=[bsrc], writes=[B_mx])
                          P.emit(dve, lambda: nc.vector.match_replace(out=work[:], in_to_replace=mx8[:],
                                                                      in_values=src[:], imm_value=0.0),
                                 reads=[B_mx, bsrc], writes=[B_work])
                          src, bsrc = work, B_work
                      P.emit(dve, lambda: nc.vector.tensor_tensor(out=work[:], in0=affT[:], in1=work[:],
                                                                  op=ALU.subtract),
                             reads=[B_aff, B_work], writes=[B_work])
                      P.emit(dve, lambda: nc.vector.tensor_single_scalar(out=affT[:], in_=work[:], scalar=0.0,
                                                                         op=ALU.is_gt),
                             reads=[B_work], writes=[B_aff])
                      P.emit(dve, lambda: nc.vector.tensor_tensor_scan(out=t3[:], data0=affT[:], data1=affT[:],
                                                                       initial=0.0, op0=ALU.add, op1=ALU.max),
                             reads=[B_aff], writes=[B_t3])
                      P.emit(dve, lambda: nc.vector.tensor_tensor(out=t3[:], in0=t3[:], in1=affT[:], op=ALU.mult),
                             reads=[B_t3, B_aff], writes=[B_t3])
                      for (srcT, bsrcT, dstT, bdstT) in ((t3, B_t3, posmT, B_posmT), (work, B_work, gmT, B_gmT)):
                          ps = next_ps()
                          for tc in range(16):
                              mm(ps, ps[0][:, tc * 16:(tc + 1) * 16], srcT[:, tc * 128:(tc + 1) * 128],
                                 identF[0:NE, 0:NE], [bsrcT, B_c2], last=(tc == 15), start=True, stop=True)
                          copy_ps(dstT[:, :], ps[0][:, 0:256], [ps[1]], [bdstT], act)
                      P.emit(dve, lambda: nc.vector.tensor_copy(out=gmHL[:, 0, :], in_=gmT[:, :]), reads=[B_gmT],
                             writes=[B_gmHL])
                      P.emit(dve, lambda: nc.vector.tensor_tensor(out=gmT[:, :], in0=gmT[:, :], in1=gmHL[:, 0, :],
                                                                  op=ALU.subtract),
                             reads=[B_gmT, B_gmHL], writes=[B_gmT])
                      P.emit(dve, lambda: nc.vector.tensor_copy(out=gmHL[:, 1, :], in_=gmT[:, :]), reads=[B_gmT],
                             writes=[B_gmHL])
                  P.barrier()
                  ck("route")
                  PTg = hnT[:, 0:2, :]
                  Pm = hnT[:, 2:4, :].rearrange("p a (b c) -> p (a b) c", c=256)
                  xeT = hnT[:, 4, :].rearrange("p (a c) -> p a c", c=256)
                  hm = hnT[:, 5, :].rearrange("p (a c) -> p a c", c=256)
                  yeb = hnT[:, 6, :].rearrange("p (k d) -> p k d", k=2)
                  B_PT, B_xe, B_hm, B_ye = Buf("PT"), Buf("xe"), Buf("hm"), Buf("ye")
                  Pbufs = [(Pm, Buf("P0")), (sbm([128, 16, 256], BF16), Buf("P1"))]

                  def build_P(e_):
                      Pm_, B_P_ = Pbufs[e_ % 2]
                      for tc in range(16):
                          P.emit(dve, lambda: nc.vector.tensor_scalar(
                              out=Pm_[:, tc, :], in0=iota1[:, :], scalar1=posmT[:, tc * 16 + e_:tc * 16 + e_ + 1],
                              scalar2=None, op0=ALU.is_equal), reads=[B_posmT, B_c2], writes=[B_P_])

                  we_ring = Ring([(sbm([128, KC, D], BF16), Buf("we%d" % i), P.dsem("we%d" % i)) for i in range(3)])
                  s_ring = Ring([(sbm([128, 256], F32), Buf("s%d" % i)) for i in range(2)])
                  gc_ring = Ring([(sbm([128, 2], F32), Buf("gc%d" % i)) for i in range(2)])
                  for e in range(NE):
                      ws = []
                      for wd_ in (weg_d, weu_d, wed_d):
                          wt, bw, dsw = we_ring.next()
                          P.dma(pool, dsw, [wt[:, 0:4, :], wt[:, 4:8, :]],
                                [wd_[l, e, 0:512, :].rearrange("(k p) c -> p k c", p=128),
                                 wd_[l, e, 512:1024, :].rearrange("(k p) c -> p k c", p=128)], writes=[bw])
                          ws.append((wt, bw))
                      (wg, bwg), (wu, bwu), (wdn, bwd) = ws
                      Pm, B_P = Pbufs[e % 2]
                      if e == 0:
                          build_P(0)
                      for k in range(2):
                          for tt in range(4):
                              ps = next_ps()
                              for j in range(4):
                                  tc = tt * 4 + j
                                  mm(ps, ps[0][:, j * 128:(j + 1) * 128], Pm[:, tc, k * 128:(k + 1) * 128], identB[:],
                                     [B_P, B_c2], last=(j == 3), start=True, stop=True)
                              copy_ps(PTg[:, k, tt * 512:(tt + 1) * 512], ps[0][:, :], [ps[1]], [B_PT])
                      psg = next_ps()
                      for k in range(2):
                          for tc in range(16):
                              mm(psg, psg[0][:, 2 * k:2 * k + 2], Pm[:, tc, k * 128:(k + 1) * 128],
                                 gmHL[:, :, tc * 16 + e], [B_P, B_gmHL], last=(k == 1 and tc == 15), start=(tc == 0), stop=(tc == 15))
                      gc, bgc = gc_ring.next()
                      P.emit(dve, lambda: nc.vector.reduce_sum(
                          out=gc[:, :], in_=psg[0][:, 0:4].rearrange("p (k h) -> p k h", h=2), axis=AX.X),
                          reads=[psg[1]], writes=[bgc])
                      for half in range(4):
                          ps = next_ps()
                          for j in range(2):
                              c = half * 2 + j
                              for tc in range(16):
                                  mm(ps, ps[0][:, j * 256:(j + 1) * 256], hn2[:, tc, c * 128:(c + 1) * 128], Pm[:, tc, :],
                                     [B_hn2, B_P], last=(j == 1 and tc == 15), start=(tc == 0), stop=(tc == 15))
                          copy_ps(xeT[:, half * 2:half * 2 + 2, :], ps[0][:, :].rearrange("p (a c) -> p a c", c=256),
                                  [ps[1]], [B_xe], act)
                      if e + 1 < NE:
                          build_P(e + 1)
                      for f in range(8):
                          ps = next_ps()
                          for c in range(KC):
                              mm(ps, ps[0][:, 0:256], wg[:, c, f * 128:(f + 1) * 128], xeT[:, c, :], [bwg, B_xe],
                                 start=(c == 0), stop=(c == KC - 1))
                          for c in range(KC):
                              mm(ps, ps[0][:, 256:512], wu[:, c, f * 128:(f + 1) * 128], xeT[:, c, :], [bwu, B_xe],
                                 last=(c == KC - 1), start=(c == 0))
                          st, bst = s_ring.next()
                          P.emit(act, lambda: nc.scalar.activation(out=st[:], in_=ps[0][:, 0:256], func=AF.Silu),
                                 reads=[ps[1]], writes=[bst])
                          P.emit(dve, lambda: nc.vector.tensor_tensor(out=hm[:, f, :], in0=ps[0][:, 256:512], in1=st[:],
                                                                      op=ALU.mult),
                                 reads=[ps[1], bst], writes=[B_hm])
                      for k in range(2):
                          for dh in range(2):
                              ps = next_ps()
                              for f in range(8):
                                  mm(ps, ps[0][:, :], hm[:, f, k * 128:(k + 1) * 128], wdn[:, f, dh * 512:(dh + 1) * 512],
                                     [bwd, B_hm], last=(f == 7))
                              P.emit(act, lambda: nc.scalar.activation(out=yeb[:, k, dh * 512:(dh + 1) * 512],
                                                                       in_=ps[0][:, :], func=AF.Copy,
                                                                       scale=gc[:, k:k + 1]),
                                     reads=[ps[1], bgc], writes=[B_ye])
                      for jo in range(8):
                          for tt in range(4):
                              tsl = slice(tt * 512, (tt + 1) * 512)
                              ps = next_ps()
                              for k in range(2):
                                  mm(ps, ps[0][:, :], yeb[:, k, jo * 128:(jo + 1) * 128], PTg[:, k, tsl], [B_ye, B_PT],
                                     last=(k == 1))
                              P.emit(dve, lambda: nc.vector.tensor_tensor(out=hT[:, jo, tsl], in0=ps[0][:, :],
                                                                          in1=hT[:, jo, tsl], op=ALU.add),
                                     reads=[ps[1], B_hT], writes=[B_hT])
                  P.barrier()

          with ExitStack() as ph:
              def sbf(shape, dt):
                  return ph.enter_context(nc.sbuf_tensor(uname(), list(shape), dt))

              sq_ring = Ring([(sbf([128, 512], BF16), Buf("sq%d" % i)) for i in range(2)])
              rs_ring = Ring([(sbf([128, 512], F32), Buf("rs%d" % i)) for i in range(2)])
              stage_ring = Ring([(sbf([128, 512], F32), Buf("stg%d" % i), P.dsem("out%d" % i)) for i in range(3)])
              rmsnorm(V_FIN, sq_ring, rs_ring, final_seq=s, stage_ring=stage_ring)
              P.barrier()

    except _Stop:
        P.barrier()
        for c in range(KC):
            P.dma(sp, ds_out, outT[0, c * 128:(c + 1) * 128, :], hT[:, c, :], reads=[B_hT])

    for d in P.dsems:
        if d.val and sp.waited.get(d.key, 0) < d.val:
            sp.h.wait_ge(d.sem, d.val)
    nc._keep_es = es if False else None; globals().setdefault("_KEEP", []).append(es)
    return nc


def _tables():
    kk = np.arange(128)[:, None]
    qq = np.arange(128)[None, :]
    tabs = np.zeros((128, 2, 3, 128), np.float32)
    for ti in range(3):
        rel = np.abs(kk + (ti - 1) * 128 - qq).astype(np.float32)
        tabs[:, 0, ti, :] = np.where(rel <= 128, rel, 1e6)
        tabs[:, 1, ti, :] = np.where(rel <= 64, rel, 1e6)
    cst2 = np.zeros((128, 384), np.float32)
    cst2[:, 0:128] = np.eye(128, dtype=np.float32)
    cst2[:, 128:384] = np.arange(1, 257, dtype=np.float32)[None, :]
    return tabs.reshape(128, -1), cst2


def prep_weights(norm_mix, w_in, w_branch_a, w_branch_b, b_gate, sink_logit, w_out, norm_ffn, w_router,
                 w_expert_gate, w_expert_up, w_expert_down, norm_final, L=DEPTH):
    f = np.float32
    w_in = np.asarray(w_in, f)
    wa = np.empty((L, 6, D, 384), f)
    for jp in range(2):
        for g in range(3):
            c0 = g * 256 + jp * 128
            u = jp * 3 + g
            wa[:, u, :, 0:128] = w_in[:L, :, c0:c0 + 128]
            wa[:, u, :, 128:256] = w_in[:L, :, 768 + c0:768 + c0 + 128]
            wa[:, u, :, 256:384] = w_in[:L, :, 1536 + c0:1536 + c0 + 128]
    q0, k0, v0, g0 = 2304, 3328, 3584, 3840
    wqw = np.ascontiguousarray(w_in[:L, :, q0:q0 + 1024].reshape(L, D, 8, 128).transpose(0, 2, 1, 3))
    wkvw = np.empty((L, 4, D, 192), f)
    for g in range(4):
        wkvw[:, g, :, 0:64] = w_in[:L, :, k0 + g * 64:k0 + (g + 1) * 64]
        wkvw[:, g, :, 64:128] = w_in[:L, :, k0 + g * 64:k0 + (g + 1) * 64]
        wkvw[:, g, :, 128:192] = w_in[:L, :, v0 + g * 64:v0 + (g + 1) * 64]
    wtail = np.empty((L, 8, D, 256), f)
    for j in range(8):
        wtail[:, j, :, 0:128] = w_in[:L, :, g0 + j * 128:g0 + (j + 1) * 128]
        wtail[:, j, :, 128:256] = w_in[:L, :, g0 + 1024 + j * 128:g0 + 1024 + (j + 1) * 128]
    wba = np.ascontiguousarray(np.asarray(w_branch_a, f)[:L].reshape(L, 256, 8, 128).transpose(0, 2, 1, 3))
    wbb = np.ascontiguousarray(np.asarray(w_branch_b, f)[:L].reshape(L, D, 8, 128).transpose(0, 2, 1, 3))

    def pm(v):
        v = np.asarray(v, f)
        return v.reshape(v.shape[:-1] + (8, 128))

    NV = L * 40 + 8
    vecs = np.zeros((128, NV), f)
    vecs[:, 0:L * 8] = pm(norm_mix)[:L].transpose(2, 0, 1).reshape(128, L * 8)
    vecs[:, L * 8:L * 16] = pm(norm_ffn)[:L].transpose(2, 0, 1).reshape(128, L * 8)
    bg = np.asarray(b_gate, f)[:L].reshape(L, 2, 8, 128)
    vecs[:, L * 16:L * 32] = bg.transpose(3, 0, 1, 2).reshape(128, L * 16)
    vecs[:, L * 32:L * 32 + 8] = pm(norm_final).transpose(1, 0)
    sk = np.asarray(sink_logit, f)[:L].reshape(L, 8, 2)
    sp = np.repeat(sk.transpose(2, 0, 1)[:, None], 64, axis=1).reshape(128, L * 8)
    vecs[:, L * 32 + 8:] = sp
    tabs, cst2 = _tables()
    return {
        "wa": wa, "wqw": wqw, "wkvw": wkvw, "wtail": wtail, "wba": wba, "wbb": wbb,
        "wout": np.ascontiguousarray(np.asarray(w_out, f)[:L]),
        "wr": np.ascontiguousarray(np.asarray(w_router, f)[:L]),
        "weg": np.ascontiguousarray(np.asarray(w_expert_gate, f)[:L]),
        "weu": np.ascontiguousarray(np.asarray(w_expert_up, f)[:L]),
        "wed": np.ascontiguousarray(np.asarray(w_expert_down, f)[:L]),
        "vecs": vecs, "tabs": tabs, "cst2": cst2,
    }


def kernel(x, norm_mix, w_in, w_branch_a, w_branch_b, b_gate, sink_logit, w_out, norm_ffn, w_router,
           w_expert_gate, w_expert_up, w_expert_down, norm_final):
    x = np.asarray(x, np.float32)
    wts = prep_weights(norm_mix, w_in, w_branch_a, w_branch_b, b_gate, sink_logit, w_out, norm_ffn, w_router,
                       w_expert_gate, w_expert_up, w_expert_down, norm_final)
    nc = build_program(DEPTH, NSEQ)
    in_maps = []
    for c in range(NCORES):
        m = dict(wts)
        m["xT"] = np.ascontiguousarray(x[c * NSEQ:(c + 1) * NSEQ].transpose(0, 2, 1))
        in_maps.append(m)
    res = run_bass_kernel_spmd(nc, in_maps, core_ids=list(range(NCORES)))
    out = np.empty((NCORES * NSEQ, S, D), np.float32)
    for c in range(NCORES):
        out[c * NSEQ:(c + 1) * NSEQ] = res.results[c]["outT"].transpose(0, 2, 1)
    return out
```

```python
import numpy as np
from contextlib import ExitStack
import concourse.bass as bass
import concourse.mybir as mybir
from concourse.bass_utils import run_bass_kernel_spmd

F32 = mybir.dt.float32
BF16 = mybir.dt.bfloat16
AF = mybir.ActivationFunctionType
ALU = mybir.AluOpType
AX = mybir.AxisListType

D = 1024
S = 2048
DEPTH = 4
NCORES = 8
NSEQ = 4
KC = 8
NE = 16
CAP = 256
EPS = 1e-6
DIL_R = (1, 4, 16)


def _slopes(n):
    return [float(2.0 ** (-8.0 * i / n)) for i in range(1, n + 1)]


SL_DIL = _slopes(12)
SL_WIN = _slopes(16)


class Ev:
    __slots__ = ("sem", "val", "key")

    def __init__(self, sem, val, key):
        self.sem, self.val, self.key = sem, val, key


class Buf:
    __slots__ = ("name", "w", "r", "excl")

    def __init__(self, name, excl=False):
        self.name = name
        self.w = None
        self.r = {}
        self.excl = excl


class Eng:
    def __init__(self, name, h, sem, is_pe=False):
        self.name, self.h, self.sem, self.is_pe = name, h, sem, is_pe
        self.cnt = 0
        self.waited = {}


class DSem:
    def __init__(self, sem, key):
        self.sem, self.key, self.val = sem, key, 0


class Prog:
    def __init__(self, nc, es):
        self.nc = nc
        self.es = es
        mk = lambda n: es.enter_context(nc.semaphore(n))
        self.pe = Eng("pe", nc.tensor, mk("s_pe"), True)
        self.act = Eng("act", nc.scalar, mk("s_act"))
        self.dve = Eng("dve", nc.vector, mk("s_dve"))
        self.pool = Eng("pool", nc.gpsimd, mk("s_pool"))
        self.sp = Eng("sp", nc.sync, mk("s_sp"))
        self.engs = [self.pe, self.act, self.dve, self.pool, self.sp]
        self.dsems = []
        self.dtags = {}
        self.nds = 0

    def dsem(self, tag=None):
        if tag is not None and tag in self.dtags:
            return self.dtags[tag]
        self.nds += 1
        d = DSem(self.es.enter_context(self.nc.semaphore("s_dma%d" % self.nds)), "dma%d" % self.nds)
        self.dsems.append(d)
        if tag is not None:
            self.dtags[tag] = d
        return d

    def need(self, eng, ev, raw):
        if ev is None:
            return
        if ev.key == eng.name and eng.is_pe:
            return
        if eng.waited.get(ev.key, 0) >= ev.val:
            return
        eng.h.wait_ge(ev.sem, ev.val)
        eng.waited[ev.key] = ev.val

    def deps(self, eng, reads, writes):
        for b in reads:
            self.need(eng, b.w, True)
            if b.excl:
                for ev in b.r.values():
                    if ev.key != eng.name:
                        self.need(eng, ev, False)
        for b in writes:
            self.need(eng, b.w, False)
            for ev in b.r.values():
                self.need(eng, ev, False)

    def mark(self, ev, reads, writes):
        for b in writes:
            b.w = ev
            b.r = {}
        for b in reads:
            o = b.r.get(ev.key)
            if o is None or o.val < ev.val:
                b.r[ev.key] = ev

    def emit(self, eng, fn, reads=(), writes=(), signal=True):
        self.deps(eng, reads, writes)
        ins = fn()
        if signal:
            eng.cnt += 1
            ins.then_inc(eng.sem, 1)
            ev = Ev(eng.sem, eng.cnt, eng.name)
        else:
            ev = Ev(eng.sem, eng.cnt + 1, eng.name)
        self.mark(ev, reads, writes)
        return ins

    def dma(self, q, ds, out_ap, in_ap, reads=(), writes=()):
        self.deps(q, reads, writes)
        if not isinstance(out_ap, list):
            out_ap, in_ap = [out_ap], [in_ap]
        for o, i in zip(out_ap, in_ap):
            ds.val += 16
            q.h.dma_start(out=o, in_=i).then_inc(ds.sem, 16)
        ev = Ev(ds.sem, ds.val, ds.key)
        self.mark(ev, reads, writes)

    def barrier(self):
        for e in self.engs:
            for f in self.engs:
                if f is e or f.cnt == 0:
                    continue
                if e.waited.get(f.name, 0) < f.cnt:
                    e.h.wait_ge(f.sem, f.cnt)
                    e.waited[f.name] = f.cnt
            for d in self.dsems:
                if d.val and e.waited.get(d.key, 0) < d.val:
                    e.h.wait_ge(d.sem, d.val)
                    e.waited[d.key] = d.val


class _Stop(Exception):
    pass


class Ring:
    def __init__(self, items):
        self.items = items
        self.i = -1

    def next(self):
        self.i = (self.i + 1) % len(self.items)
        return self.items[self.i]


def build_program(L=DEPTH, NS=NSEQ, stop_after=None):
    nc = bass.Bass("TRN2", target_bir_lowering=False)
    es = ExitStack()
    P = Prog(nc, es)
    pe, act, dve, pool, sp = P.pe, P.act, P.dve, P.pool, P.sp

    def din(name, shape):
        return nc.dram_tensor(name, list(shape), F32, kind="ExternalInput").ap()

    NV = L * 8 + L * 8 + L * 16 + 8 + L * 8
    V_MIX, V_FFN, V_BG, V_FIN, V_SINK = 0, L * 8, L * 16, L * 32, L * 32 + 8
    xT = din("xT", [NS, D, S])
    wa_d = din("wa", [L, 6, D, 384])
    wqw_d = din("wqw", [L, 8, D, 128])
    wkvw_d = din("wkvw", [L, 4, D, 192])
    wtail_d = din("wtail", [L, 8, D, 256])
    wba_d = din("wba", [L, 8, 256, 128])
    wbb_d = din("wbb", [L, 8, D, 128])
    wout_d = din("wout", [L, D, D])
    wr_d = din("wr", [L, D, NE])
    weg_d = din("weg", [L, NE, D, D])
    weu_d = din("weu", [L, NE, D, D])
    wed_d = din("wed", [L, NE, D, D])
    vecs_d = din("vecs", [128, NV])
    tabs_d = din("tabs", [128, 2 * 3 * 128])
    cst2_d = din("cst2", [128, 384])
    outT = nc.dram_tensor("outT", [NS, D, S], F32, kind="ExternalOutput").ap()

    def sb(name, shape, dt):
        return es.enter_context(nc.sbuf_tensor(name, list(shape), dt))

    uctr = [0]

    def uname():
        uctr[0] += 1
        return "t%d" % uctr[0]

    hT = sb("hT", [128, KC, S], F32)
    hnT = sb("hnT", [128, KC, S], BF16)
    vec = sb("vec", [128, NV], F32)
    esink = sb("esink", [128, L * 8], F32)
    tab = sb("tab", [128, 2, 3, 128], F32)
    ones16 = sb("ones16", [NE, NE], F32)
    onesF = sb("onesF", [128, 128], BF16)
    onesA0 = sb("onesA0", [128, 128], BF16)
    ones0B = sb("ones0B", [128, 128], BF16)
    wr_sb = sb("wr_sb", [128, L, KC, NE], BF16)
    epsb = sb("epsb", [128, 1], F32)
    B_hT, B_hnT, B_const = Buf("hT"), Buf("hnT"), Buf("const")
    B_wr = Buf("wr")
    identF = sb("identF", [128, 128], F32)
    iota1 = sb("iota1", [128, 256], F32)
    identB = sb("identB", [128, 128], BF16)
    B_c2 = Buf("c2")

    PSB = []
    for i in range(8):
        t = es.enter_context(nc.psum_tensor("ps%d" % i, [128, 512], F32))
        PSB.append((t, Buf("ps%d" % i, excl=True)))
    ps_ring = Ring(PSB)
    fresh = {}

    def next_ps():
        t, b = ps_ring.next()
        fresh[b.name] = True
        return t, b

    def mm(psb, out_ap, lhsT, rhs, reads, last=False, sig=False, start=None, stop=None):
        b = psb[1]
        st = fresh[b.name] if start is None else start
        fresh[b.name] = False
        sp_ = last if stop is None else (stop or last)
        P.emit(pe, lambda: nc.tensor.matmul(out_ap, lhsT, rhs, start=st, stop=sp_),
               reads=reads, writes=[b], signal=(last or sig))

    ds_c = P.dsem()
    P.dma(sp, ds_c, [vec[:, :], tab[:].rearrange("p a b c -> p (a b c)")],
          [vecs_d[:, :], tabs_d[:, :]], writes=[B_const])
    ds_c2 = P.dsem()
    P.dma(sp, ds_c2, [identF[:, :], iota1[:, :]], [cst2_d[:, 0:128], cst2_d[:, 128:384]], writes=[B_c2])
    ds_c3 = P.dsem()
    P.dma(pool, ds_c3, identB[:, :], cst2_d[:, 0:128], writes=[B_c2])
    ds_wr = P.dsem()
    P.dma(pool, ds_wr, [wr_sb[:, l, :, :] for l in range(L)],
          [wr_d[l].rearrange("(k p) e -> p k e", p=128) for l in range(L)], writes=[B_wr])
    B_ones = Buf("ones")
    P.emit(dve, lambda: nc.vector.memset(onesF[:], 1.0), writes=[B_ones])
    P.emit(dve, lambda: nc.vector.memset(onesA0[:], 0.0), writes=[B_ones])
    P.emit(dve, lambda: nc.vector.memset(ones0B[:], 0.0), writes=[B_ones])
    P.emit(dve, lambda: nc.vector.memset(onesA0[:, 0:64], 1.0), writes=[B_ones])
    P.emit(dve, lambda: nc.vector.memset(ones0B[:, 64:128], 1.0), writes=[B_ones])
    P.emit(dve, lambda: nc.vector.memset(ones16[:], 1.0), writes=[B_ones])
    P.emit(dve, lambda: nc.vector.memset(epsb[:], EPS), writes=[B_ones])
    B_esink = Buf("esink")
    P.emit(act, lambda: nc.scalar.activation(out=esink[:], in_=vec[:, V_SINK:V_SINK + L * 8], func=AF.Exp),
           reads=[B_const], writes=[B_esink])

    ds_x = P.dsem("x")
    ds_out = P.dsem("dbg")

    def rmsnorm(gcol, sq_ring, rs_ring, final_seq=None, stage_ring=None):
        for tt in range(4):
            tsl = slice(tt * 512, (tt + 1) * 512)
            ps = next_ps()
            for c in range(KC):
                sq, bsq = sq_ring.next()
                P.emit(act, lambda: nc.scalar.activation(out=sq[:], in_=hT[:, c, tsl], func=AF.Square),
                       reads=[B_hT], writes=[bsq])
                mm(ps, ps[0][:, :], onesF[:], sq[:], [bsq, B_ones], last=(c == KC - 1), sig=True)
            rs, brs = rs_ring.next()
            P.emit(act, lambda: nc.scalar.activation(out=rs[:], in_=ps[0][:, :], func=AF.Sqrt,
                                                     scale=1.0 / D, bias=epsb[:, 0:1]),
                   reads=[ps[1], B_ones], writes=[brs])
            P.emit(dve, lambda: nc.vector.reciprocal(rs[:], rs[:]), reads=[brs], writes=[brs])
            for c in range(KC):
                if final_seq is None:
                    P.emit(dve, lambda: nc.vector.scalar_tensor_tensor(
                        out=hnT[:, c, tsl], in0=hT[:, c, tsl], scalar=vec[:, gcol + c:gcol + c + 1],
                        in1=rs[:], op0=ALU.mult, op1=ALU.mult),
                        reads=[B_hT, brs, B_const], writes=[B_hnT])
                else:
                    st, bst, dso = stage_ring.next()
                    P.emit(dve, lambda: nc.vector.scalar_tensor_tensor(
                        out=st[:], in0=hT[:, c, tsl], scalar=vec[:, gcol + c:gcol + c + 1],
                        in1=rs[:], op0=ALU.mult, op1=ALU.mult),
                        reads=[B_hT, brs, B_const], writes=[bst])
                    P.dma(sp, dso, outT[final_seq, c * 128:(c + 1) * 128, tsl], st[:], reads=[bst])

    def perm512(c, ut, r):
        if r == 1:
            return hnT[:, c, ut * 512:(ut + 1) * 512]
        if r == 4:
            return hnT[:, c, ut:S:4]
        return hnT[:, c, :].rearrange("p (a q) -> p q a", q=16)[:, 4 * ut:4 * ut + 4, :]

    def perm128(c, uc, r):
        if r == 1:
            return hnT[:, c, uc * 128:(uc + 1) * 128]
        if r == 4:
            sub, a0 = uc // 4, (uc % 4) * 128
            return hnT[:, c, sub + 4 * a0:sub + 4 * (a0 + 127) + 1:4]
        return hnT[:, c, uc:S:16]

    def nat128(qt, r):
        if r == 1:
            return slice(qt * 128, (qt + 1) * 128)
        if r == 4:
            sub, a0 = qt // 4, (qt % 4) * 128
            return slice(sub + 4 * a0, sub + 4 * (a0 + 127) + 1, 4)
        return slice(qt, S, 16)

    def ps_view(ps, r):
        if r == 16:
            return ps[0][:, :].rearrange("p (q a) -> p q a", q=4)
        return ps[0][:, :]

    cp_flip = [0]

    def copy_ps(out_ap, in_ap, reads, writes, eng=None):
        if eng is None:
            cp_flip[0] ^= 1
            eng = act if cp_flip[0] else dve
        if eng is act:
            P.emit(act, lambda: nc.scalar.copy(out=out_ap, in_=in_ap), reads=reads, writes=writes)
        else:
            P.emit(dve, lambda: nc.vector.tensor_copy(out=out_ap, in_=in_ap), reads=reads, writes=writes)

    def ck(name):
        if stop_after == name:
            raise _Stop()

    try:
      for s in range(NS):
          P.dma(sp, ds_x, [hT[:, c, :] for c in range(KC)], [xT[s, c * 128:(c + 1) * 128, :] for c in range(KC)],
                writes=[B_hT])
          for l in range(L):
              with ExitStack() as ph:
                  def sbp(name, shape, dt):
                      return ph.enter_context(nc.sbuf_tensor(uname(), list(shape), dt))

                  sq_ring = Ring([(sbp("sq", [128, 512], BF16), Buf("sq%d" % i)) for i in range(2)])
                  rs_ring = Ring([(sbp("rs", [128, 512], F32), Buf("rs%d" % i)) for i in range(2)])
                  OT = sbp("OT", [128, 10, S], BF16)
                  B_OT = [Buf("OT%d" % i) for i in range(10)]
                  ck("load")
                  rmsnorm(V_MIX + l * 8, sq_ring, rs_ring)
                  ck("norm")

                  with ExitStack() as ph2:
                      def sb2(shape, dt):
                          return ph2.enter_context(nc.sbuf_tensor(uname(), list(shape), dt))

                      QA0, Q0B, K2 = sb2([128, S], BF16), sb2([128, S], BF16), sb2([128, S], BF16)
                      VA0, V0B = sb2([128, 16, 128], BF16), sb2([128, 16, 128], BF16)
                      acc = sb2([128, 2, S], F32)
                      B_QA, B_QB, B_K, B_VA, B_VB, B_acc = (Buf(n) for n in ("QA", "QB", "K", "VA", "VB", "acc"))
                      wa_ring = Ring([(sb2([128, KC, 384], BF16), Buf("wa%d" % i), P.dsem("wa%d" % i)) for i in range(1)])
                      wq_ring = Ring([(sb2([128, KC, 128], BF16), Buf("wq%d" % i), P.dsem("wq%d" % i)) for i in range(1)])
                      wkv_ring = Ring([(sb2([128, KC, 192], BF16), Buf("wkv%d" % i), P.dsem("wkv%d" % i)) for i in range(1)])
                      tS_ring = Ring([(sb2([128, 384], F32), Buf("tS%d" % i)) for i in range(2)])
                      PT_ring = Ring([(sb2([128, 384], BF16), Buf("PT%d" % i)) for i in range(4)])
                      dn_ring = Ring([(sb2([128, 128], F32), Buf("dn%d" % i)) for i in range(2)])
                      P.emit(dve, lambda: nc.vector.memset(QA0[64:128, :], 0.0), writes=[B_QA])
                      P.emit(dve, lambda: nc.vector.memset(Q0B[0:64, :], 0.0), writes=[B_QB])
                      P.emit(dve, lambda: nc.vector.memset(VA0[:, :, 64:128], 0.0), writes=[B_VA])
                      P.emit(dve, lambda: nc.vector.memset(V0B[:, :, 0:64], 0.0), writes=[B_VB])
                      ck("ph2alloc")

                      def perm_views(dst, src_ps, ut, r):
                          if r == 1:
                              return dst[:, ut * 512:(ut + 1) * 512], src_ps
                          n = 512 // r
                          o = dst.rearrange("p (c a) -> p c a", c=r)[:, :, ut * n:(ut + 1) * n]
                          i = src_ps.rearrange("p (a c) -> p c a", c=r)
                          return o, i

                      def proj_q(wt, bw, col0, r):
                          for ut in range(4):
                              ps = next_ps()
                              for c in range(KC):
                                  mm(ps, ps[0][:, :], wt[:, c, col0:col0 + 128], hnT[:, c, ut * 512:(ut + 1) * 512],
                                     [bw, B_hnT], last=(c == KC - 1))
                              o, i = perm_views(QA0[0:64, :], ps[0][0:64, :], ut, r)
                              copy_ps(o, i, [ps[1]], [B_QA], act)
                              o, i = perm_views(Q0B[64:128, :], ps[0][64:128, :], ut, r)
                              copy_ps(o, i, [ps[1]], [B_QB], dve)

                      def proj_k(wt, bw, col0, r):
                          for ut in range(4):
                              ps = next_ps()
                              for c in range(KC):
                                  mm(ps, ps[0][:, :], wt[:, c, col0:col0 + 128], hnT[:, c, ut * 512:(ut + 1) * 512],
                                     [bw, B_hnT], last=(c == KC - 1))
                              o, i = perm_views(K2[:, :], ps[0][:, :], ut, r)
                              copy_ps(o, i, [ps[1]], [B_K])

                      def attention(r, tabi, scA, scB, epilogue):
                          tps = (S // r) // 128

                          def stage1(qt):
                              jj = qt % tps
                              chunks = []
                              if jj > 0:
                                  chunks.append((qt - 1, 0))
                              chunks.append((qt, 1))
                              if jj < tps - 1:
                                  chunks.append((qt + 1, 2))
                              lo, hi = chunks[0][1] * 128, (chunks[-1][1] + 1) * 128
                              qsl = slice(qt * 128, (qt + 1) * 128)
                              pts = []
                              for (QX, BQ, sc) in ((QA0, B_QA, scA), (Q0B, B_QB, scB)):
                                  ps = next_ps()
                                  for i, (kc, ti) in enumerate(chunks):
                                      mm(ps, ps[0][:, ti * 128:(ti + 1) * 128], K2[:, kc * 128:(kc + 1) * 128],
                                         QX[:, qsl], [B_K, BQ], last=(i == len(chunks) - 1))
                                  tS, btS = tS_ring.next()
                                  P.emit(dve, lambda: nc.vector.scalar_tensor_tensor(
                                      out=tS[:, lo:hi], in0=tab[:, tabi].rearrange("p a b -> p (a b)")[:, lo:hi],
                                      scalar=sc, in1=ps[0][:, lo:hi], op0=ALU.mult, op1=ALU.add),
                                      reads=[ps[1], B_const], writes=[btS])
                                  PT, bPT = PT_ring.next()
                                  P.emit(act, lambda: nc.scalar.activation(out=PT[:, lo:hi], in_=tS[:, lo:hi],
                                                                           func=AF.Exp, scale=0.125),
                                         reads=[btS], writes=[bPT])
                                  pts.append((PT, bPT))
                              return chunks, pts

                          def stage2(qt, chunks, pts):
                              pso = next_ps()
                              n = 0
                              tot = 4 * len(chunks)
                              for (PT, bPT), VX, BV, oX in ((pts[0], VA0, B_VA, onesA0), (pts[1], V0B, B_VB, ones0B)):
                                  for (kc, ti) in chunks:
                                      n += 1
                                      mm(pso, pso[0][:, 0:128], VX[:, kc, :], PT[:, ti * 128:(ti + 1) * 128],
                                         [BV, bPT], last=False)
                                      n += 1
                                      mm(pso, pso[0][:, 128:256], oX[:], PT[:, ti * 128:(ti + 1) * 128],
                                         [B_ones, bPT], last=(n == tot))
                              epilogue(pso, qt)

                          cur = stage1(0)
                          for qt in range(16):
                              nxt = stage1(qt + 1) if qt + 1 < 16 else None
                              stage2(qt, *cur)
                              cur = nxt

                      for jp in range(2):
                          for g in range(3):
                              r = DIL_R[g]
                              wt, bw, dsw = wa_ring.next()
                              P.dma(pool, dsw, wt[:], wa_d[l, jp * 3 + g].rearrange("(k p) c -> p k c", p=128),
                                    writes=[bw])
                              ck("wadma")
                              proj_q(wt, bw, 0, r)
                              ck("projq")
                              proj_k(wt, bw, 128, r)
                              ck("proj%d" % g)
                              for u0 in range(0, 16, 4):
                                  ps = next_ps()
                                  for uu in range(4):
                                      for c in range(KC):
                                          mm(ps, ps[0][:, uu * 128:(uu + 1) * 128], perm128(c, u0 + uu, r),
                                             wt[:, c, 256:384], [bw, B_hnT], last=(uu == 3 and c == KC - 1),
                                             start=(c == 0), stop=(c == KC - 1))
                                  ck("vmm")
                                  pv = ps[0][:, :].rearrange("p (n c) -> p n c", c=128)
                                  copy_ps(VA0[:, u0:u0 + 4, 0:64], pv[:, :, 0:64], [ps[1]], [B_VA], act)
                                  ck("vcpa")
                                  copy_ps(V0B[:, u0:u0 + 4, 64:128], pv[:, :, 64:128], [ps[1]], [B_VB], dve)
                              sA = -8.0 * SL_DIL[g * 4 + 2 * jp] * r
                              sB = -8.0 * SL_DIL[g * 4 + 2 * jp + 1] * r

                              def epi_dil(pso, qt, g=g, r=r):
                                  nat = nat128(qt, r)
                                  pv = pso[0][:, 0:256].rearrange("p (t q) -> p t q", t=2)
                                  if g == 0:
                                      copy_ps(acc[:, :, nat], pv, [pso[1]], [B_acc], act)
                                  else:
                                      P.emit(dve, lambda: nc.vector.tensor_tensor(out=acc[:, :, nat], in0=pv,
                                                                                  in1=acc[:, :, nat], op=ALU.add),
                                             reads=[pso[1], B_acc], writes=[B_acc])

                              ck("vproj%d" % g)
                              attention(r, 1, sA, sB, epi_dil)
                              ck("att%d" % g)
                          P.emit(dve, lambda: nc.vector.reciprocal(acc[:, 1, :], acc[:, 1, :]), reads=[B_acc],
                                 writes=[B_acc])
                          P.emit(dve, lambda: nc.vector.tensor_tensor(out=OT[:, jp, :], in0=acc[:, 0, :],
                                                                      in1=acc[:, 1, :], op=ALU.mult),
                                 reads=[B_acc], writes=[B_OT[jp]])

                      for m in range(8):
                          g = m // 2
                          if m % 2 == 0:
                              wkv, bkv, dskv = wkv_ring.next()
                              P.dma(pool, dskv, wkv[:], wkvw_d[l, g].rearrange("(k p) c -> p k c", p=128),
                                    writes=[bkv])
                              proj_k(wkv, bkv, 0, 1)
                              for u0 in range(0, 16, 8):
                                  ps = next_ps()
                                  for uu in range(8):
                                      for c in range(KC):
                                          mm(ps, ps[0][:, uu * 64:(uu + 1) * 64], perm128(c, u0 + uu, 1),
                                             wkv[:, c, 128:192], [bkv, B_hnT], last=(uu == 7 and c == KC - 1),
                                             start=(c == 0), stop=(c == KC - 1))
                                  pv = ps[0][:, :].rearrange("p (n c) -> p n c", c=64)
                                  copy_ps(VA0[:, u0:u0 + 8, 0:64], pv, [ps[1]], [B_VA], act)
                                  copy_ps(V0B[:, u0:u0 + 8, 64:128], pv, [ps[1]], [B_VB], dve)
                          wq, bq, dsq = wq_ring.next()
                          P.dma(pool, dsq, wq[:], wqw_d[l, m].rearrange("(k p) c -> p k c", p=128), writes=[bq])
                          proj_q(wq, bq, 0, 1)

                          def epi_win(pso, qt, m=m):
                              dn, bdn = dn_ring.next()
                              P.emit(dve, lambda: nc.vector.tensor_scalar(
                                  out=dn[:], in0=pso[0][:, 128:256], scalar1=esink[:, l * 8 + m:l * 8 + m + 1],
                                  scalar2=None, op0=ALU.add), reads=[pso[1], B_esink], writes=[bdn])
                              P.emit(dve, lambda: nc.vector.reciprocal(dn[:], dn[:]), reads=[bdn], writes=[bdn])
                              P.emit(dve, lambda: nc.vector.tensor_tensor(
                                  out=OT[:, 2 + m, qt * 128:(qt + 1) * 128], in0=pso[0][:, 0:128], in1=dn[:],
                                  op=ALU.mult), reads=[pso[1], bdn], writes=[B_OT[2 + m]])

                          attention(1, 0, -8.0 * SL_WIN[2 * m], -8.0 * SL_WIN[2 * m + 1], epi_win)
                          ck("win%d" % m)
                  P.barrier()

                  with ExitStack() as ph3:
                      def sb3(shape, dt):
                          return ph3.enter_context(nc.sbuf_tensor(uname(), list(shape), dt))

                      wo = sb3([128, KC, D], BF16)
                      B_wo, ds_wo = Buf("wo"), P.dsem("wo")
                      P.dma(pool, ds_wo, wo[:], wout_d[l].rearrange("(k p) c -> p k c", p=128), writes=[B_wo])
                      tw_ring = Ring([(sb3([128, KC, 256], BF16), sb3([128, KC, 128], BF16), sb3([128, 2, 128], BF16),
                                       Buf("tw%d" % i), P.dsem("tw%d" % i)) for i in range(2)])
                      mg_ring = Ring([(sb3([128, KC, 512], BF16), Buf("mg%d" % i)) for i in range(1)])
                      g_ring = Ring([(sb3([128, 512], F32), Buf("g%d" % i)) for i in range(4)])
                      for tt in range(4):
                          tsl = slice(tt * 512, (tt + 1) * 512)
                          mg, bmg = mg_ring.next()
                          for j in range(8):
                              wtj, wbbj, wbaj, btw, dstw = tw_ring.next()
                              P.dma(pool, dstw, [wtj[:], wbbj[:], wbaj[:]],
                                    [wtail_d[l, j].rearrange("(k p) c -> p k c", p=128),
                                     wbb_d[l, j].rearrange("(k p) c -> p k c", p=128),
                                     wba_d[l, j].rearrange("(k p) c -> p k c", p=128)], writes=[btw])
                              gts = []
                              for b in range(2):
                                  ps = next_ps()
                                  for c in range(KC):
                                      mm(ps, ps[0][:, :], wtj[:, c, b * 128:(b + 1) * 128], hnT[:, c, tsl],
                                         [btw, B_hnT], last=(c == KC - 1))
                                  gt, bgt = g_ring.next()
                                  col = V_BG + l * 16 + b * 8 + j
                                  P.emit(act, lambda: nc.scalar.activation(out=gt[:], in_=ps[0][:, :], func=AF.Sigmoid,
                                                                           bias=vec[:, col:col + 1], scale=1.0),
                                         reads=[ps[1], B_const], writes=[bgt])
                                  gts.append((gt, bgt))
                              psa = next_ps()
                              for p_ in range(2):
                                  mm(psa, psa[0][:, :], wbaj[:, p_, :], OT[:, p_, tsl], [btw, B_OT[p_]], last=(p_ == 1))
                              P.emit(dve, lambda: nc.vector.tensor_tensor(out=gts[0][0][:], in0=psa[0][:, :],
                                                                          in1=gts[0][0][:], op=ALU.mult),
                                     reads=[psa[1], gts[0][1]], writes=[gts[0][1]])
                              psb = next_ps()
                              for mm_ in range(8):
                                  mm(psb, psb[0][:, :], wbbj[:, mm_, :], OT[:, 2 + mm_, tsl], [btw, B_OT[2 + mm_]],
                                     last=(mm_ == 7))
                              P.emit(dve, lambda: nc.vector.tensor_tensor(out=gts[1][0][:], in0=psb[0][:, :],
                                                                          in1=gts[1][0][:], op=ALU.mult),
                                     reads=[psb[1], gts[1][1]], writes=[gts[1][1]])
                              P.emit(dve, lambda: nc.vector.tensor_tensor(out=mg[:, j, :], in0=gts[0][0][:],
                                                                          in1=gts[1][0][:], op=ALU.add),
                                     reads=[gts[0][1], gts[1][1]], writes=[bmg])
                          for jo in range(8):
                              ps = next_ps()
                              for c in range(KC):
                                  mm(ps, ps[0][:, :], wo[:, c, jo * 128:(jo + 1) * 128], mg[:, c, :], [B_wo, bmg],
                                     last=(c == KC - 1))
                              P.emit(dve, lambda: nc.vector.tensor_tensor(out=hT[:, jo, tsl], in0=ps[0][:, :],
                                                                          in1=hT[:, jo, tsl], op=ALU.add),
                                     reads=[ps[1], B_hT], writes=[B_hT])
                  P.barrier()
              ck("mixer")

              with ExitStack() as ph:
                  def sbm(shape, dt):
                      return ph.enter_context(nc.sbuf_tensor(uname(), list(shape), dt))

                  hn2 = sbm([128, 16, D], BF16)
                  posmT = sbm([128, 256], F32)
                  gmT = sbm([128, 256], F32)
                  gmHL = sbm([128, 2, 256], BF16)
                  B_hn2, B_posmT, B_gmT, B_gmHL = Buf("hn2"), Buf("posmT"), Buf("gmT"), Buf("gmHL")
                  with ExitStack() as ph1:
                      def sb1(shape, dt):
                          return ph1.enter_context(nc.sbuf_tensor(uname(), list(shape), dt))

                      sq_ring = Ring([(sb1([128, 512], BF16), Buf("sq%d" % i)) for i in range(2)])
                      rs_ring = Ring([(sb1([128, 512], F32), Buf("rs%d" % i)) for i in range(2)])
                      rmsnorm(V_FFN + l * 8, sq_ring, rs_ring)
                      affT = sb1([NE, S], F32)
                      work = sb1([NE, S], F32)
                      t3 = sb1([NE, S], F32)
                      mx8 = sb1([NE, 8], F32)
                      B_aff, B_work, B_t3, B_mx = Buf("aff"), Buf("work"), Buf("t3"), Buf("mx8")
                      ex_ring = Ring([(sb1([NE, 512], F32), Buf("ex%d" % i)) for i in range(2)])
                      for tt in range(4):
                          tsl = slice(tt * 512, (tt + 1) * 512)
                          ps = next_ps()
                          for c in range(KC):
                              mm(ps, ps[0][0:NE, :], wr_sb[:, l, c, :], hnT[:, c, tsl], [B_wr, B_hnT],
                                 last=(c == KC - 1))
                          ex, bex = ex_ring.next()
                          P.emit(act, lambda: nc.scalar.activation(out=ex[:], in_=ps[0][0:NE, :], func=AF.Exp),
                                 reads=[ps[1]], writes=[bex])
                          ps2 = next_ps()
                          mm(ps2, ps2[0][0:NE, :], ones16[:], ex[:], [bex, B_ones], last=True)
                          P.emit(dve, lambda: nc.vector.reciprocal(affT[:, tsl], ps2[0][0:NE, :]), reads=[ps2[1]],
                                 writes=[B_aff])
                          P.emit(dve, lambda: nc.vector.tensor_tensor(out=affT[:, tsl], in0=ex[:], in1=affT[:, tsl],
                                                                      op=ALU.mult), reads=[bex, B_aff], writes=[B_aff])
                      for tc in range(16):
                          for half in range(2):
                              ps = next_ps()
                              for j in range(4):
                                  c = half * 4 + j
                                  mm(ps, ps[0][:, j * 128:(j + 1) * 128], hnT[:, c, tc * 128:(tc + 1) * 128], identB[:],
                                     [B_hnT, B_c2], last=(j == 3), start=True, stop=True)
                              copy_ps(hn2[:, tc, half * 512:(half + 1) * 512], ps[0][:, :], [ps[1]], [B_hn2], act)
                      src = affT
                      bsrc = B_aff
                      for it in range(CAP // 8):
                          P.emit(dve, lambda: nc.vector.max(out=mx8[:], in_=src[:]), reads=[bsrc], writes=[B_mx])
                          P.emit(dve, lambda: nc.vector.match_replace(out=work[:], in_to_replace=mx8[:],
                                                                      in_values=src[:], imm_value=0.0),
                                 reads=[B_mx, bsrc], writes=[B_work])
                          src, bsrc = work, B_work
                      P.emit(dve, lambda: nc.vector.tensor_tensor(out=work[:], in0=affT[:], in1=work[:],
                                                                  op=ALU.subtract),
                             reads=[B_aff, B_work], writes=[B_work])
                      P.emit(dve, lambda: nc.vector.tensor_single_scalar(out=affT[:], in_=work[:], scalar=0.0,
                                                                         op=ALU.is_gt),
                             reads=[B_work], writes=[B_aff])
                      P.emit(dve, lambda: nc.vector.tensor_tensor_scan(out=t3[:], data0=affT[:], data1=affT[:],
                                                                       initial=0.0, op0=ALU.add, op1=ALU.max),
                             reads=[B_aff], writes=[B_t3])
                      P.emit(dve, lambda: nc.vector.tensor_tensor(out=t3[:], in0=t3[:], in1=affT[:], op=ALU.mult),
                             reads=[B_t3, B_aff], writes=[B_t3])
                      for (srcT, bsrcT, dstT, bdstT) in ((t3, B_t3, posmT, B_posmT), (work, B_work, gmT, B_gmT)):
                          ps = next_ps()
                          for tc in range(16):
                              mm(ps, ps[0][:, tc * 16:(tc + 1) * 16], srcT[:, tc * 128:(tc + 1) * 128],
                                 identF[0:NE, 0:NE], [bsrcT, B_c2], last=(tc == 15), start=True, stop=True)
                          copy_ps(dstT[:, :], ps[0][:, 0:256], [ps[1]], [bdstT], act)
                      P.emit(dve, lambda: nc.vector.tensor_copy(out=gmHL[:, 0, :], in_=gmT[:, :]), reads=[B_gmT],
                             writes=[B_gmHL])
                      P.emit(dve, lambda: nc.vector.tensor_tensor(out=gmT[:, :], in0=gmT[:, :], in1=gmHL[:, 0, :],
                                                                  op=ALU.subtract),
                             reads=[B_gmT, B_gmHL], writes=[B_gmT])
                      P.emit(dve, lambda: nc.vector.tensor_copy(out=gmHL[:, 1, :], in_=gmT[:, :]), reads=[B_gmT],
                             writes=[B_gmHL])
                  P.barrier()
                  ck("route")
                  PTg = hnT[:, 0:2, :]
                  Pm = hnT[:, 2:4, :].rearrange("p a (b c) -> p (a b) c", c=256)
                  xeT = hnT[:, 4, :].rearrange("p (a c) -> p a c", c=256)
                  hm = hnT[:, 5, :].rearrange("p (a c) -> p a c", c=256)
                  yeb = hnT[:, 6, :].rearrange("p (k d) -> p k d", k=2)
                  B_PT, B_xe, B_hm, B_ye = Buf("PT"), Buf("xe"), Buf("hm"), Buf("ye")
                  Pbufs = [(Pm, Buf("P0")), (sbm([128, 16, 256], BF16), Buf("P1"))]

                  def build_P(e_):
                      Pm_, B_P_ = Pbufs[e_ % 2]
                      for tc in range(16):
                          P.emit(dve, lambda: nc.vector.tensor_scalar(
                              out=Pm_[:, tc, :], in0=iota1[:, :], scalar1=posmT[:, tc * 16 + e_:tc * 16 + e_ + 1],
                              scalar2=None, op0=ALU.is_equal), reads=[B_posmT, B_c2], writes=[B_P_])

                  we_ring = Ring([(sbm([128, KC, D], BF16), Buf("we%d" % i), P.dsem("we%d" % i)) for i in range(3)])
                  s_ring = Ring([(sbm([128, 256], F32), Buf("s%d" % i)) for i in range(2)])
                  gc_ring = Ring([(sbm([128, 2], F32), Buf("gc%d" % i)) for i in range(2)])
                  for e in range(NE):
                      ws = []
                      for wd_ in (weg_d, weu_d, wed_d):
                          wt, bw, dsw = we_ring.next()
                          P.dma(pool, dsw, [wt[:, 0:4, :], wt[:, 4:8, :]],
                                [wd_[l, e, 0:512, :].rearrange("(k p) c -> p k c", p=128),
                                 wd_[l, e, 512:1024, :].rearrange("(k p) c -> p k c", p=128)], writes=[bw])
                          ws.append((wt, bw))
                      (wg, bwg), (wu, bwu), (wdn, bwd) = ws
                      Pm, B_P = Pbufs[e % 2]
                      if e == 0:
                          build_P(0)
                      for k in range(2):
                          for tt in range(4):
                              ps = next_ps()
                              for j in range(4):
                                  tc = tt * 4 + j
                                  mm(ps, ps[0][:, j * 128:(j + 1) * 128], Pm[:, tc, k * 128:(k + 1) * 128], identB[:],
                                     [B_P, B_c2], last=(j == 3), start=True, stop=True)
                              copy_ps(PTg[:, k, tt * 512:(tt + 1) * 512], ps[0][:, :], [ps[1]], [B_PT])
                      psg = next_ps()
                      for k in range(2):
                          for tc in range(16):
                              mm(psg, psg[0][:, 2 * k:2 * k + 2], Pm[:, tc, k * 128:(k + 1) * 128],
                                 gmHL[:, :, tc * 16 + e], [B_P, B_gmHL], last=(k == 1 and tc == 15), start=(tc == 0), stop=(tc == 15))
                      gc, bgc = gc_ring.next()
                      P.emit(dve, lambda: nc.vector.reduce_sum(
                          out=gc[:, :], in_=psg[0][:, 0:4].rearrange("p (k h) -> p k h", h=2), axis=AX.X),
                          reads=[psg[1]], writes=[bgc])
                      for half in range(4):
                          ps = next_ps()
                          for j in range(2):
                              c = half * 2 + j
                              for tc in range(16):
                                  mm(ps, ps[0][:, j * 256:(j + 1) * 256], hn2[:, tc, c * 128:(c + 1) * 128], Pm[:, tc, :],
                                     [B_hn2, B_P], last=(j == 1 and tc == 15), start=(tc == 0), stop=(tc == 15))
                          copy_ps(xeT[:, half * 2:half * 2 + 2, :], ps[0][:, :].rearrange("p (a c) -> p a c", c=256),
                                  [ps[1]], [B_xe], act)
                      if e + 1 < NE:
                          build_P(e + 1)
                      for f in range(8):
                          ps = next_ps()
                          for c in range(KC):
                              mm(ps, ps[0][:, 0:256], wg[:, c, f * 128:(f + 1) * 128], xeT[:, c, :], [bwg, B_xe],
                                 start=(c == 0), stop=(c == KC - 1))
                          for c in range(KC):
                              mm(ps, ps[0][:, 256:512], wu[:, c, f * 128:(f + 1) * 128], xeT[:, c, :], [bwu, B_xe],
                                 last=(c == KC - 1), start=(c == 0))
                          st, bst = s_ring.next()
                          P.emit(act, lambda: nc.scalar.activation(out=st[:], in_=ps[0][:, 0:256], func=AF.Silu),
                                 reads=[ps[1]], writes=[bst])
                          P.emit(dve, lambda: nc.vector.tensor_tensor(out=hm[:, f, :], in0=ps[0][:, 256:512], in1=st[:],
                                                                      op=ALU.mult),
                                 reads=[ps[1], bst], writes=[B_hm])
                      for k in range(2):
                          for dh in range(2):
                              ps = next_ps()
                              for f in range(8):
                                  mm(ps, ps[0][:, :], hm[:, f, k * 128:(k + 1) * 128], wdn[:, f, dh * 512:(dh + 1) * 512],
                                     [bwd, B_hm], last=(f == 7))
                              P.emit(act, lambda: nc.scalar.activation(out=yeb[:, k, dh * 512:(dh + 1) * 512],
                                                                       in_=ps[0][:, :], func=AF.Copy,
                                                                       scale=gc[:, k:k + 1]),
                                     reads=[ps[1], bgc], writes=[B_ye])
                      for jo in range(8):
                          for tt in range(4):
                              tsl = slice(tt * 512, (tt + 1) * 512)
                              ps = next_ps()
                              for k in range(2):
                                  mm(ps, ps[0][:, :], yeb[:, k, jo * 128:(jo + 1) * 128], PTg[:, k, tsl], [B_ye, B_PT],
                                     last=(k == 1))
                              P.emit(dve, lambda: nc.vector.tensor_tensor(out=hT[:, jo, tsl], in0=ps[0][:, :],
                                                                          in1=hT[:, jo, tsl], op=ALU.add),
                                     reads=[ps[1], B_hT], writes=[B_hT])
                  P.barrier()

          with ExitStack() as ph:
              def sbf(shape, dt):
                  return ph.enter_context(nc.sbuf_tensor(uname(), list(shape), dt))

              sq_ring = Ring([(sbf([128, 512], BF16), Buf("sq%d" % i)) for i in range(2)])
              rs_ring = Ring([(sbf([128, 512], F32), Buf("rs%d" % i)) for i in range(2)])
              stage_ring = Ring([(sbf([128, 512], F32), Buf("stg%d" % i), P.dsem("out%d" % i)) for i in range(3)])
              rmsnorm(V_FIN, sq_ring, rs_ring, final_seq=s, stage_ring=stage_ring)
              P.barrier()

    except _Stop:
        P.barrier()
        for c in range(KC):
            P.dma(sp, ds_out, outT[0, c * 128:(c + 1) * 128, :], hT[:, c, :], reads=[B_hT])

    for d in P.dsems:
        if d.val and sp.waited.get(d.key, 0) < d.val:
            sp.h.wait_ge(d.sem, d.val)
    nc._keep_es = es if False else None; globals().setdefault("_KEEP", []).append(es)
    return nc


def _tables():
    kk = np.arange(128)[:, None]
    qq = np.arange(128)[None, :]
    tabs = np.zeros((128, 2, 3, 128), np.float32)
    for ti in range(3):
        rel = np.abs(kk + (ti - 1) * 128 - qq).astype(np.float32)
        tabs[:, 0, ti, :] = np.where(rel <= 128, rel, 1e6)
        tabs[:, 1, ti, :] = np.where(rel <= 64, rel, 1e6)
    cst2 = np.zeros((128, 384), np.float32)
    cst2[:, 0:128] = np.eye(128, dtype=np.float32)
    cst2[:, 128:384] = np.arange(1, 257, dtype=np.float32)[None, :]
    return tabs.reshape(128, -1), cst2


def prep_weights(norm_mix, w_in, w_branch_a, w_branch_b, b_gate, sink_logit, w_out, norm_ffn, w_router,
                 w_expert_gate, w_expert_up, w_expert_down, norm_final, L=DEPTH):
    f = np.float32
    w_in = np.asarray(w_in, f)
    wa = np.empty((L, 6, D, 384), f)
    for jp in range(2):
        for g in range(3):
            c0 = g * 256 + jp * 128
            u = jp * 3 + g
            wa[:, u, :, 0:128] = w_in[:L, :, c0:c0 + 128]
            wa[:, u, :, 128:256] = w_in[:L, :, 768 + c0:768 + c0 + 128]
            wa[:, u, :, 256:384] = w_in[:L, :, 1536 + c0:1536 + c0 + 128]
    q0, k0, v0, g0 = 2304, 3328, 3584, 3840
    wqw = np.ascontiguousarray(w_in[:L, :, q0:q0 + 1024].reshape(L, D, 8, 128).transpose(0, 2, 1, 3))
    wkvw = np.empty((L, 4, D, 192), f)
    for g in range(4):
        wkvw[:, g, :, 0:64] = w_in[:L, :, k0 + g * 64:k0 + (g + 1) * 64]
        wkvw[:, g, :, 64:128] = w_in[:L, :, k0 + g * 64:k0 + (g + 1) * 64]
        wkvw[:, g, :, 128:192] = w_in[:L, :, v0 + g * 64:v0 + (g + 1) * 64]
    wtail = np.empty((L, 8, D, 256), f)
    for j in range(8):
        wtail[:, j, :, 0:128] = w_in[:L, :, g0 + j * 128:g0 + (j + 1) * 128]
        wtail[:, j, :, 128:256] = w_in[:L, :, g0 + 1024 + j * 128:g0 + 1024 + (j + 1) * 128]
    wba = np.ascontiguousarray(np.asarray(w_branch_a, f)[:L].reshape(L, 256, 8, 128).transpose(0, 2, 1, 3))
    wbb = np.ascontiguousarray(np.asarray(w_branch_b, f)[:L].reshape(L, D, 8, 128).transpose(0, 2, 1, 3))

    def pm(v):
        v = np.asarray(v, f)
        return v.reshape(v.shape[:-1] + (8, 128))

    NV = L * 40 + 8
    vecs = np.zeros((128, NV), f)
    vecs[:, 0:L * 8] = pm(norm_mix)[:L].transpose(2, 0, 1).reshape(128, L * 8)
    vecs[:, L * 8:L * 16] = pm(norm_ffn)[:L].transpose(2, 0, 1).reshape(128, L * 8)
    bg = np.asarray(b_gate, f)[:L].reshape(L, 2, 8, 128)
    vecs[:, L * 16:L * 32] = bg.transpose(3, 0, 1, 2).reshape(128, L * 16)
    vecs[:, L * 32:L * 32 + 8] = pm(norm_final).transpose(1, 0)
    sk = np.asarray(sink_logit, f)[:L].reshape(L, 8, 2)
    sp = np.repeat(sk.transpose(2, 0, 1)[:, None], 64, axis=1).reshape(128, L * 8)
    vecs[:, L * 32 + 8:] = sp
    tabs, cst2 = _tables()
    return {
        "wa": wa, "wqw": wqw, "wkvw": wkvw, "wtail": wtail, "wba": wba, "wbb": wbb,
        "wout": np.ascontiguousarray(np.asarray(w_out, f)[:L]),
        "wr": np.ascontiguousarray(np.asarray(w_router, f)[:L]),
        "weg": np.ascontiguousarray(np.asarray(w_expert_gate, f)[:L]),
        "weu": np.ascontiguousarray(np.asarray(w_expert_up, f)[:L]),
        "wed": np.ascontiguousarray(np.asarray(w_expert_down, f)[:L]),
        "vecs": vecs, "tabs": tabs, "cst2": cst2,
    }


def kernel(x, norm_mix, w_in, w_branch_a, w_branch_b, b_gate, sink_logit, w_out, norm_ffn, w_router,
           w_expert_gate, w_expert_up, w_expert_down, norm_final):
    x = np.asarray(x, np.float32)
    wts = prep_weights(norm_mix, w_in, w_branch_a, w_branch_b, b_gate, sink_logit, w_out, norm_ffn, w_router,
                       w_expert_gate, w_expert_up, w_expert_down, norm_final)
    nc = build_program(DEPTH, NSEQ)
    in_maps = []
    for c in range(NCORES):
        m = dict(wts)
        m["xT"] = np.ascontiguousarray(x[c * NSEQ:(c + 1) * NSEQ].transpose(0, 2, 1))
        in_maps.append(m)
    res = run_bass_kernel_spmd(nc, in_maps, core_ids=list(range(NCORES)))
    out = np.empty((NCORES * NSEQ, S, D), np.float32)
    for c in range(NCORES):
        out[c * NSEQ:(c + 1) * NSEQ] = res.results[c]["outT"].transpose(0, 2, 1)
    return out
```

```python
import numpy as np
from contextlib import ExitStack
import concourse.bass as bass
import concourse.mybir as mybir
from concourse.bass_utils import run_bass_kernel_spmd

F32 = mybir.dt.float32
BF16 = mybir.dt.bfloat16
AF = mybir.ActivationFunctionType
ALU = mybir.AluOpType
AX = mybir.AxisListType

D = 1024
S = 2048
DEPTH = 4
NCORES = 8
NSEQ = 4
KC = 8
NE = 16
CAP = 256
EPS = 1e-6
DIL_R = (1, 4, 16)


def _slopes(n):
    return [float(2.0 ** (-8.0 * i / n)) for i in range(1, n + 1)]


SL_DIL = _slopes(12)
SL_WIN = _slopes(16)


class Ev:
    __slots__ = ("sem", "val", "key")

    def __init__(self, sem, val, key):
        self.sem, self.val, self.key = sem, val, key


class Buf:
    __slots__ = ("name", "w", "r", "excl")

    def __init__(self, name, excl=False):
        self.name = name
        self.w = None
        self.r = {}
        self.excl = excl


class Eng:
    def __init__(self, name, h, sem, is_pe=False):
        self.name, self.h, self.sem, self.is_pe = name, h, sem, is_pe
        self.cnt = 0
        self.waited = {}


class DSem:
    def __init__(self, sem, key):
        self.sem, self.key, self.val = sem, key, 0


class Prog:
    def __init__(self, nc, es):
        self.nc = nc
        self.es = es
        mk = lambda n: es.enter_context(nc.semaphore(n))
        self.pe = Eng("pe", nc.tensor, mk("s_pe"), True)
        self.act = Eng("act", nc.scalar, mk("s_act"))
        self.dve = Eng("dve", nc.vector, mk("s_dve"))
        self.pool = Eng("pool", nc.gpsimd, mk("s_pool"))
        self.sp = Eng("sp", nc.sync, mk("s_sp"))
        self.engs = [self.pe, self.act, self.dve, self.pool, self.sp]
        self.dsems = []
        self.dtags = {}
        self.nds = 0

    def dsem(self, tag=None):
        if tag is not None and tag in self.dtags:
            return self.dtags[tag]
        self.nds += 1
        d = DSem(self.es.enter_context(self.nc.semaphore("s_dma%d" % self.nds)), "dma%d" % self.nds)
        self.dsems.append(d)
        if tag is not None:
            self.dtags[tag] = d
        return d

    def need(self, eng, ev, raw):
        if ev is None:
            return
        if ev.key == eng.name and eng.is_pe:
            return
        if eng.waited.get(ev.key, 0) >= ev.val:
            return
        eng.h.wait_ge(ev.sem, ev.val)
        eng.waited[ev.key] = ev.val

    def deps(self, eng, reads, writes):
        for b in reads:
            self.need(eng, b.w, True)
            if b.excl:
                for ev in b.r.values():
                    if ev.key != eng.name:
                        self.need(eng, ev, False)
        for b in writes:
            self.need(eng, b.w, False)
            for ev in b.r.values():
                self.need(eng, ev, False)

    def mark(self, ev, reads, writes):
        for b in writes:
            b.w = ev
            b.r = {}
        for b in reads:
            o = b.r.get(ev.key)
            if o is None or o.val < ev.val:
                b.r[ev.key] = ev

    def emit(self, eng, fn, reads=(), writes=(), signal=True):
        self.deps(eng, reads, writes)
        ins = fn()
        if signal:
            eng.cnt += 1
            ins.then_inc(eng.sem, 1)
            ev = Ev(eng.sem, eng.cnt, eng.name)
        else:
            ev = Ev(eng.sem, eng.cnt + 1, eng.name)
        self.mark(ev, reads, writes)
        return ins

    def dma(self, q, ds, out_ap, in_ap, reads=(), writes=()):
        self.deps(q, reads, writes)
        if not isinstance(out_ap, list):
            out_ap, in_ap = [out_ap], [in_ap]
        for o, i in zip(out_ap, in_ap):
            ds.val += 16
            q.h.dma_start(out=o, in_=i).then_inc(ds.sem, 16)
        ev = Ev(ds.sem, ds.val, ds.key)
        self.mark(ev, reads, writes)

    def barrier(self):
        for e in self.engs:
            for f in self.engs:
                if f is e or f.cnt == 0:
                    continue
                if e.waited.get(f.name, 0) < f.cnt:
                    e.h.wait_ge(f.sem, f.cnt)
                    e.waited[f.name] = f.cnt
            for d in self.dsems:
                if d.val and e.waited.get(d.key, 0) < d.val:
                    e.h.wait_ge(d.sem, d.val)
                    e.waited[d.key] = d.val


class _Stop(Exception):
    pass


class Ring:
    def __init__(self, items):
        self.items = items
        self.i = -1

    def next(self):
        self.i = (self.i + 1) % len(self.items)
        return self.items[self.i]


def build_program(L=DEPTH, NS=NSEQ, stop_after=None):
    nc = bass.Bass("TRN2", target_bir_lowering=False)
    es = ExitStack()
    P = Prog(nc, es)
    pe, act, dve, pool, sp = P.pe, P.act, P.dve, P.pool, P.sp

    def din(name, shape):
        return nc.dram_tensor(name, list(shape), F32, kind="ExternalInput").ap()

    NV = L * 8 + L * 8 + L * 16 + 8 + L * 8
    V_MIX, V_FFN, V_BG, V_FIN, V_SINK = 0, L * 8, L * 16, L * 32, L * 32 + 8
    xT = din("xT", [NS, D, S])
    wa_d = din("wa", [L, 6, D, 384])
    wqw_d = din("wqw", [L, 8, D, 128])
    wkvw_d = din("wkvw", [L, 4, D, 192])
    wtail_d = din("wtail", [L, 8, D, 256])
    wba_d = din("wba", [L, 8, 256, 128])
    wbb_d = din("wbb", [L, 8, D, 128])
    wout_d = din("wout", [L, D, D])
    wr_d = din("wr", [L, D, NE])
    weg_d = din("weg", [L, NE, D, D])
    weu_d = din("weu", [L, NE, D, D])
    wed_d = din("wed", [L, NE, D, D])
    vecs_d = din("vecs", [128, NV])
    tabs_d = din("tabs", [128, 2 * 3 * 128])
    cst2_d = din("cst2", [128, 384])
    outT = nc.dram_tensor("outT", [NS, D, S], F32, kind="ExternalOutput").ap()

    def sb(name, shape, dt):
        return es.enter_context(nc.sbuf_tensor(name, list(shape), dt))

    uctr = [0]

    def uname():
        uctr[0] += 1
        return "t%d" % uctr[0]

    hT = sb("hT", [128, KC, S], F32)
    hnT = sb("hnT", [128, KC, S], BF16)
    vec = sb("vec", [128, NV], F32)
    esink = sb("esink", [128, L * 8], F32)
    tab = sb("tab", [128, 2, 3, 128], F32)
    ones16 = sb("ones16", [NE, NE], F32)
    onesF = sb("onesF", [128, 128], BF16)
    onesA0 = sb("onesA0", [128, 128], BF16)
    ones0B = sb("ones0B", [128, 128], BF16)
    wr_sb = sb("wr_sb", [128, L, KC, NE], BF16)
    epsb = sb("epsb", [128, 1], F32)
    B_hnT, B_const = Buf("hnT"), Buf("const")
    B_hTt = {(c_, t_): Buf("hT%d_%d" % (c_, t_)) for c_ in range(KC) for t_ in range(4)}
    B_hT_all = list(B_hTt.values())
    B_wr = Buf("wr")
    identF = sb("identF", [128, 128], F32)
    iota1 = sb("iota1", [128, 256], F32)
    identB = sb("identB", [128, 128], BF16)
    B_c2 = Buf("c2")

    PSB = []
    for i in range(8):
        t = es.enter_context(nc.psum_tensor("ps%d" % i, [128, 512], F32))
        PSB.append((t, Buf("ps%d" % i, excl=True)))
    ps_ring = Ring(PSB)
    fresh = {}

    def next_ps():
        t, b = ps_ring.next()
        fresh[b.name] = True
        return t, b

    def mm(psb, out_ap, lhsT, rhs, reads, last=False, sig=False, start=None, stop=None):
        b = psb[1]
        st = fresh[b.name] if start is None else start
        fresh[b.name] = False
        sp_ = last if stop is None else (stop or last)
        P.emit(pe, lambda: nc.tensor.matmul(out_ap, lhsT, rhs, start=st, stop=sp_),
               reads=reads, writes=[b], signal=(last or sig))

    ds_c = P.dsem()
    P.dma(sp, ds_c, [vec[:, :], tab[:].rearrange("p a b c -> p (a b c)")],
          [vecs_d[:, :], tabs_d[:, :]], writes=[B_const])
    ds_c2 = P.dsem()
    P.dma(sp, ds_c2, [identF[:, :], iota1[:, :]], [cst2_d[:, 0:128], cst2_d[:, 128:384]], writes=[B_c2])
    ds_c3 = P.dsem()
    P.dma(pool, ds_c3, identB[:, :], cst2_d[:, 0:128], writes=[B_c2])
    ds_wr = P.dsem()
    P.dma(pool, ds_wr, [wr_sb[:, l, :, :] for l in range(L)],
          [wr_d[l].rearrange("(k p) e -> p k e", p=128) for l in range(L)], writes=[B_wr])
    B_ones = Buf("ones")
    P.emit(dve, lambda: nc.vector.memset(onesF[:], 1.0), writes=[B_ones])
    P.emit(dve, lambda: nc.vector.memset(onesA0[:], 0.0), writes=[B_ones])
    P.emit(dve, lambda: nc.vector.memset(ones0B[:], 0.0), writes=[B_ones])
    P.emit(dve, lambda: nc.vector.memset(onesA0[:, 0:64], 1.0), writes=[B_ones])
    P.emit(dve, lambda: nc.vector.memset(ones0B[:, 64:128], 1.0), writes=[B_ones])
    P.emit(dve, lambda: nc.vector.memset(ones16[:], 1.0), writes=[B_ones])
    P.emit(dve, lambda: nc.vector.memset(epsb[:], EPS), writes=[B_ones])
    B_esink = Buf("esink")
    P.emit(act, lambda: nc.scalar.activation(out=esink[:], in_=vec[:, V_SINK:V_SINK + L * 8], func=AF.Exp),
           reads=[B_const], writes=[B_esink])

    ds_x = P.dsem("x")
    ds_out = P.dsem("dbg")

    def rmsnorm(gcol, sq_ring, rs_ring, final_seq=None, stage_ring=None):
        for tt in range(4):
            tsl = slice(tt * 512, (tt + 1) * 512)
            ps = next_ps()
            for c in range(KC):
                sq, bsq = sq_ring.next()
                P.emit(act, lambda: nc.scalar.activation(out=sq[:], in_=hT[:, c, tsl], func=AF.Square),
                       reads=[B_hTt[(c, tt)]], writes=[bsq])
                mm(ps, ps[0][:, :], onesF[:], sq[:], [bsq, B_ones], last=(c == KC - 1), sig=True)
            rs, brs = rs_ring.next()
            P.emit(act, lambda: nc.scalar.activation(out=rs[:], in_=ps[0][:, :], func=AF.Sqrt,
                                                     scale=1.0 / D, bias=epsb[:, 0:1]),
                   reads=[ps[1], B_ones], writes=[brs])
            P.emit(dve, lambda: nc.vector.reciprocal(rs[:], rs[:]), reads=[brs], writes=[brs])
            for c in range(KC):
                if final_seq is None:
                    P.emit(dve, lambda: nc.vector.scalar_tensor_tensor(
                        out=hnT[:, c, tsl], in0=hT[:, c, tsl], scalar=vec[:, gcol + c:gcol + c + 1],
                        in1=rs[:], op0=ALU.mult, op1=ALU.mult),
                        reads=[B_hTt[(c, tt)], brs, B_const], writes=[B_hnT])
                else:
                    st, bst, dso = stage_ring.next()
                    P.emit(dve, lambda: nc.vector.scalar_tensor_tensor(
                        out=st[:], in0=hT[:, c, tsl], scalar=vec[:, gcol + c:gcol + c + 1],
                        in1=rs[:], op0=ALU.mult, op1=ALU.mult),
                        reads=[B_hTt[(c, tt)], brs, B_const], writes=[bst])
                    P.dma(sp, dso, outT[final_seq, c * 128:(c + 1) * 128, tsl], st[:], reads=[bst])

    def perm512(c, ut, r):
        if r == 1:
            return hnT[:, c, ut * 512:(ut + 1) * 512]
        if r == 4:
            return hnT[:, c, ut:S:4]
        return hnT[:, c, :].rearrange("p (a q) -> p q a", q=16)[:, 4 * ut:4 * ut + 4, :]

    def perm128(c, uc, r):
        if r == 1:
            return hnT[:, c, uc * 128:(uc + 1) * 128]
        if r == 4:
            sub, a0 = uc // 4, (uc % 4) * 128
            return hnT[:, c, sub + 4 * a0:sub + 4 * (a0 + 127) + 1:4]
        return hnT[:, c, uc:S:16]

    def nat128(qt, r):
        if r == 1:
            return slice(qt * 128, (qt + 1) * 128)
        if r == 4:
            sub, a0 = qt // 4, (qt % 4) * 128
            return slice(sub + 4 * a0, sub + 4 * (a0 + 127) + 1, 4)
        return slice(qt, S, 16)

    def ps_view(ps, r):
        if r == 16:
            return ps[0][:, :].rearrange("p (q a) -> p q a", q=4)
        return ps[0][:, :]

    cp_flip = [0]

    def copy_ps(out_ap, in_ap, reads, writes, eng=None):
        if eng is None:
            cp_flip[0] ^= 1
            eng = act if cp_flip[0] else dve
        if eng is act:
            P.emit(act, lambda: nc.scalar.copy(out=out_ap, in_=in_ap), reads=reads, writes=writes)
        else:
            P.emit(dve, lambda: nc.vector.tensor_copy(out=out_ap, in_=in_ap), reads=reads, writes=writes)

    def ck(name):
        if stop_after == name:
            raise _Stop()

    try:
      for s in range(NS):
          P.dma(sp, ds_x, [hT[:, c, :] for c in range(KC)], [xT[s, c * 128:(c + 1) * 128, :] for c in range(KC)],
                writes=B_hT_all)
          for l in range(L):
              with ExitStack() as ph:
                  def sbp(name, shape, dt):
                      return ph.enter_context(nc.sbuf_tensor(uname(), list(shape), dt))

                  sq_ring = Ring([(sbp("sq", [128, 512], BF16), Buf("sq%d" % i)) for i in range(2)])
                  rs_ring = Ring([(sbp("rs", [128, 512], F32), Buf("rs%d" % i)) for i in range(2)])
                  OT = sbp("OT", [128, 10, S], BF16)
                  B_OT = [Buf("OT%d" % i) for i in range(10)]
                  ck("load")
                  rmsnorm(V_MIX + l * 8, sq_ring, rs_ring)
                  ck("norm")

                  with ExitStack() as ph2:
                      def sb2(shape, dt):
                          return ph2.enter_context(nc.sbuf_tensor(uname(), list(shape), dt))

                      QA0, Q0B, K2 = sb2([128, S], BF16), sb2([128, S], BF16), sb2([128, S], BF16)
                      VA0, V0B = sb2([128, 16, 128], BF16), sb2([128, 16, 128], BF16)
                      acc = sb2([128, 2, S], F32)
                      B_QA, B_QB, B_K, B_VA, B_VB, B_acc = (Buf(n) for n in ("QA", "QB", "K", "VA", "VB", "acc"))
                      wa_ring = Ring([(sb2([128, KC, 384], BF16), Buf("wa%d" % i), P.dsem("wa%d" % i)) for i in range(1)])
                      wq_ring = Ring([(sb2([128, KC, 128], BF16), Buf("wq%d" % i), P.dsem("wq%d" % i)) for i in range(1)])
                      wkv_ring = Ring([(sb2([128, KC, 192], BF16), Buf("wkv%d" % i), P.dsem("wkv%d" % i)) for i in range(1)])
                      tS_ring = Ring([(sb2([128, 384], F32), Buf("tS%d" % i)) for i in range(2)])
                      PT_ring = Ring([(sb2([128, 384], BF16), Buf("PT%d" % i)) for i in range(4)])
                      dn_ring = Ring([(sb2([128, 128], F32), Buf("dn%d" % i)) for i in range(2)])
                      P.emit(dve, lambda: nc.vector.memset(QA0[64:128, :], 0.0), writes=[B_QA])
                      P.emit(dve, lambda: nc.vector.memset(Q0B[0:64, :], 0.0), writes=[B_QB])
                      P.emit(dve, lambda: nc.vector.memset(VA0[:, :, 64:128], 0.0), writes=[B_VA])
                      P.emit(dve, lambda: nc.vector.memset(V0B[:, :, 0:64], 0.0), writes=[B_VB])
                      ck("ph2alloc")

                      def perm_views(dst, src_ps, ut, r):
                          if r == 1:
                              return dst[:, ut * 512:(ut + 1) * 512], src_ps
                          n = 512 // r
                          o = dst.rearrange("p (c a) -> p c a", c=r)[:, :, ut * n:(ut + 1) * n]
                          i = src_ps.rearrange("p (a c) -> p c a", c=r)
                          return o, i

                      def proj_q(wt, bw, col0, r):
                          for ut in range(4):
                              ps = next_ps()
                              for c in range(KC):
                                  mm(ps, ps[0][:, :], wt[:, c, col0:col0 + 128], hnT[:, c, ut * 512:(ut + 1) * 512],
                                     [bw, B_hnT], last=(c == KC - 1))
                              o, i = perm_views(QA0[0:64, :], ps[0][0:64, :], ut, r)
                              copy_ps(o, i, [ps[1]], [B_QA], act)
                              o, i = perm_views(Q0B[64:128, :], ps[0][64:128, :], ut, r)
                              copy_ps(o, i, [ps[1]], [B_QB], dve)

                      def proj_k(wt, bw, col0, r):
                          for ut in range(4):
                              ps = next_ps()
                              for c in range(KC):
                                  mm(ps, ps[0][:, :], wt[:, c, col0:col0 + 128], hnT[:, c, ut * 512:(ut + 1) * 512],
                                     [bw, B_hnT], last=(c == KC - 1))
                              o, i = perm_views(K2[:, :], ps[0][:, :], ut, r)
                              copy_ps(o, i, [ps[1]], [B_K])

                      def attention(r, tabi, scA, scB, epilogue):
                          tps = (S // r) // 128

                          def stage1(qt):
                              jj = qt % tps
                              chunks = []
                              if jj > 0:
                                  chunks.append((qt - 1, 0))
                              chunks.append((qt, 1))
                              if jj < tps - 1:
                                  chunks.append((qt + 1, 2))
                              lo, hi = chunks[0][1] * 128, (chunks[-1][1] + 1) * 128
                              qsl = slice(qt * 128, (qt + 1) * 128)
                              pts = []
                              for (QX, BQ, sc) in ((QA0, B_QA, scA), (Q0B, B_QB, scB)):
                                  ps = next_ps()
                                  for i, (kc, ti) in enumerate(chunks):
                                      mm(ps, ps[0][:, ti * 128:(ti + 1) * 128], K2[:, kc * 128:(kc + 1) * 128],
                                         QX[:, qsl], [B_K, BQ], last=(i == len(chunks) - 1))
                                  tS, btS = tS_ring.next()
                                  P.emit(dve, lambda: nc.vector.scalar_tensor_tensor(
                                      out=tS[:, lo:hi], in0=tab[:, tabi].rearrange("p a b -> p (a b)")[:, lo:hi],
                                      scalar=sc, in1=ps[0][:, lo:hi], op0=ALU.mult, op1=ALU.add),
                                      reads=[ps[1], B_const], writes=[btS])
                                  PT, bPT = PT_ring.next()
                                  P.emit(act, lambda: nc.scalar.activation(out=PT[:, lo:hi], in_=tS[:, lo:hi],
                                                                           func=AF.Exp, scale=0.125),
                                         reads=[btS], writes=[bPT])
                                  pts.append((PT, bPT))
                              return chunks, pts

                          def stage2(qt, chunks, pts):
                              pso = next_ps()
                              n = 0
                              tot = 4 * len(chunks)
                              for (PT, bPT), VX, BV, oX in ((pts[0], VA0, B_VA, onesA0), (pts[1], V0B, B_VB, ones0B)):
                                  for (kc, ti) in chunks:
                                      n += 1
                                      mm(pso, pso[0][:, 0:128], VX[:, kc, :], PT[:, ti * 128:(ti + 1) * 128],
                                         [BV, bPT], last=False)
                                      n += 1
                                      mm(pso, pso[0][:, 128:256], oX[:], PT[:, ti * 128:(ti + 1) * 128],
                                         [B_ones, bPT], last=(n == tot))
                              epilogue(pso, qt)

                          cur = stage1(0)
                          for qt in range(16):
                              nxt = stage1(qt + 1) if qt + 1 < 16 else None
                              stage2(qt, *cur)
                              cur = nxt

                      for jp in range(2):
                          for g in range(3):
                              r = DIL_R[g]
                              wt, bw, dsw = wa_ring.next()
                              P.dma(pool, dsw, wt[:], wa_d[l, jp * 3 + g].rearrange("(k p) c -> p k c", p=128),
                                    writes=[bw])
                              ck("wadma")
                              proj_q(wt, bw, 0, r)
                              ck("projq")
                              proj_k(wt, bw, 128, r)
                              ck("proj%d" % g)
                              for u0 in range(0, 16, 4):
                                  ps = next_ps()
                                  for uu in range(4):
                                      for c in range(KC):
                                          mm(ps, ps[0][:, uu * 128:(uu + 1) * 128], perm128(c, u0 + uu, r),
                                             wt[:, c, 256:384], [bw, B_hnT], last=(uu == 3 and c == KC - 1),
                                             start=(c == 0), stop=(c == KC - 1))
                                  ck("vmm")
                                  pv = ps[0][:, :].rearrange("p (n c) -> p n c", c=128)
                                  copy_ps(VA0[:, u0:u0 + 4, 0:64], pv[:, :, 0:64], [ps[1]], [B_VA], act)
                                  ck("vcpa")
                                  copy_ps(V0B[:, u0:u0 + 4, 64:128], pv[:, :, 64:128], [ps[1]], [B_VB], dve)
                              sA = -8.0 * SL_DIL[g * 4 + 2 * jp] * r
                              sB = -8.0 * SL_DIL[g * 4 + 2 * jp + 1] * r

                              def epi_dil(pso, qt, g=g, r=r):
                                  nat = nat128(qt, r)
                                  pv = pso[0][:, 0:256].rearrange("p (t q) -> p t q", t=2)
                                  if g == 0:
                                      copy_ps(acc[:, :, nat], pv, [pso[1]], [B_acc], act)
                                  else:
                                      P.emit(dve, lambda: nc.vector.tensor_tensor(out=acc[:, :, nat], in0=pv,
                                                                                  in1=acc[:, :, nat], op=ALU.add),
                                             reads=[pso[1], B_acc], writes=[B_acc])

                              ck("vproj%d" % g)
                              attention(r, 1, sA, sB, epi_dil)
                              ck("att%d" % g)
                          P.emit(dve, lambda: nc.vector.reciprocal(acc[:, 1, :], acc[:, 1, :]), reads=[B_acc],
                                 writes=[B_acc])
                          P.emit(dve, lambda: nc.vector.tensor_tensor(out=OT[:, jp, :], in0=acc[:, 0, :],
                                                                      in1=acc[:, 1, :], op=ALU.mult),
                                 reads=[B_acc], writes=[B_OT[jp]])

                      for m in range(8):
                          g = m // 2
                          if m % 2 == 0:
                              wkv, bkv, dskv = wkv_ring.next()
                              P.dma(pool, dskv, wkv[:], wkvw_d[l, g].rearrange("(k p) c -> p k c", p=128),
                                    writes=[bkv])
                              proj_k(wkv, bkv, 0, 1)
                              for u0 in range(0, 16, 8):
                                  ps = next_ps()
                                  for uu in range(8):
                                      for c in range(KC):
                                          mm(ps, ps[0][:, uu * 64:(uu + 1) * 64], perm128(c, u0 + uu, 1),
                                             wkv[:, c, 128:192], [bkv, B_hnT], last=(uu == 7 and c == KC - 1),
                                             start=(c == 0), stop=(c == KC - 1))
                                  pv = ps[0][:, :].rearrange("p (n c) -> p n c", c=64)
                                  copy_ps(VA0[:, u0:u0 + 8, 0:64], pv, [ps[1]], [B_VA], act)
                                  copy_ps(V0B[:, u0:u0 + 8, 64:128], pv, [ps[1]], [B_VB], dve)
                          wq, bq, dsq = wq_ring.next()
                          P.dma(pool, dsq, wq[:], wqw_d[l, m].rearrange("(k p) c -> p k c", p=128), writes=[bq])
                          proj_q(wq, bq, 0, 1)

                          def epi_win(pso, qt, m=m):
                              dn, bdn = dn_ring.next()
                              P.emit(dve, lambda: nc.vector.tensor_scalar(
                                  out=dn[:], in0=pso[0][:, 128:256], scalar1=esink[:, l * 8 + m:l * 8 + m + 1],
                                  scalar2=None, op0=ALU.add), reads=[pso[1], B_esink], writes=[bdn])
                              P.emit(dve, lambda: nc.vector.reciprocal(dn[:], dn[:]), reads=[bdn], writes=[bdn])
                              P.emit(dve, lambda: nc.vector.tensor_tensor(
                                  out=OT[:, 2 + m, qt * 128:(qt + 1) * 128], in0=pso[0][:, 0:128], in1=dn[:],
                                  op=ALU.mult), reads=[pso[1], bdn], writes=[B_OT[2 + m]])

                          attention(1, 0, -8.0 * SL_WIN[2 * m], -8.0 * SL_WIN[2 * m + 1], epi_win)
                          ck("win%d" % m)
                  P.barrier()

                  with ExitStack() as ph3:
                      def sb3(shape, dt):
                          return ph3.enter_context(nc.sbuf_tensor(uname(), list(shape), dt))

                      wo = sb3([128, KC, D], BF16)
                      B_wo, ds_wo = Buf("wo"), P.dsem("wo")
                      P.dma(pool, ds_wo, wo[:], wout_d[l].rearrange("(k p) c -> p k c", p=128), writes=[B_wo])
                      tw_ring = Ring([(sb3([128, KC, 256], BF16), sb3([128, KC, 128], BF16), sb3([128, 2, 128], BF16),
                                       Buf("tw%d" % i), P.dsem("tw%d" % i)) for i in range(2)])
                      mg_ring = Ring([(sb3([128, KC, 512], BF16), Buf("mg%d" % i)) for i in range(1)])
                      g_ring = Ring([(sb3([128, 512], F32), Buf("g%d" % i)) for i in range(4)])
                      for tt in range(4):
                          tsl = slice(tt * 512, (tt + 1) * 512)
                          mg, bmg = mg_ring.next()
                          for j in range(8):
                              wtj, wbbj, wbaj, btw, dstw = tw_ring.next()
                              P.dma(pool, dstw, [wtj[:], wbbj[:], wbaj[:]],
                                    [wtail_d[l, j].rearrange("(k p) c -> p k c", p=128),
                                     wbb_d[l, j].rearrange("(k p) c -> p k c", p=128),
                                     wba_d[l, j].rearrange("(k p) c -> p k c", p=128)], writes=[btw])
                              gts = []
                              for b in range(2):
                                  ps = next_ps()
                                  for c in range(KC):
                                      mm(ps, ps[0][:, :], wtj[:, c, b * 128:(b + 1) * 128], hnT[:, c, tsl],
                                         [btw, B_hnT], last=(c == KC - 1))
                                  gt, bgt = g_ring.next()
                                  col = V_BG + l * 16 + b * 8 + j
                                  P.emit(act, lambda: nc.scalar.activation(out=gt[:], in_=ps[0][:, :], func=AF.Sigmoid,
                                                                           bias=vec[:, col:col + 1], scale=1.0),
                                         reads=[ps[1], B_const], writes=[bgt])
                                  gts.append((gt, bgt))
                              psa = next_ps()
                              for p_ in range(2):
                                  mm(psa, psa[0][:, :], wbaj[:, p_, :], OT[:, p_, tsl], [btw, B_OT[p_]], last=(p_ == 1))
                              P.emit(dve, lambda: nc.vector.tensor_tensor(out=gts[0][0][:], in0=psa[0][:, :],
                                                                          in1=gts[0][0][:], op=ALU.mult),
                                     reads=[psa[1], gts[0][1]], writes=[gts[0][1]])
                              psb = next_ps()
                              for mm_ in range(8):
                                  mm(psb, psb[0][:, :], wbbj[:, mm_, :], OT[:, 2 + mm_, tsl], [btw, B_OT[2 + mm_]],
                                     last=(mm_ == 7))
                              P.emit(dve, lambda: nc.vector.tensor_tensor(out=gts[1][0][:], in0=psb[0][:, :],
                                                                          in1=gts[1][0][:], op=ALU.mult),
                                     reads=[psb[1], gts[1][1]], writes=[gts[1][1]])
                              P.emit(dve, lambda: nc.vector.tensor_tensor(out=mg[:, j, :], in0=gts[0][0][:],
                                                                          in1=gts[1][0][:], op=ALU.add),
                                     reads=[gts[0][1], gts[1][1]], writes=[bmg])
                          for jo in range(8):
                              ps = next_ps()
                              for c in range(KC):
                                  mm(ps, ps[0][:, :], wo[:, c, jo * 128:(jo + 1) * 128], mg[:, c, :], [B_wo, bmg],
                                     last=(c == KC - 1))
                              P.emit(dve, lambda: nc.vector.tensor_tensor(out=hT[:, jo, tsl], in0=ps[0][:, :],
                                                                          in1=hT[:, jo, tsl], op=ALU.add),
                                     reads=[ps[1], B_hTt[(jo, tt)]], writes=[B_hTt[(jo, tt)]])
                  P.barrier()
              ck("mixer")

              with ExitStack() as ph:
                  def sbm(shape, dt):
                      return ph.enter_context(nc.sbuf_tensor(uname(), list(shape), dt))

                  hn2 = sbm([128, 16, D], BF16)
                  posmT = sbm([128, 256], F32)
                  gmT = sbm([128, 256], F32)
                  gmHL = sbm([128, 2, 256], BF16)
                  B_hn2, B_posmT, B_gmT, B_gmHL = Buf("hn2"), Buf("posmT"), Buf("gmT"), Buf("gmHL")
                  with ExitStack() as ph1:
                      def sb1(shape, dt):
                          return ph1.enter_context(nc.sbuf_tensor(uname(), list(shape), dt))

                      sq_ring = Ring([(sb1([128, 512], BF16), Buf("sq%d" % i)) for i in range(2)])
                      rs_ring = Ring([(sb1([128, 512], F32), Buf("rs%d" % i)) for i in range(2)])
                      rmsnorm(V_FFN + l * 8, sq_ring, rs_ring)
                      affT = sb1([NE, S], F32)
                      work = sb1([NE, S], F32)
                      t3 = sb1([NE, S], F32)
                      mx8 = sb1([NE, 8], F32)
                      B_aff, B_work, B_t3, B_mx = Buf("aff"), Buf("work"), Buf("t3"), Buf("mx8")
                      ex_ring = Ring([(sb1([NE, 512], F32), Buf("ex%d" % i)) for i in range(2)])
                      for tt in range(4):
                          tsl = slice(tt * 512, (tt + 1) * 512)
                          ps = next_ps()
                          for c in range(KC):
                              mm(ps, ps[0][0:NE, :], wr_sb[:, l, c, :], hnT[:, c, tsl], [B_wr, B_hnT],
                                 last=(c == KC - 1))
                          ex, bex = ex_ring.next()
                          P.emit(act, lambda: nc.scalar.activation(out=ex[:], in_=ps[0][0:NE, :], func=AF.Exp),
                                 reads=[ps[1]], writes=[bex])
                          ps2 = next_ps()
                          mm(ps2, ps2[0][0:NE, :], ones16[:], ex[:], [bex, B_ones], last=True)
                          P.emit(dve, lambda: nc.vector.reciprocal(affT[:, tsl], ps2[0][0:NE, :]), reads=[ps2[1]],
                                 writes=[B_aff])
                          P.emit(dve, lambda: nc.vector.tensor_tensor(out=affT[:, tsl], in0=ex[:], in1=affT[:, tsl],
                                                                      op=ALU.mult), reads=[bex, B_aff], writes=[B_aff])
                      for tc in range(16):
                          for half in range(2):
                              ps = next_ps()
                              for j in range(4):
                                  c = half * 4 + j
                                  mm(ps, ps[0][:, j * 128:(j + 1) * 128], hnT[:, c, tc * 128:(tc + 1) * 128], identB[:],
                                     [B_hnT, B_c2], last=(j == 3), start=True, stop=True)
                              copy_ps(hn2[:, tc, half * 512:(half + 1) * 512], ps[0][:, :], [ps[1]], [B_hn2], act)
                      src = affT
                      bsrc = B_aff
                      for it in range(CAP // 8):
                          P.emit(dve, lambda: nc.vector.max(out=mx8[:], in_=src[:]), reads=[bsrc], writes=[B_mx])
                          P.emit(dve, lambda: nc.vector.match_replace(out=work[:], in_to_replace=mx8[:],
                                                                      in_values=src[:], imm_value=0.0),
                                 reads=[B_mx, bsrc], writes=[B_work])
                          src, bsrc = work, B_work
                      P.emit(dve, lambda: nc.vector.tensor_tensor(out=work[:], in0=affT[:], in1=work[:],
                                                                  op=ALU.subtract),
                             reads=[B_aff, B_work], writes=[B_work])
                      P.emit(dve, lambda: nc.vector.tensor_single_scalar(out=affT[:], in_=work[:], scalar=0.0,
                                                                         op=ALU.is_gt),
                             reads=[B_work], writes=[B_aff])
                      P.emit(dve, lambda: nc.vector.tensor_tensor_scan(out=t3[:], data0=affT[:], data1=affT[:],
                                                                       initial=0.0, op0=ALU.add, op1=ALU.max),
                             reads=[B_aff], writes=[B_t3])
                      P.emit(dve, lambda: nc.vector.tensor_tensor(out=t3[:], in0=t3[:], in1=affT[:], op=ALU.mult),
                             reads=[B_t3, B_aff], writes=[B_t3])
                      for (srcT, bsrcT, dstT, bdstT) in ((t3, B_t3, posmT, B_posmT), (work, B_work, gmT, B_gmT)):
                          ps = next_ps()
                          for tc in range(16):
                              mm(ps, ps[0][:, tc * 16:(tc + 1) * 16], srcT[:, tc * 128:(tc + 1) * 128],
                                 identF[0:NE, 0:NE], [bsrcT, B_c2], last=(tc == 15), start=True, stop=True)
                          copy_ps(dstT[:, :], ps[0][:, 0:256], [ps[1]], [bdstT], act)
                      P.emit(dve, lambda: nc.vector.tensor_copy(out=gmHL[:, 0, :], in_=gmT[:, :]), reads=[B_gmT],
                             writes=[B_gmHL])
                      P.emit(dve, lambda: nc.vector.tensor_tensor(out=gmT[:, :], in0=gmT[:, :], in1=gmHL[:, 0, :],
                                                                  op=ALU.subtract),
                             reads=[B_gmT, B_gmHL], writes=[B_gmT])
                      P.emit(dve, lambda: nc.vector.tensor_copy(out=gmHL[:, 1, :], in_=gmT[:, :]), reads=[B_gmT],
                             writes=[B_gmHL])
                  P.barrier()
                  ck("route")
                  PTg = hnT[:, 0:2, :]
                  Pm = hnT[:, 2:4, :].rearrange("p a (b c) -> p (a b) c", c=256)
                  xeT = hnT[:, 4, :].rearrange("p (a c) -> p a c", c=256)
                  hm = hnT[:, 5, :].rearrange("p (a c) -> p a c", c=256)
                  yeb = hnT[:, 6, :].rearrange("p (k d) -> p k d", k=2)
                  B_PT, B_xe, B_hm, B_ye = Buf("PT"), Buf("xe"), Buf("hm"), Buf("ye")
                  Pbufs = [(Pm, Buf("P0")), (sbm([128, 16, 256], BF16), Buf("P1"))]

                  def build_P(e_):
                      Pm_, B_P_ = Pbufs[e_ % 2]
                      for tc in range(16):
                          P.emit(dve, lambda: nc.vector.tensor_scalar(
                              out=Pm_[:, tc, :], in0=iota1[:, :], scalar1=posmT[:, tc * 16 + e_:tc * 16 + e_ + 1],
                              scalar2=None, op0=ALU.is_equal), reads=[B_posmT, B_c2], writes=[B_P_])

                  we_ring = Ring([(sbm([128, KC, D], BF16), Buf("we%d" % i), P.dsem("we%d" % i)) for i in range(3)])
                  s_ring = Ring([(sbm([128, 256], F32), Buf("s%d" % i)) for i in range(2)])
                  gc_ring = Ring([(sbm([128, 2], F32), Buf("gc%d" % i)) for i in range(2)])
                  for e in range(NE):
                      ws = []
                      for wd_ in (weg_d, weu_d, wed_d):
                          wt, bw, dsw = we_ring.next()
                          P.dma(pool, dsw, [wt[:, 0:4, :], wt[:, 4:8, :]],
                                [wd_[l, e, 0:512, :].rearrange("(k p) c -> p k c", p=128),
                                 wd_[l, e, 512:1024, :].rearrange("(k p) c -> p k c", p=128)], writes=[bw])
                          ws.append((wt, bw))
                      (wg, bwg), (wu, bwu), (wdn, bwd) = ws
                      Pm, B_P = Pbufs[e % 2]
                      if e == 0:
                          build_P(0)
                      for k in range(2):
                          for tt in range(4):
                              ps = next_ps()
                              for j in range(4):
                                  tc = tt * 4 + j
                                  mm(ps, ps[0][:, j * 128:(j + 1) * 128], Pm[:, tc, k * 128:(k + 1) * 128], identB[:],
                                     [B_P, B_c2], last=(j == 3), start=True, stop=True)
                              copy_ps(PTg[:, k, tt * 512:(tt + 1) * 512], ps[0][:, :], [ps[1]], [B_PT])
                      psg = next_ps()
                      for k in range(2):
                          for tc in range(16):
                              mm(psg, psg[0][:, 2 * k:2 * k + 2], Pm[:, tc, k * 128:(k + 1) * 128],
                                 gmHL[:, :, tc * 16 + e], [B_P, B_gmHL], last=(k == 1 and tc == 15), start=(tc == 0), stop=(tc == 15))
                      gc, bgc = gc_ring.next()
                      P.emit(dve, lambda: nc.vector.reduce_sum(
                          out=gc[:, :], in_=psg[0][:, 0:4].rearrange("p (k h) -> p k h", h=2), axis=AX.X),
                          reads=[psg[1]], writes=[bgc])
                      for half in range(4):
                          ps = next_ps()
                          for j in range(2):
                              c = half * 2 + j
                              for tc in range(16):
                                  mm(ps, ps[0][:, j * 256:(j + 1) * 256], hn2[:, tc, c * 128:(c + 1) * 128], Pm[:, tc, :],
                                     [B_hn2, B_P], last=(j == 1 and tc == 15), start=(tc == 0), stop=(tc == 15))
                          copy_ps(xeT[:, half * 2:half * 2 + 2, :], ps[0][:, :].rearrange("p (a c) -> p a c", c=256),
                                  [ps[1]], [B_xe], act)
                      if e + 1 < NE:
                          build_P(e + 1)
                      for f in range(8):
                          ps = next_ps()
                          for c in range(KC):
                              mm(ps, ps[0][:, 0:256], wg[:, c, f * 128:(f + 1) * 128], xeT[:, c, :], [bwg, B_xe],
                                 start=(c == 0), stop=(c == KC - 1))
                          for c in range(KC):
                              mm(ps, ps[0][:, 256:512], wu[:, c, f * 128:(f + 1) * 128], xeT[:, c, :], [bwu, B_xe],
                                 last=(c == KC - 1), start=(c == 0))
                          st, bst = s_ring.next()
                          P.emit(act, lambda: nc.scalar.activation(out=st[:], in_=ps[0][:, 0:256], func=AF.Silu),
                                 reads=[ps[1]], writes=[bst])
                          P.emit(dve, lambda: nc.vector.tensor_tensor(out=hm[:, f, :], in0=ps[0][:, 256:512], in1=st[:],
                                                                      op=ALU.mult),
                                 reads=[ps[1], bst], writes=[B_hm])
                      for k in range(2):
                          for dh in range(2):
                              ps = next_ps()
                              for f in range(8):
                                  mm(ps, ps[0][:, :], hm[:, f, k * 128:(k + 1) * 128], wdn[:, f, dh * 512:(dh + 1) * 512],
                                     [bwd, B_hm], last=(f == 7))
                              P.emit(act, lambda: nc.scalar.activation(out=yeb[:, k, dh * 512:(dh + 1) * 512],
                                                                       in_=ps[0][:, :], func=AF.Copy,
                                                                       scale=gc[:, k:k + 1]),
                                     reads=[ps[1], bgc], writes=[B_ye])
                      for jo in range(8):
                          for tt in range(4):
                              tsl = slice(tt * 512, (tt + 1) * 512)
                              ps = next_ps()
                              for k in range(2):
                                  mm(ps, ps[0][:, :], yeb[:, k, jo * 128:(jo + 1) * 128], PTg[:, k, tsl], [B_ye, B_PT],
                                     last=(k == 1))
                              P.emit(dve, lambda: nc.vector.tensor_tensor(out=hT[:, jo, tsl], in0=ps[0][:, :],
                                                                          in1=hT[:, jo, tsl], op=ALU.add),
                                     reads=[ps[1], B_hTt[(jo, tt)]], writes=[B_hTt[(jo, tt)]])
                  P.barrier()

          with ExitStack() as ph:
              def sbf(shape, dt):
                  return ph.enter_context(nc.sbuf_tensor(uname(), list(shape), dt))

              sq_ring = Ring([(sbf([128, 512], BF16), Buf("sq%d" % i)) for i in range(2)])
              rs_ring = Ring([(sbf([128, 512], F32), Buf("rs%d" % i)) for i in range(2)])
              stage_ring = Ring([(sbf([128, 512], F32), Buf("stg%d" % i), P.dsem("out%d" % i)) for i in range(3)])
              rmsnorm(V_FIN, sq_ring, rs_ring, final_seq=s, stage_ring=stage_ring)
              P.barrier()

    except _Stop:
        P.barrier()
        for c in range(KC):
            P.dma(sp, ds_out, outT[0, c * 128:(c + 1) * 128, :], hT[:, c, :], reads=B_hT_all)

    for d in P.dsems:
        if d.val and sp.waited.get(d.key, 0) < d.val:
            sp.h.wait_ge(d.sem, d.val)
    nc._keep_es = es if False else None; globals().setdefault("_KEEP", []).append(es)
    return nc


def _tables():
    kk = np.arange(128)[:, None]
    qq = np.arange(128)[None, :]
    tabs = np.zeros((128, 2, 3, 128), np.float32)
    for ti in range(3):
        rel = np.abs(kk + (ti - 1) * 128 - qq).astype(np.float32)
        tabs[:, 0, ti, :] = np.where(rel <= 128, rel, 1e6)
        tabs[:, 1, ti, :] = np.where(rel <= 64, rel, 1e6)
    cst2 = np.zeros((128, 384), np.float32)
    cst2[:, 0:128] = np.eye(128, dtype=np.float32)
    cst2[:, 128:384] = np.arange(1, 257, dtype=np.float32)[None, :]
    return tabs.reshape(128, -1), cst2


def prep_weights(norm_mix, w_in, w_branch_a, w_branch_b, b_gate, sink_logit, w_out, norm_ffn, w_router,
                 w_expert_gate, w_expert_up, w_expert_down, norm_final, L=DEPTH):
    f = np.float32
    w_in = np.asarray(w_in, f)
    wa = np.empty((L, 6, D, 384), f)
    for jp in range(2):
        for g in range(3):
            c0 = g * 256 + jp * 128
            u = jp * 3 + g
            wa[:, u, :, 0:128] = w_in[:L, :, c0:c0 + 128]
            wa[:, u, :, 128:256] = w_in[:L, :, 768 + c0:768 + c0 + 128]
            wa[:, u, :, 256:384] = w_in[:L, :, 1536 + c0:1536 + c0 + 128]
    q0, k0, v0, g0 = 2304, 3328, 3584, 3840
    wqw = np.ascontiguousarray(w_in[:L, :, q0:q0 + 1024].reshape(L, D, 8, 128).transpose(0, 2, 1, 3))
    wkvw = np.empty((L, 4, D, 192), f)
    for g in range(4):
        wkvw[:, g, :, 0:64] = w_in[:L, :, k0 + g * 64:k0 + (g + 1) * 64]
        wkvw[:, g, :, 64:128] = w_in[:L, :, k0 + g * 64:k0 + (g + 1) * 64]
        wkvw[:, g, :, 128:192] = w_in[:L, :, v0 + g * 64:v0 + (g + 1) * 64]
    wtail = np.empty((L, 8, D, 256), f)
    for j in range(8):
        wtail[:, j, :, 0:128] = w_in[:L, :, g0 + j * 128:g0 + (j + 1) * 128]
        wtail[:, j, :, 128:256] = w_in[:L, :, g0 + 1024 + j * 128:g0 + 1024 + (j + 1) * 128]
    wba = np.ascontiguousarray(np.asarray(w_branch_a, f)[:L].reshape(L, 256, 8, 128).transpose(0, 2, 1, 3))
    wbb = np.ascontiguousarray(np.asarray(w_branch_b, f)[:L].reshape(L, D, 8, 128).transpose(0, 2, 1, 3))

    def pm(v):
        v = np.asarray(v, f)
        return v.reshape(v.shape[:-1] + (8, 128))

    NV = L * 40 + 8
    vecs = np.zeros((128, NV), f)
    vecs[:, 0:L * 8] = pm(norm_mix)[:L].transpose(2, 0, 1).reshape(128, L * 8)
    vecs[:, L * 8:L * 16] = pm(norm_ffn)[:L].transpose(2, 0, 1).reshape(128, L * 8)
    bg = np.asarray(b_gate, f)[:L].reshape(L, 2, 8, 128)
    vecs[:, L * 16:L * 32] = bg.transpose(3, 0, 1, 2).reshape(128, L * 16)
    vecs[:, L * 32:L * 32 + 8] = pm(norm_final).transpose(1, 0)
    sk = np.asarray(sink_logit, f)[:L].reshape(L, 8, 2)
    sp = np.repeat(sk.transpose(2, 0, 1)[:, None], 64, axis=1).reshape(128, L * 8)
    vecs[:, L * 32 + 8:] = sp
    tabs, cst2 = _tables()
    return {
        "wa": wa, "wqw": wqw, "wkvw": wkvw, "wtail": wtail, "wba": wba, "wbb": wbb,
        "wout": np.ascontiguousarray(np.asarray(w_out, f)[:L]),
        "wr": np.ascontiguousarray(np.asarray(w_router, f)[:L]),
        "weg": np.ascontiguousarray(np.asarray(w_expert_gate, f)[:L]),
        "weu": np.ascontiguousarray(np.asarray(w_expert_up, f)[:L]),
        "wed": np.ascontiguousarray(np.asarray(w_expert_down, f)[:L]),
        "vecs": vecs, "tabs": tabs, "cst2": cst2,
    }


def kernel(x, norm_mix, w_in, w_branch_a, w_branch_b, b_gate, sink_logit, w_out, norm_ffn, w_router,
           w_expert_gate, w_expert_up, w_expert_down, norm_final):
    x = np.asarray(x, np.float32)
    wts = prep_weights(norm_mix, w_in, w_branch_a, w_branch_b, b_gate, sink_logit, w_out, norm_ffn, w_router,
                       w_expert_gate, w_expert_up, w_expert_down, norm_final)
    nc = build_program(DEPTH, NSEQ)
    in_maps = []
    for c in range(NCORES):
        m = dict(wts)
        m["xT"] = np.ascontiguousarray(x[c * NSEQ:(c + 1) * NSEQ].transpose(0, 2, 1))
        in_maps.append(m)
    res = run_bass_kernel_spmd(nc, in_maps, core_ids=list(range(NCORES)))
    out = np.empty((NCORES * NSEQ, S, D), np.float32)
    for c in range(NCORES):
        out[c * NSEQ:(c + 1) * NSEQ] = res.results[c]["outT"].transpose(0, 2, 1)
    return out
```

```python
import numpy as np
from contextlib import ExitStack
import concourse.bass as bass
import concourse.mybir as mybir
from concourse.bass_utils import run_bass_kernel_spmd

F32 = mybir.dt.float32
BF16 = mybir.dt.bfloat16
AF = mybir.ActivationFunctionType
ALU = mybir.AluOpType
AX = mybir.AxisListType

D = 1024
S = 2048
DEPTH = 4
NCORES = 8
NSEQ = 4
KC = 8
NE = 16
CAP = 256
EPS = 1e-6
DIL_R = (1, 4, 16)


def _slopes(n):
    return [float(2.0 ** (-8.0 * i / n)) for i in range(1, n + 1)]


SL_DIL = _slopes(12)
SL_WIN = _slopes(16)


class Ev:
    __slots__ = ("sem", "val", "key")

    def __init__(self, sem, val, key):
        self.sem, self.val, self.key = sem, val, key


class Buf:
    __slots__ = ("name", "w", "r", "excl")

    def __init__(self, name, excl=False):
        self.name = name
        self.w = None
        self.r = {}
        self.excl = excl


class Eng:
    def __init__(self, name, h, sem, is_pe=False):
        self.name, self.h, self.sem, self.is_pe = name, h, sem, is_pe
        self.cnt = 0
        self.waited = {}


class DSem:
    def __init__(self, sem, key):
        self.sem, self.key, self.val = sem, key, 0


class Prog:
    def __init__(self, nc, es):
        self.nc = nc
        self.es = es
        mk = lambda n: es.enter_context(nc.semaphore(n))
        self.pe = Eng("pe", nc.tensor, mk("s_pe"), True)
        self.act = Eng("act", nc.scalar, mk("s_act"))
        self.dve = Eng("dve", nc.vector, mk("s_dve"))
        self.pool = Eng("pool", nc.gpsimd, mk("s_pool"))
        self.sp = Eng("sp", nc.sync, mk("s_sp"))
        self.engs = [self.pe, self.act, self.dve, self.pool, self.sp]
        self.dsems = []
        self.dtags = {}
        self.nds = 0

    def dsem(self, tag=None):
        if tag is not None and tag in self.dtags:
            return self.dtags[tag]
        self.nds += 1
        d = DSem(self.es.enter_context(self.nc.semaphore("s_dma%d" % self.nds)), "dma%d" % self.nds)
        self.dsems.append(d)
        if tag is not None:
            self.dtags[tag] = d
        return d

    def need(self, eng, ev, raw):
        if ev is None:
            return
        if ev.key == eng.name and eng.is_pe:
            return
        if eng.waited.get(ev.key, 0) >= ev.val:
            return
        eng.h.wait_ge(ev.sem, ev.val)
        eng.waited[ev.key] = ev.val

    def deps(self, eng, reads, writes):
        for b in reads:
            self.need(eng, b.w, True)
            if b.excl:
                for ev in b.r.values():
                    if ev.key != eng.name:
                        self.need(eng, ev, False)
        for b in writes:
            self.need(eng, b.w, False)
            for ev in b.r.values():
                self.need(eng, ev, False)

    def mark(self, ev, reads, writes):
        for b in writes:
            b.w = ev
            b.r = {}
        for b in reads:
            o = b.r.get(ev.key)
            if o is None or o.val < ev.val:
                b.r[ev.key] = ev

    def emit(self, eng, fn, reads=(), writes=(), signal=True):
        self.deps(eng, reads, writes)
        ins = fn()
        if signal:
            eng.cnt += 1
            ins.then_inc(eng.sem, 1)
            ev = Ev(eng.sem, eng.cnt, eng.name)
        else:
            ev = Ev(eng.sem, eng.cnt + 1, eng.name)
        self.mark(ev, reads, writes)
        return ins

    def dma(self, q, ds, out_ap, in_ap, reads=(), writes=()):
        self.deps(q, reads, writes)
        if not isinstance(out_ap, list):
            out_ap, in_ap = [out_ap], [in_ap]
        for o, i in zip(out_ap, in_ap):
            ds.val += 16
            q.h.dma_start(out=o, in_=i).then_inc(ds.sem, 16)
        ev = Ev(ds.sem, ds.val, ds.key)
        self.mark(ev, reads, writes)

    def barrier(self):
        for e in self.engs:
            for f in self.engs:
                if f is e or f.cnt == 0:
                    continue
                if e.waited.get(f.name, 0) < f.cnt:
                    e.h.wait_ge(f.sem, f.cnt)
                    e.waited[f.name] = f.cnt
            for d in self.dsems:
                if d.val and e.waited.get(d.key, 0) < d.val:
                    e.h.wait_ge(d.sem, d.val)
                    e.waited[d.key] = d.val


class _Stop(Exception):
    pass


class Ring:
    def __init__(self, items):
        self.items = items
        self.i = -1

    def next(self):
        self.i = (self.i + 1) % len(self.items)
        return self.items[self.i]


def build_program(L=DEPTH, NS=NSEQ, stop_after=None):
    nc = bass.Bass("TRN2", target_bir_lowering=False)
    es = ExitStack()
    P = Prog(nc, es)
    pe, act, dve, pool, sp = P.pe, P.act, P.dve, P.pool, P.sp

    def din(name, shape):
        return nc.dram_tensor(name, list(shape), F32, kind="ExternalInput").ap()

    NV = L * 8 + L * 8 + L * 16 + 8 + L * 8
    V_MIX, V_FFN, V_BG, V_FIN, V_SINK = 0, L * 8, L * 16, L * 32, L * 32 + 8
    xT = din("xT", [NS, D, S])
    wa_d = din("wa", [L, 6, D, 384])
    wqw_d = din("wqw", [L, 8, D, 128])
    wkvw_d = din("wkvw", [L, 4, D, 192])
    wtail_d = din("wtail", [L, 8, D, 256])
    wba_d = din("wba", [L, 8, 256, 128])
    wbb_d = din("wbb", [L, 8, D, 128])
    wout_d = din("wout", [L, D, D])
    wr_d = din("wr", [L, D, NE])
    weg_d = din("weg", [L, NE, D, D])
    weu_d = din("weu", [L, NE, D, D])
    wed_d = din("wed", [L, NE, D, D])
    vecs_d = din("vecs", [128, NV])
    tabs_d = din("tabs", [128, 2 * 3 * 128])
    cst2_d = din("cst2", [128, 384])
    outT = nc.dram_tensor("outT", [NS, D, S], F32, kind="ExternalOutput").ap()

    def sb(name, shape, dt):
        return es.enter_context(nc.sbuf_tensor(name, list(shape), dt))

    uctr = [0]

    def uname():
        uctr[0] += 1
        return "t%d" % uctr[0]

    hT = sb("hT", [128, KC, S], F32)
    hnT = sb("hnT", [128, KC, S], BF16)
    vec = sb("vec", [128, NV], F32)
    esink = sb("esink", [128, L * 8], F32)
    ones16 = sb("ones16", [NE, NE], F32)
    onesF = sb("onesF", [128, 128], BF16)
    onesA0 = sb("onesA0", [128, 128], BF16)
    ones0B = sb("ones0B", [128, 128], BF16)
    wr_sb = sb("wr_sb", [128, L, KC, NE], BF16)
    epsb = sb("epsb", [128, 1], F32)
    B_hnT, B_const = Buf("hnT"), Buf("const")
    B_hTt = {(c_, t_): Buf("hT%d_%d" % (c_, t_)) for c_ in range(KC) for t_ in range(4)}
    B_hT_all = list(B_hTt.values())
    B_wr = Buf("wr")
    identF = sb("identF", [128, 128], F32)
    iota1 = sb("iota1", [128, 256], F32)
    identB = sb("identB", [128, 128], BF16)
    B_c2 = Buf("c2")

    PSB = []
    for i in range(8):
        t = es.enter_context(nc.psum_tensor("ps%d" % i, [128, 512], F32))
        PSB.append((t, Buf("ps%d" % i, excl=True)))
    ps_ring = Ring(PSB)
    fresh = {}

    def next_ps():
        t, b = ps_ring.next()
        fresh[b.name] = True
        return t, b

    def mm(psb, out_ap, lhsT, rhs, reads, last=False, sig=False, start=None, stop=None):
        b = psb[1]
        st = fresh[b.name] if start is None else start
        fresh[b.name] = False
        sp_ = last if stop is None else (stop or last)
        P.emit(pe, lambda: nc.tensor.matmul(out_ap, lhsT, rhs, start=st, stop=sp_),
               reads=reads, writes=[b], signal=(last or sig))

    ds_c = P.dsem()
    P.dma(sp, ds_c, [vec[:, :]], [vecs_d[:, :]], writes=[B_const])
    ds_c2 = P.dsem()
    P.dma(sp, ds_c2, [identF[:, :], iota1[:, :]], [cst2_d[:, 0:128], cst2_d[:, 128:384]], writes=[B_c2])
    ds_c3 = P.dsem()
    P.dma(pool, ds_c3, identB[:, :], cst2_d[:, 0:128], writes=[B_c2])
    ds_wr = P.dsem()
    P.dma(pool, ds_wr, [wr_sb[:, l, :, :] for l in range(L)],
          [wr_d[l].rearrange("(k p) e -> p k e", p=128) for l in range(L)], writes=[B_wr])
    B_ones = Buf("ones")
    P.emit(dve, lambda: nc.vector.memset(onesF[:], 1.0), writes=[B_ones])
    P.emit(dve, lambda: nc.vector.memset(onesA0[:], 0.0), writes=[B_ones])
    P.emit(dve, lambda: nc.vector.memset(ones0B[:], 0.0), writes=[B_ones])
    P.emit(dve, lambda: nc.vector.memset(onesA0[:, 0:64], 1.0), writes=[B_ones])
    P.emit(dve, lambda: nc.vector.memset(ones0B[:, 64:128], 1.0), writes=[B_ones])
    P.emit(dve, lambda: nc.vector.memset(ones16[:], 1.0), writes=[B_ones])
    P.emit(dve, lambda: nc.vector.memset(epsb[:], EPS), writes=[B_ones])
    B_esink = Buf("esink")
    P.emit(act, lambda: nc.scalar.activation(out=esink[:], in_=vec[:, V_SINK:V_SINK + L * 8], func=AF.Exp),
           reads=[B_const], writes=[B_esink])

    ds_x = P.dsem("x")
    ds_out = P.dsem("dbg")

    def rmsnorm(gcol, sq_ring, rs_ring, final_seq=None, stage_ring=None):
        for tt in range(4):
            tsl = slice(tt * 512, (tt + 1) * 512)
            ps = next_ps()
            for c in range(KC):
                sq, bsq = sq_ring.next()
                P.emit(act, lambda: nc.scalar.activation(out=sq[:], in_=hT[:, c, tsl], func=AF.Square),
                       reads=[B_hTt[(c, tt)]], writes=[bsq])
                mm(ps, ps[0][:, :], onesF[:], sq[:], [bsq, B_ones], last=(c == KC - 1), sig=True)
            rs, brs = rs_ring.next()
            P.emit(act, lambda: nc.scalar.activation(out=rs[:], in_=ps[0][:, :], func=AF.Sqrt,
                                                     scale=1.0 / D, bias=epsb[:, 0:1]),
                   reads=[ps[1], B_ones], writes=[brs])
            P.emit(dve, lambda: nc.vector.reciprocal(rs[:], rs[:]), reads=[brs], writes=[brs])
            for c in range(KC):
                if final_seq is None:
                    P.emit(dve, lambda: nc.vector.scalar_tensor_tensor(
                        out=hnT[:, c, tsl], in0=hT[:, c, tsl], scalar=vec[:, gcol + c:gcol + c + 1],
                        in1=rs[:], op0=ALU.mult, op1=ALU.mult),
                        reads=[B_hTt[(c, tt)], brs, B_const], writes=[B_hnT])
                else:
                    st, bst, dso = stage_ring.next()
                    P.emit(dve, lambda: nc.vector.scalar_tensor_tensor(
                        out=st[:], in0=hT[:, c, tsl], scalar=vec[:, gcol + c:gcol + c + 1],
                        in1=rs[:], op0=ALU.mult, op1=ALU.mult),
                        reads=[B_hTt[(c, tt)], brs, B_const], writes=[bst])
                    P.dma(sp, dso, outT[final_seq, c * 128:(c + 1) * 128, tsl], st[:], reads=[bst])

    def perm512(c, ut, r):
        if r == 1:
            return hnT[:, c, ut * 512:(ut + 1) * 512]
        if r == 4:
            return hnT[:, c, ut:S:4]
        return hnT[:, c, :].rearrange("p (a q) -> p q a", q=16)[:, 4 * ut:4 * ut + 4, :]

    def perm128(c, uc, r):
        if r == 1:
            return hnT[:, c, uc * 128:(uc + 1) * 128]
        if r == 4:
            sub, a0 = uc // 4, (uc % 4) * 128
            return hnT[:, c, sub + 4 * a0:sub + 4 * (a0 + 127) + 1:4]
        return hnT[:, c, uc:S:16]

    def nat128(qt, r):
        if r == 1:
            return slice(qt * 128, (qt + 1) * 128)
        if r == 4:
            sub, a0 = qt // 4, (qt % 4) * 128
            return slice(sub + 4 * a0, sub + 4 * (a0 + 127) + 1, 4)
        return slice(qt, S, 16)

    def ps_view(ps, r):
        if r == 16:
            return ps[0][:, :].rearrange("p (q a) -> p q a", q=4)
        return ps[0][:, :]

    cp_flip = [0]

    def copy_ps(out_ap, in_ap, reads, writes, eng=None):
        if eng is None:
            cp_flip[0] ^= 1
            eng = act if cp_flip[0] else dve
        if eng is act:
            P.emit(act, lambda: nc.scalar.copy(out=out_ap, in_=in_ap), reads=reads, writes=writes)
        else:
            P.emit(dve, lambda: nc.vector.tensor_copy(out=out_ap, in_=in_ap), reads=reads, writes=writes)

    def ck(name):
        if stop_after == name:
            raise _Stop()

    try:
      for s in range(NS):
          P.dma(sp, ds_x, [hT[:, c, :] for c in range(KC)], [xT[s, c * 128:(c + 1) * 128, :] for c in range(KC)],
                writes=B_hT_all)
          for l in range(L):
              with ExitStack() as ph:
                  def sbp(name, shape, dt):
                      return ph.enter_context(nc.sbuf_tensor(uname(), list(shape), dt))

                  sq_ring = Ring([(sbp("sq", [128, 512], BF16), Buf("sq%d" % i)) for i in range(2)])
                  rs_ring = Ring([(sbp("rs", [128, 512], F32), Buf("rs%d" % i)) for i in range(2)])
                  OT = sbp("OT", [128, 10, S], BF16)
                  tab = sbp("tab", [128, 2, 3, 128], F32)
                  B_tab = Buf("tab")
                  P.dma(sp, P.dsem("tab"), tab[:].rearrange("p a b c -> p (a b c)"), tabs_d[:, :], writes=[B_tab])
                  B_OT = [Buf("OT%d" % i) for i in range(10)]
                  ck("load")
                  rmsnorm(V_MIX + l * 8, sq_ring, rs_ring)
                  ck("norm")

                  with ExitStack() as ph2:
                      def sb2(shape, dt):
                          return ph2.enter_context(nc.sbuf_tensor(uname(), list(shape), dt))

                      QA0, Q0B, K2 = sb2([128, S], BF16), sb2([128, S], BF16), sb2([128, S], BF16)
                      VA0, V0B = sb2([128, 16, 128], BF16), sb2([128, 16, 128], BF16)
                      acc = sb2([128, 2, S], F32)
                      B_QA, B_QB, B_K, B_VA, B_VB, B_acc = (Buf(n) for n in ("QA", "QB", "K", "VA", "VB", "acc"))
                      wa_ring = Ring([(sb2([128, KC, 384], BF16), Buf("wa%d" % i), P.dsem("wa%d" % i)) for i in range(1)])
                      wq_ring = Ring([(sb2([128, KC, 128], BF16), Buf("wq%d" % i), P.dsem("wq%d" % i)) for i in range(1)])
                      wkv_ring = Ring([(sb2([128, KC, 192], BF16), Buf("wkv%d" % i), P.dsem("wkv%d" % i)) for i in range(1)])
                      tS_ring = Ring([(sb2([128, 384], F32), Buf("tS%d" % i)) for i in range(2)])
                      PT_ring = Ring([(sb2([128, 384], BF16), Buf("PT%d" % i)) for i in range(4)])
                      dn_ring = Ring([(sb2([128, 128], F32), Buf("dn%d" % i)) for i in range(2)])
                      P.emit(dve, lambda: nc.vector.memset(QA0[64:128, :], 0.0), writes=[B_QA])
                      P.emit(dve, lambda: nc.vector.memset(Q0B[0:64, :], 0.0), writes=[B_QB])
                      P.emit(dve, lambda: nc.vector.memset(VA0[:, :, 64:128], 0.0), writes=[B_VA])
                      P.emit(dve, lambda: nc.vector.memset(V0B[:, :, 0:64], 0.0), writes=[B_VB])
                      ck("ph2alloc")

                      def perm_views(dst, src_ps, ut, r):
                          if r == 1:
                              return dst[:, ut * 512:(ut + 1) * 512], src_ps
                          n = 512 // r
                          o = dst.rearrange("p (c a) -> p c a", c=r)[:, :, ut * n:(ut + 1) * n]
                          i = src_ps.rearrange("p (a c) -> p c a", c=r)
                          return o, i

                      def proj_q(wt, bw, col0, r):
                          for ut in range(4):
                              ps = next_ps()
                              for c in range(KC):
                                  mm(ps, ps[0][:, :], wt[:, c, col0:col0 + 128], hnT[:, c, ut * 512:(ut + 1) * 512],
                                     [bw, B_hnT], last=(c == KC - 1))
                              o, i = perm_views(QA0[0:64, :], ps[0][0:64, :], ut, r)
                              copy_ps(o, i, [ps[1]], [B_QA], act)
                              o, i = perm_views(Q0B[64:128, :], ps[0][64:128, :], ut, r)
                              copy_ps(o, i, [ps[1]], [B_QB], dve)

                      def proj_k(wt, bw, col0, r):
                          for ut in range(4):
                              ps = next_ps()
                              for c in range(KC):
                                  mm(ps, ps[0][:, :], wt[:, c, col0:col0 + 128], hnT[:, c, ut * 512:(ut + 1) * 512],
                                     [bw, B_hnT], last=(c == KC - 1))
                              o, i = perm_views(K2[:, :], ps[0][:, :], ut, r)
                              copy_ps(o, i, [ps[1]], [B_K])

                      def attention(r, tabi, scA, scB, epilogue):
                          tps = (S // r) // 128

                          def stage1(qt):
                              jj = qt % tps
                              chunks = []
                              if jj > 0:
                                  chunks.append((qt - 1, 0))
                              chunks.append((qt, 1))
                              if jj < tps - 1:
                                  chunks.append((qt + 1, 2))
                              lo, hi = chunks[0][1] * 128, (chunks[-1][1] + 1) * 128
                              qsl = slice(qt * 128, (qt + 1) * 128)
                              pts = []
                              for (QX, BQ, sc) in ((QA0, B_QA, scA), (Q0B, B_QB, scB)):
                                  ps = next_ps()
                                  for i, (kc, ti) in enumerate(chunks):
                                      mm(ps, ps[0][:, ti * 128:(ti + 1) * 128], K2[:, kc * 128:(kc + 1) * 128],
                                         QX[:, qsl], [B_K, BQ], last=(i == len(chunks) - 1))
                                  tS, btS = tS_ring.next()
                                  P.emit(dve, lambda: nc.vector.scalar_tensor_tensor(
                                      out=tS[:, lo:hi], in0=tab[:, tabi].rearrange("p a b -> p (a b)")[:, lo:hi],
                                      scalar=sc, in1=ps[0][:, lo:hi], op0=ALU.mult, op1=ALU.add),
                                      reads=[ps[1], B_tab], writes=[btS])
                                  PT, bPT = PT_ring.next()
                                  P.emit(act, lambda: nc.scalar.activation(out=PT[:, lo:hi], in_=tS[:, lo:hi],
                                                                           func=AF.Exp, scale=0.125),
                                         reads=[btS], writes=[bPT])
                                  pts.append((PT, bPT))
                              return chunks, pts

                          def stage2(qt, chunks, pts):
                              pso = next_ps()
                              n = 0
                              tot = 4 * len(chunks)
                              for (PT, bPT), VX, BV, oX in ((pts[0], VA0, B_VA, onesA0), (pts[1], V0B, B_VB, ones0B)):
                                  for (kc, ti) in chunks:
                                      n += 1
                                      mm(pso, pso[0][:, 0:128], VX[:, kc, :], PT[:, ti * 128:(ti + 1) * 128],
                                         [BV, bPT], last=False)
                                      n += 1
                                      mm(pso, pso[0][:, 128:256], oX[:], PT[:, ti * 128:(ti + 1) * 128],
                                         [B_ones, bPT], last=(n == tot))
                              epilogue(pso, qt)

                          cur = stage1(0)
                          for qt in range(16):
                              nxt = stage1(qt + 1) if qt + 1 < 16 else None
                              stage2(qt, *cur)
                              cur = nxt

                      for jp in range(2):
                          for g in range(3):
                              r = DIL_R[g]
                              wt, bw, dsw = wa_ring.next()
                              P.dma(pool, dsw, wt[:], wa_d[l, jp * 3 + g].rearrange("(k p) c -> p k c", p=128),
                                    writes=[bw])
                              ck("wadma")
                              proj_q(wt, bw, 0, r)
                              ck("projq")
                              proj_k(wt, bw, 128, r)
                              ck("proj%d" % g)
                              for u0 in range(0, 16, 4):
                                  ps = next_ps()
                                  for uu in range(4):
                                      for c in range(KC):
                                          mm(ps, ps[0][:, uu * 128:(uu + 1) * 128], perm128(c, u0 + uu, r),
                                             wt[:, c, 256:384], [bw, B_hnT], last=(uu == 3 and c == KC - 1),
                                             start=(c == 0), stop=(c == KC - 1))
                                  ck("vmm")
                                  pv = ps[0][:, :].rearrange("p (n c) -> p n c", c=128)
                                  copy_ps(VA0[:, u0:u0 + 4, 0:64], pv[:, :, 0:64], [ps[1]], [B_VA], act)
                                  ck("vcpa")
                                  copy_ps(V0B[:, u0:u0 + 4, 64:128], pv[:, :, 64:128], [ps[1]], [B_VB], dve)
                              sA = -8.0 * SL_DIL[g * 4 + 2 * jp] * r
                              sB = -8.0 * SL_DIL[g * 4 + 2 * jp + 1] * r

                              def epi_dil(pso, qt, g=g, r=r):
                                  nat = nat128(qt, r)
                                  pv = pso[0][:, 0:256].rearrange("p (t q) -> p t q", t=2)
                                  if g == 0:
                                      copy_ps(acc[:, :, nat], pv, [pso[1]], [B_acc], act)
                                  else:
                                      P.emit(dve, lambda: nc.vector.tensor_tensor(out=acc[:, :, nat], in0=pv,
                                                                                  in1=acc[:, :, nat], op=ALU.add),
                                             reads=[pso[1], B_acc], writes=[B_acc])

                              ck("vproj%d" % g)
                              attention(r, 1, sA, sB, epi_dil)
                              ck("att%d" % g)
                          P.emit(dve, lambda: nc.vector.reciprocal(acc[:, 1, :], acc[:, 1, :]), reads=[B_acc],
                                 writes=[B_acc])
                          P.emit(dve, lambda: nc.vector.tensor_tensor(out=OT[:, jp, :], in0=acc[:, 0, :],
                                                                      in1=acc[:, 1, :], op=ALU.mult),
                                 reads=[B_acc], writes=[B_OT[jp]])

                      for m in range(8):
                          g = m // 2
                          if m % 2 == 0:
                              wkv, bkv, dskv = wkv_ring.next()
                              P.dma(pool, dskv, wkv[:], wkvw_d[l, g].rearrange("(k p) c -> p k c", p=128),
                                    writes=[bkv])
                              proj_k(wkv, bkv, 0, 1)
                              for u0 in range(0, 16, 8):
                                  ps = next_ps()
                                  for uu in range(8):
                                      for c in range(KC):
                                          mm(ps, ps[0][:, uu * 64:(uu + 1) * 64], perm128(c, u0 + uu, 1),
                                             wkv[:, c, 128:192], [bkv, B_hnT], last=(uu == 7 and c == KC - 1),
                                             start=(c == 0), stop=(c == KC - 1))
                                  pv = ps[0][:, :].rearrange("p (n c) -> p n c", c=64)
                                  copy_ps(VA0[:, u0:u0 + 8, 0:64], pv, [ps[1]], [B_VA], act)
                                  copy_ps(V0B[:, u0:u0 + 8, 64:128], pv, [ps[1]], [B_VB], dve)
                          wq, bq, dsq = wq_ring.next()
                          P.dma(pool, dsq, wq[:], wqw_d[l, m].rearrange("(k p) c -> p k c", p=128), writes=[bq])
                          proj_q(wq, bq, 0, 1)

                          def epi_win(pso, qt, m=m):
                              dn, bdn = dn_ring.next()
                              P.emit(dve, lambda: nc.vector.tensor_scalar(
                                  out=dn[:], in0=pso[0][:, 128:256], scalar1=esink[:, l * 8 + m:l * 8 + m + 1],
                                  scalar2=None, op0=ALU.add), reads=[pso[1], B_esink], writes=[bdn])
                              P.emit(dve, lambda: nc.vector.reciprocal(dn[:], dn[:]), reads=[bdn], writes=[bdn])
                              P.emit(dve, lambda: nc.vector.tensor_tensor(
                                  out=OT[:, 2 + m, qt * 128:(qt + 1) * 128], in0=pso[0][:, 0:128], in1=dn[:],
                                  op=ALU.mult), reads=[pso[1], bdn], writes=[B_OT[2 + m]])

                          attention(1, 0, -8.0 * SL_WIN[2 * m], -8.0 * SL_WIN[2 * m + 1], epi_win)
                          ck("win%d" % m)
                  P.barrier()

                  with ExitStack() as ph3:
                      def sb3(shape, dt):
                          return ph3.enter_context(nc.sbuf_tensor(uname(), list(shape), dt))

                      wo = sb3([128, KC, D], BF16)
                      B_wo, ds_wo = Buf("wo"), P.dsem("wo")
                      P.dma(pool, ds_wo, wo[:], wout_d[l].rearrange("(k p) c -> p k c", p=128), writes=[B_wo])
                      tw_ring = Ring([(sb3([128, KC, 256], BF16), sb3([128, KC, 128], BF16), sb3([128, 2, 128], BF16),
                                       Buf("tw%d" % i), P.dsem("tw%d" % i)) for i in range(2)])
                      mg_ring = Ring([(sb3([128, KC, 512], BF16), Buf("mg%d" % i)) for i in range(1)])
                      g_ring = Ring([(sb3([128, 512], F32), Buf("g%d" % i)) for i in range(4)])
                      for tt in range(4):
                          tsl = slice(tt * 512, (tt + 1) * 512)
                          mg, bmg = mg_ring.next()
                          for j in range(8):
                              wtj, wbbj, wbaj, btw, dstw = tw_ring.next()
                              P.dma(pool, dstw, [wtj[:], wbbj[:], wbaj[:]],
                                    [wtail_d[l, j].rearrange("(k p) c -> p k c", p=128),
                                     wbb_d[l, j].rearrange("(k p) c -> p k c", p=128),
                                     wba_d[l, j].rearrange("(k p) c -> p k c", p=128)], writes=[btw])
                              gts = []
                              for b in range(2):
                                  ps = next_ps()
                                  for c in range(KC):
                                      mm(ps, ps[0][:, :], wtj[:, c, b * 128:(b + 1) * 128], hnT[:, c, tsl],
                                         [btw, B_hnT], last=(c == KC - 1))
                                  gt, bgt = g_ring.next()
                                  col = V_BG + l * 16 + b * 8 + j
                                  P.emit(act, lambda: nc.scalar.activation(out=gt[:], in_=ps[0][:, :], func=AF.Sigmoid,
                                                                           bias=vec[:, col:col + 1], scale=1.0),
                                         reads=[ps[1], B_const], writes=[bgt])
                                  gts.append((gt, bgt))
                              psa = next_ps()
                              for p_ in range(2):
                                  mm(psa, psa[0][:, :], wbaj[:, p_, :], OT[:, p_, tsl], [btw, B_OT[p_]], last=(p_ == 1))
                              P.emit(dve, lambda: nc.vector.tensor_tensor(out=gts[0][0][:], in0=psa[0][:, :],
                                                                          in1=gts[0][0][:], op=ALU.mult),
                                     reads=[psa[1], gts[0][1]], writes=[gts[0][1]])
                              psb = next_ps()
                              for mm_ in range(8):
                                  mm(psb, psb[0][:, :], wbbj[:, mm_, :], OT[:, 2 + mm_, tsl], [btw, B_OT[2 + mm_]],
                                     last=(mm_ == 7))
                              P.emit(dve, lambda: nc.vector.tensor_tensor(out=gts[1][0][:], in0=psb[0][:, :],
                                                                          in1=gts[1][0][:], op=ALU.mult),
                                     reads=[psb[1], gts[1][1]], writes=[gts[1][1]])
                              P.emit(dve, lambda: nc.vector.tensor_tensor(out=mg[:, j, :], in0=gts[0][0][:],
                                                                          in1=gts[1][0][:], op=ALU.add),
                                     reads=[gts[0][1], gts[1][1]], writes=[bmg])
                          for jo in range(8):
                              ps = next_ps()
                              for c in range(KC):
                                  mm(ps, ps[0][:, :], wo[:, c, jo * 128:(jo + 1) * 128], mg[:, c, :], [B_wo, bmg],
                                     last=(c == KC - 1))
                              P.emit(dve, lambda: nc.vector.tensor_tensor(out=hT[:, jo, tsl], in0=ps[0][:, :],
                                                                          in1=hT[:, jo, tsl], op=ALU.add),
                                     reads=[ps[1], B_hTt[(jo, tt)]], writes=[B_hTt[(jo, tt)]])
                  P.barrier()
              ck("mixer")

              with ExitStack() as ph:
                  def sbm(shape, dt):
                      return ph.enter_context(nc.sbuf_tensor(uname(), list(shape), dt))

                  hn2 = sbm([128, 16, D], BF16)
                  posmT = sbm([128, 256], F32)
                  gmHL = sbm([128, 2, 256], BF16)
                  B_hn2, B_posmT, B_gmT, B_gmHL = Buf("hn2"), Buf("posmT"), Buf("gmT"), Buf("gmHL")
                  with ExitStack() as ph1:
                      def sb1(shape, dt):
                          return ph1.enter_context(nc.sbuf_tensor(uname(), list(shape), dt))

                      sq_ring = Ring([(sb1([128, 512], BF16), Buf("sq%d" % i)) for i in range(2)])
                      rs_ring = Ring([(sb1([128, 512], F32), Buf("rs%d" % i)) for i in range(2)])
                      rmsnorm(V_FFN + l * 8, sq_ring, rs_ring)
                      affT = sb1([NE, S], F32)
                      gmT = sb1([128, 256], F32)
                      work = sb1([NE, S], F32)
                      t3 = sb1([NE, S], F32)
                      mx8 = sb1([NE, 8], F32)
                      B_aff, B_work, B_t3, B_mx = Buf("aff"), Buf("work"), Buf("t3"), Buf("mx8")
                      ex_ring = Ring([(sb1([NE, 512], F32), Buf("ex%d" % i)) for i in range(2)])
                      for tt in range(4):
                          tsl = slice(tt * 512, (tt + 1) * 512)
                          ps = next_ps()
                          for c in range(KC):
                              mm(ps, ps[0][0:NE, :], wr_sb[:, l, c, :], hnT[:, c, tsl], [B_wr, B_hnT],
                                 last=(c == KC - 1))
                          ex, bex = ex_ring.next()
                          P.emit(act, lambda: nc.scalar.activation(out=ex[:], in_=ps[0][0:NE, :], func=AF.Exp),
                                 reads=[ps[1]], writes=[bex])
                          ps2 = next_ps()
                          mm(ps2, ps2[0][0:NE, :], ones16[:], ex[:], [bex, B_ones], last=True)
                          P.emit(dve, lambda: nc.vector.reciprocal(affT[:, tsl], ps2[0][0:NE, :]), reads=[ps2[1]],
                                 writes=[B_aff])
                          P.emit(dve, lambda: nc.vector.tensor_tensor(out=affT[:, tsl], in0=ex[:], in1=affT[:, tsl],
                                                                      op=ALU.mult), reads=[bex, B_aff], writes=[B_aff])
                      for tc in range(16):
                          for half in range(2):
                              ps = next_ps()
                              for j in range(4):
                                  c = half * 4 + j
                                  mm(ps, ps[0][:, j * 128:(j + 1) * 128], hnT[:, c, tc * 128:(tc + 1) * 128], identB[:],
                                     [B_hnT, B_c2], last=(j == 3), start=True, stop=True)
                              copy_ps(hn2[:, tc, half * 512:(half + 1) * 512], ps[0][:, :], [ps[1]], [B_hn2], act)
                      src = affT
                      bsrc = B_aff
                      for it in range(CAP // 8):
                          P.emit(dve, lambda: nc.vector.max(out=mx8[:], in_=src[:]), reads=[bsrc], writes=[B_mx])
                          P.emit(dve, lambda: nc.vector.match_replace(out=work[:], in_to_replace=mx8[:],
                                                                      in_values=src[:], imm_value=0.0),
                                 reads=[B_mx, bsrc], writes=[B_work])
                          src, bsrc = work, B_work
                      P.emit(dve, lambda: nc.vector.tensor_tensor(out=work[:], in0=affT[:], in1=work[:],
                                                                  op=ALU.subtract),
                             reads=[B_aff, B_work], writes=[B_work])
                      P.emit(dve, lambda: nc.vector.tensor_single_scalar(out=affT[:], in_=work[:], scalar=0.0,
                                                                         op=ALU.is_gt),
                             reads=[B_work], writes=[B_aff])
                      P.emit(dve, lambda: nc.vector.tensor_tensor_scan(out=t3[:], data0=affT[:], data1=affT[:],
                                                                       initial=0.0, op0=ALU.add, op1=ALU.max),
                             reads=[B_aff], writes=[B_t3])
                      P.emit(dve, lambda: nc.vector.tensor_tensor(out=t3[:], in0=t3[:], in1=affT[:], op=ALU.mult),
                             reads=[B_t3, B_aff], writes=[B_t3])
                      for (srcT, bsrcT, dstT, bdstT) in ((t3, B_t3, posmT, B_posmT), (work, B_work, gmT, B_gmT)):
                          ps = next_ps()
                          for tc in range(16):
                              mm(ps, ps[0][:, tc * 16:(tc + 1) * 16], srcT[:, tc * 128:(tc + 1) * 128],
                                 identF[0:NE, 0:NE], [bsrcT, B_c2], last=(tc == 15), start=True, stop=True)
                          copy_ps(dstT[:, :], ps[0][:, 0:256], [ps[1]], [bdstT], act)
                      P.emit(dve, lambda: nc.vector.tensor_copy(out=gmHL[:, 0, :], in_=gmT[:, :]), reads=[B_gmT],
                             writes=[B_gmHL])
                      P.emit(dve, lambda: nc.vector.tensor_tensor(out=gmT[:, :], in0=gmT[:, :], in1=gmHL[:, 0, :],
                                                                  op=ALU.subtract),
                             reads=[B_gmT, B_gmHL], writes=[B_gmT])
                      P.emit(dve, lambda: nc.vector.tensor_copy(out=gmHL[:, 1, :], in_=gmT[:, :]), reads=[B_gmT],
                             writes=[B_gmHL])
                  P.barrier()
                  ck("route")
                  PTg = hnT[:, 0:2, :]
                  Pm = hnT[:, 2:4, :].rearrange("p a (b c) -> p (a b) c", c=256)
                  xeT = hnT[:, 4, :].rearrange("p (a c) -> p a c", c=256)
                  hm = hnT[:, 5, :].rearrange("p (a c) -> p a c", c=256)
                  yeb = hnT[:, 6, :].rearrange("p (k d) -> p k d", k=2)
                  B_xe, B_hm = Buf("xe"), Buf("hm")
                  PTgs = [(PTg, Buf("PT0")), (sbm([128, 2, S], BF16), Buf("PT1"))]
                  yebs = [(yeb, Buf("ye0")), (sbm([128, 2, D], BF16), Buf("ye1"))]
                  Pbufs = [(Pm, Buf("P0")), (sbm([128, 16, 256], BF16), Buf("P1"))]

                  def build_P(e_):
                      Pm_, B_P_ = Pbufs[e_ % 2]
                      for tc in range(16):
                          P.emit(dve, lambda: nc.vector.tensor_scalar(
                              out=Pm_[:, tc, :], in0=iota1[:, :], scalar1=posmT[:, tc * 16 + e_:tc * 16 + e_ + 1],
                              scalar2=None, op0=ALU.is_equal), reads=[B_posmT, B_c2], writes=[B_P_])

                  we_ring = Ring([(sbm([128, KC, D], BF16), Buf("we%d" % i), P.dsem("we%d" % i)) for i in range(3)])
                  s_ring = Ring([(sbm([128, 256], F32), Buf("s%d" % i)) for i in range(2)])
                  gc_ring = Ring([(sbm([128, 2], F32), Buf("gc%d" % i)) for i in range(2)])
                  for e in range(NE):
                      ws = []
                      for wd_ in (weg_d, weu_d, wed_d):
                          wt, bw, dsw = we_ring.next()
                          P.dma(pool, dsw, [wt[:, 0:4, :], wt[:, 4:8, :]],
                                [wd_[l, e, 0:512, :].rearrange("(k p) c -> p k c", p=128),
                                 wd_[l, e, 512:1024, :].rearrange("(k p) c -> p k c", p=128)], writes=[bw])
                          ws.append((wt, bw))
                      (wg, bwg), (wu, bwu), (wdn, bwd) = ws
                      Pm, B_P = Pbufs[e % 2]
                      PTg, B_PT = PTgs[e % 2]
                      yeb, B_ye = yebs[e % 2]
                      if e == 0:
                          build_P(0)
                      for k in range(2):
                          for tt in range(4):
                              ps = next_ps()
                              for j in range(4):
                                  tc = tt * 4 + j
                                  mm(ps, ps[0][:, j * 128:(j + 1) * 128], Pm[:, tc, k * 128:(k + 1) * 128], identB[:],
                                     [B_P, B_c2], last=(j == 3), start=True, stop=True)
                              copy_ps(PTg[:, k, tt * 512:(tt + 1) * 512], ps[0][:, :], [ps[1]], [B_PT])
                      psg = next_ps()
                      for k in range(2):
                          for tc in range(16):
                              mm(psg, psg[0][:, 2 * k:2 * k + 2], Pm[:, tc, k * 128:(k + 1) * 128],
                                 gmHL[:, :, tc * 16 + e], [B_P, B_gmHL], last=(k == 1 and tc == 15), start=(tc == 0), stop=(tc == 15))
                      gc, bgc = gc_ring.next()
                      P.emit(dve, lambda: nc.vector.reduce_sum(
                          out=gc[:, :], in_=psg[0][:, 0:4].rearrange("p (k h) -> p k h", h=2), axis=AX.X),
                          reads=[psg[1]], writes=[bgc])
                      for half in range(4):
                          ps = next_ps()
                          for j in range(2):
                              c = half * 2 + j
                              for tc in range(16):
                                  mm(ps, ps[0][:, j * 256:(j + 1) * 256], hn2[:, tc, c * 128:(c + 1) * 128], Pm[:, tc, :],
                                     [B_hn2, B_P], last=(j == 1 and tc == 15), start=(tc == 0), stop=(tc == 15))
                          copy_ps(xeT[:, half * 2:half * 2 + 2, :], ps[0][:, :].rearrange("p (a c) -> p a c", c=256),
                                  [ps[1]], [B_xe], act)
                      if e + 1 < NE:
                          build_P(e + 1)
                      for f in range(8):
                          ps = next_ps()
                          for c in range(KC):
                              mm(ps, ps[0][:, 0:256], wg[:, c, f * 128:(f + 1) * 128], xeT[:, c, :], [bwg, B_xe],
                                 start=(c == 0), stop=(c == KC - 1))
                          for c in range(KC):
                              mm(ps, ps[0][:, 256:512], wu[:, c, f * 128:(f + 1) * 128], xeT[:, c, :], [bwu, B_xe],
                                 last=(c == KC - 1), start=(c == 0))
                          st, bst = s_ring.next()
                          P.emit(act, lambda: nc.scalar.activation(out=st[:], in_=ps[0][:, 0:256], func=AF.Silu),
                                 reads=[ps[1]], writes=[bst])
                          P.emit(dve, lambda: nc.vector.tensor_tensor(out=hm[:, f, :], in0=ps[0][:, 256:512], in1=st[:],
                                                                      op=ALU.mult),
                                 reads=[ps[1], bst], writes=[B_hm])
                      for k in range(2):
                          for dh in range(2):
                              ps = next_ps()
                              for f in range(8):
                                  mm(ps, ps[0][:, :], hm[:, f, k * 128:(k + 1) * 128], wdn[:, f, dh * 512:(dh + 1) * 512],
                                     [bwd, B_hm], last=(f == 7))
                              P.emit(act, lambda: nc.scalar.activation(out=yeb[:, k, dh * 512:(dh + 1) * 512],
                                                                       in_=ps[0][:, :], func=AF.Copy,
                                                                       scale=gc[:, k:k + 1]),
                                     reads=[ps[1], bgc], writes=[B_ye])
                      if e % 2 == 1:
                          for jo in range(8):
                              for tt in range(4):
                                  tsl = slice(tt * 512, (tt + 1) * 512)
                                  ps = next_ps()
                                  for q_ in range(2):
                                      ptq, bptq = PTgs[q_]
                                      yeq, byeq = yebs[q_]
                                      for k in range(2):
                                          mm(ps, ps[0][:, :], yeq[:, k, jo * 128:(jo + 1) * 128], ptq[:, k, tsl],
                                             [byeq, bptq], last=(q_ == 1 and k == 1))
                                  P.emit(dve, lambda: nc.vector.tensor_tensor(out=hT[:, jo, tsl], in0=ps[0][:, :],
                                                                              in1=hT[:, jo, tsl], op=ALU.add),
                                         reads=[ps[1], B_hTt[(jo, tt)]], writes=[B_hTt[(jo, tt)]])
                  P.barrier()

          with ExitStack() as ph:
              def sbf(shape, dt):
                  return ph.enter_context(nc.sbuf_tensor(uname(), list(shape), dt))

              sq_ring = Ring([(sbf([128, 512], BF16), Buf("sq%d" % i)) for i in range(2)])
              rs_ring = Ring([(sbf([128, 512], F32), Buf("rs%d" % i)) for i in range(2)])
              stage_ring = Ring([(sbf([128, 512], F32), Buf("stg%d" % i), P.dsem("out%d" % i)) for i in range(3)])
              rmsnorm(V_FIN, sq_ring, rs_ring, final_seq=s, stage_ring=stage_ring)
              P.barrier()

    except _Stop:
        P.barrier()
        for c in range(KC):
            P.dma(sp, ds_out, outT[0, c * 128:(c + 1) * 128, :], hT[:, c, :], reads=B_hT_all)

    for d in P.dsems:
        if d.val and sp.waited.get(d.key, 0) < d.val:
            sp.h.wait_ge(d.sem, d.val)
    nc._keep_es = es if False else None; globals().setdefault("_KEEP", []).append(es)
    return nc


def _tables():
    kk = np.arange(128)[:, None]
    qq = np.arange(128)[None, :]
    tabs = np.zeros((128, 2, 3, 128), np.float32)
    for ti in range(3):
        rel = np.abs(kk + (ti - 1) * 128 - qq).astype(np.float32)
        tabs[:, 0, ti, :] = np.where(rel <= 128, rel, 1e6)
        tabs[:, 1, ti, :] = np.where(rel <= 64, rel, 1e6)
    cst2 = np.zeros((128, 384), np.float32)
    cst2[:, 0:128] = np.eye(128, dtype=np.float32)
    cst2[:, 128:384] = np.arange(1, 257, dtype=np.float32)[None, :]
    return tabs.reshape(128, -1), cst2


def prep_weights(norm_mix, w_in, w_branch_a, w_branch_b, b_gate, sink_logit, w_out, norm_ffn, w_router,
                 w_expert_gate, w_expert_up, w_expert_down, norm_final, L=DEPTH):
    f = np.float32
    w_in = np.asarray(w_in, f)
    wa = np.empty((L, 6, D, 384), f)
    for jp in range(2):
        for g in range(3):
            c0 = g * 256 + jp * 128
            u = jp * 3 + g
            wa[:, u, :, 0:128] = w_in[:L, :, c0:c0 + 128]
            wa[:, u, :, 128:256] = w_in[:L, :, 768 + c0:768 + c0 + 128]
            wa[:, u, :, 256:384] = w_in[:L, :, 1536 + c0:1536 + c0 + 128]
    q0, k0, v0, g0 = 2304, 3328, 3584, 3840
    wqw = np.ascontiguousarray(w_in[:L, :, q0:q0 + 1024].reshape(L, D, 8, 128).transpose(0, 2, 1, 3))
    wkvw = np.empty((L, 4, D, 192), f)
    for g in range(4):
        wkvw[:, g, :, 0:64] = w_in[:L, :, k0 + g * 64:k0 + (g + 1) * 64]
        wkvw[:, g, :, 64:128] = w_in[:L, :, k0 + g * 64:k0 + (g + 1) * 64]
        wkvw[:, g, :, 128:192] = w_in[:L, :, v0 + g * 64:v0 + (g + 1) * 64]
    wtail = np.empty((L, 8, D, 256), f)
    for j in range(8):
        wtail[:, j, :, 0:128] = w_in[:L, :, g0 + j * 128:g0 + (j + 1) * 128]
        wtail[:, j, :, 128:256] = w_in[:L, :, g0 + 1024 + j * 128:g0 + 1024 + (j + 1) * 128]
    wba = np.ascontiguousarray(np.asarray(w_branch_a, f)[:L].reshape(L, 256, 8, 128).transpose(0, 2, 1, 3))
    wbb = np.ascontiguousarray(np.asarray(w_branch_b, f)[:L].reshape(L, D, 8, 128).transpose(0, 2, 1, 3))

    def pm(v):
        v = np.asarray(v, f)
        return v.reshape(v.shape[:-1] + (8, 128))

    NV = L * 40 + 8
    vecs = np.zeros((128, NV), f)
    vecs[:, 0:L * 8] = pm(norm_mix)[:L].transpose(2, 0, 1).reshape(128, L * 8)
    vecs[:, L * 8:L * 16] = pm(norm_ffn)[:L].transpose(2, 0, 1).reshape(128, L * 8)
    bg = np.asarray(b_gate, f)[:L].reshape(L, 2, 8, 128)
    vecs[:, L * 16:L * 32] = bg.transpose(3, 0, 1, 2).reshape(128, L * 16)
    vecs[:, L * 32:L * 32 + 8] = pm(norm_final).transpose(1, 0)
    sk = np.asarray(sink_logit, f)[:L].reshape(L, 8, 2)
    sp = np.repeat(sk.transpose(2, 0, 1)[:, None], 64, axis=1).reshape(128, L * 8)
    vecs[:, L * 32 + 8:] = sp
    tabs, cst2 = _tables()
    return {
        "wa": wa, "wqw": wqw, "wkvw": wkvw, "wtail": wtail, "wba": wba, "wbb": wbb,
        "wout": np.ascontiguousarray(np.asarray(w_out, f)[:L]),
        "wr": np.ascontiguousarray(np.asarray(w_router, f)[:L]),
        "weg": np.ascontiguousarray(np.asarray(w_expert_gate, f)[:L]),
        "weu": np.ascontiguousarray(np.asarray(w_expert_up, f)[:L]),
        "wed": np.ascontiguousarray(np.asarray(w_expert_down, f)[:L]),
        "vecs": vecs, "tabs": tabs, "cst2": cst2,
    }


def kernel(x, norm_mix, w_in, w_branch_a, w_branch_b, b_gate, sink_logit, w_out, norm_ffn, w_router,
           w_expert_gate, w_expert_up, w_expert_down, norm_final):
    x = np.asarray(x, np.float32)
    wts = prep_weights(norm_mix, w_in, w_branch_a, w_branch_b, b_gate, sink_logit, w_out, norm_ffn, w_router,
                       w_expert_gate, w_expert_up, w_expert_down, norm_final)
    nc = build_program(DEPTH, NSEQ)
    in_maps = []
    for c in range(NCORES):
        m = dict(wts)
        m["xT"] = np.ascontiguousarray(x[c * NSEQ:(c + 1) * NSEQ].transpose(0, 2, 1))
        in_maps.append(m)
    res = run_bass_kernel_spmd(nc, in_maps, core_ids=list(range(NCORES)))
    out = np.empty((NCORES * NSEQ, S, D), np.float32)
    for c in range(NCORES):
        out[c * NSEQ:(c + 1) * NSEQ] = res.results[c]["outT"].transpose(0, 2, 1)
    return out
```

```python
import numpy as np
from contextlib import ExitStack
import concourse.bass as bass
import concourse.mybir as mybir
from concourse.bass_utils import run_bass_kernel_spmd

F32 = mybir.dt.float32
BF16 = mybir.dt.bfloat16
AF = mybir.ActivationFunctionType
ALU = mybir.AluOpType
AX = mybir.AxisListType

D = 1024
S = 2048
DEPTH = 4
NCORES = 8
NSEQ = 4
KC = 8
NE = 16
CAP = 256
EPS = 1e-6
DIL_R = (1, 4, 16)


def _slopes(n):
    return [float(2.0 ** (-8.0 * i / n)) for i in range(1, n + 1)]


SL_DIL = _slopes(12)
SL_WIN = _slopes(16)


class Ev:
    __slots__ = ("sem", "val", "key")

    def __init__(self, sem, val, key):
        self.sem, self.val, self.key = sem, val, key


class Buf:
    __slots__ = ("name", "w", "r", "excl")

    def __init__(self, name, excl=False):
        self.name = name
        self.w = None
        self.r = {}
        self.excl = excl


class Eng:
    def __init__(self, name, h, sem, is_pe=False):
        self.name, self.h, self.sem, self.is_pe = name, h, sem, is_pe
        self.cnt = 0
        self.waited = {}


class DSem:
    def __init__(self, sem, key):
        self.sem, self.key, self.val = sem, key, 0


class Prog:
    def __init__(self, nc, es):
        self.nc = nc
        self.es = es
        mk = lambda n: es.enter_context(nc.semaphore(n))
        self.pe = Eng("pe", nc.tensor, mk("s_pe"), True)
        self.act = Eng("act", nc.scalar, mk("s_act"))
        self.dve = Eng("dve", nc.vector, mk("s_dve"))
        self.pool = Eng("pool", nc.gpsimd, mk("s_pool"))
        self.sp = Eng("sp", nc.sync, mk("s_sp"))
        self.engs = [self.pe, self.act, self.dve, self.pool, self.sp]
        self.dsems = []
        self.dtags = {}
        self.nds = 0

    def dsem(self, tag=None):
        if tag is not None and tag in self.dtags:
            return self.dtags[tag]
        self.nds += 1
        d = DSem(self.es.enter_context(self.nc.semaphore("s_dma%d" % self.nds)), "dma%d" % self.nds)
        self.dsems.append(d)
        if tag is not None:
            self.dtags[tag] = d
        return d

    def need(self, eng, ev, raw):
        if ev is None:
            return
        if ev.key == eng.name and eng.is_pe:
            return
        if eng.waited.get(ev.key, 0) >= ev.val:
            return
        eng.h.wait_ge(ev.sem, ev.val)
        eng.waited[ev.key] = ev.val

    def deps(self, eng, reads, writes):
        for b in reads:
            self.need(eng, b.w, True)
            if b.excl:
                for ev in b.r.values():
                    if ev.key != eng.name:
                        self.need(eng, ev, False)
        for b in writes:
            self.need(eng, b.w, False)
            for ev in b.r.values():
                self.need(eng, ev, False)

    def mark(self, ev, reads, writes):
        for b in writes:
            b.w = ev
            b.r = {}
        for b in reads:
            o = b.r.get(ev.key)
            if o is None or o.val < ev.val:
                b.r[ev.key] = ev

    def emit(self, eng, fn, reads=(), writes=(), signal=True):
        self.deps(eng, reads, writes)
        ins = fn()
        if signal:
            eng.cnt += 1
            ins.then_inc(eng.sem, 1)
            ev = Ev(eng.sem, eng.cnt, eng.name)
        else:
            ev = Ev(eng.sem, eng.cnt + 1, eng.name)
        self.mark(ev, reads, writes)
        return ins

    def dma(self, q, ds, out_ap, in_ap, reads=(), writes=()):
        self.deps(q, reads, writes)
        if not isinstance(out_ap, list):
            out_ap, in_ap = [out_ap], [in_ap]
        for o, i in zip(out_ap, in_ap):
            ds.val += 16
            q.h.dma_start(out=o, in_=i).then_inc(ds.sem, 16)
        ev = Ev(ds.sem, ds.val, ds.key)
        self.mark(ev, reads, writes)

    def barrier(self):
        for e in self.engs:
            for f in self.engs:
                if f.cnt == 0 or (f is e and e.is_pe) or (f is e and e.name in ("sp", "pool")):
                    continue
                if e.waited.get(f.name, 0) < f.cnt:
                    e.h.wait_ge(f.sem, f.cnt)
                    e.waited[f.name] = f.cnt
            for d in self.dsems:
                if d.val and e.waited.get(d.key, 0) < d.val:
                    e.h.wait_ge(d.sem, d.val)
                    e.waited[d.key] = d.val


class _Stop(Exception):
    pass


class Ring:
    def __init__(self, items):
        self.items = items
        self.i = -1

    def next(self):
        self.i = (self.i + 1) % len(self.items)
        return self.items[self.i]


def build_program(L=DEPTH, NS=NSEQ, stop_after=None):
    nc = bass.Bass("TRN2", target_bir_lowering=False)
    es = ExitStack()
    P = Prog(nc, es)
    pe, act, dve, pool, sp = P.pe, P.act, P.dve, P.pool, P.sp

    def din(name, shape):
        return nc.dram_tensor(name, list(shape), F32, kind="ExternalInput").ap()

    NV = L * 8 + L * 8 + L * 16 + 8 + L * 8
    V_MIX, V_FFN, V_BG, V_FIN, V_SINK = 0, L * 8, L * 16, L * 32, L * 32 + 8
    xT = din("xT", [NS, D, S])
    wa_d = din("wa", [L, 6, D, 384])
    wqw_d = din("wqw", [L, 8, D, 128])
    wkvw_d = din("wkvw", [L, 4, D, 192])
    wtail_d = din("wtail", [L, 8, D, 256])
    wba_d = din("wba", [L, 8, 256, 128])
    wbb_d = din("wbb", [L, 8, D, 128])
    wout_d = din("wout", [L, D, D])
    wr_d = din("wr", [L, D, NE])
    weg_d = din("weg", [L, NE, D, D])
    weu_d = din("weu", [L, NE, D, D])
    wed_d = din("wed", [L, NE, D, D])
    vecs_d = din("vecs", [128, NV])
    tabs_d = din("tabs", [128, 2 * 3 * 128])
    cst2_d = din("cst2", [128, 384])
    outT = nc.dram_tensor("outT", [NS, D, S], F32, kind="ExternalOutput").ap()

    def sb(name, shape, dt):
        return es.enter_context(nc.sbuf_tensor(name, list(shape), dt))

    uctr = [0]

    def uname():
        uctr[0] += 1
        return "t%d" % uctr[0]

    hT = sb("hT", [128, KC, S], F32)
    hnT = sb("hnT", [128, KC, S], BF16)
    vec = sb("vec", [128, NV], F32)
    esink = sb("esink", [128, L * 8], F32)
    ones16 = sb("ones16", [NE, NE], F32)
    onesF = sb("onesF", [128, 128], BF16)
    onesA0 = sb("onesA0", [128, 128], BF16)
    ones0B = sb("ones0B", [128, 128], BF16)
    wr_sb = sb("wr_sb", [128, L, KC, NE], BF16)
    epsb = sb("epsb", [128, 1], F32)
    B_hnT, B_const = Buf("hnT"), Buf("const")
    B_hTt = {(c_, t_): Buf("hT%d_%d" % (c_, t_)) for c_ in range(KC) for t_ in range(4)}
    B_hT_all = list(B_hTt.values())
    B_wr = Buf("wr")
    identF = sb("identF", [128, 128], F32)
    iota1 = sb("iota1", [128, 256], F32)
    identB = sb("identB", [128, 128], BF16)
    B_c2 = Buf("c2")

    PSB = []
    for i in range(8):
        t = es.enter_context(nc.psum_tensor("ps%d" % i, [128, 512], F32))
        PSB.append((t, Buf("ps%d" % i, excl=True)))
    ps_ring = Ring(PSB)
    fresh = {}

    def next_ps():
        t, b = ps_ring.next()
        fresh[b.name] = True
        return t, b

    def mm(psb, out_ap, lhsT, rhs, reads, last=False, sig=False, start=None, stop=None):
        b = psb[1]
        st = fresh[b.name] if start is None else start
        fresh[b.name] = False
        sp_ = last if stop is None else (stop or last)
        P.emit(pe, lambda: nc.tensor.matmul(out_ap, lhsT, rhs, start=st, stop=sp_),
               reads=reads, writes=[b], signal=(last or sig))

    ds_c = P.dsem()
    P.dma(sp, ds_c, [vec[:, :]], [vecs_d[:, :]], writes=[B_const])
    ds_c2 = P.dsem()
    P.dma(sp, ds_c2, [identF[:, :], iota1[:, :]], [cst2_d[:, 0:128], cst2_d[:, 128:384]], writes=[B_c2])
    ds_c3 = P.dsem()
    P.dma(pool, ds_c3, identB[:, :], cst2_d[:, 0:128], writes=[B_c2])
    ds_wr = P.dsem()
    P.dma(pool, ds_wr, [wr_sb[:, l, :, :] for l in range(L)],
          [wr_d[l].rearrange("(k p) e -> p k e", p=128) for l in range(L)], writes=[B_wr])
    B_ones = Buf("ones")
    P.emit(dve, lambda: nc.vector.memset(onesF[:], 1.0), writes=[B_ones])
    P.emit(dve, lambda: nc.vector.memset(onesA0[:], 0.0), writes=[B_ones])
    P.emit(dve, lambda: nc.vector.memset(ones0B[:], 0.0), writes=[B_ones])
    P.emit(dve, lambda: nc.vector.memset(onesA0[:, 0:64], 1.0), writes=[B_ones])
    P.emit(dve, lambda: nc.vector.memset(ones0B[:, 64:128], 1.0), writes=[B_ones])
    P.emit(dve, lambda: nc.vector.memset(ones16[:], 1.0), writes=[B_ones])
    P.emit(dve, lambda: nc.vector.memset(epsb[:], EPS), writes=[B_ones])
    B_esink = Buf("esink")
    P.emit(act, lambda: nc.scalar.activation(out=esink[:], in_=vec[:, V_SINK:V_SINK + L * 8], func=AF.Exp),
           reads=[B_const], writes=[B_esink])

    ds_x = P.dsem("x")
    ds_out = P.dsem("dbg")

    def rmsnorm(gcol, sq_ring, rs_ring, final_seq=None, stage_ring=None):
        for tt in range(4):
            tsl = slice(tt * 512, (tt + 1) * 512)
            ps = next_ps()
            for c in range(KC):
                sq, bsq = sq_ring.next()
                P.emit(act, lambda: nc.scalar.activation(out=sq[:], in_=hT[:, c, tsl], func=AF.Square),
                       reads=[B_hTt[(c, tt)]], writes=[bsq])
                mm(ps, ps[0][:, :], onesF[:], sq[:], [bsq, B_ones], last=(c == KC - 1), sig=True)
            rs, brs = rs_ring.next()
            P.emit(act, lambda: nc.scalar.activation(out=rs[:], in_=ps[0][:, :], func=AF.Sqrt,
                                                     scale=1.0 / D, bias=epsb[:, 0:1]),
                   reads=[ps[1], B_ones], writes=[brs])
            P.emit(dve, lambda: nc.vector.reciprocal(rs[:], rs[:]), reads=[brs], writes=[brs])
            for c in range(KC):
                if final_seq is None:
                    P.emit(dve, lambda: nc.vector.scalar_tensor_tensor(
                        out=hnT[:, c, tsl], in0=hT[:, c, tsl], scalar=vec[:, gcol + c:gcol + c + 1],
                        in1=rs[:], op0=ALU.mult, op1=ALU.mult),
                        reads=[B_hTt[(c, tt)], brs, B_const], writes=[B_hnT])
                else:
                    st, bst, dso = stage_ring.next()
                    P.emit(dve, lambda: nc.vector.scalar_tensor_tensor(
                        out=st[:], in0=hT[:, c, tsl], scalar=vec[:, gcol + c:gcol + c + 1],
                        in1=rs[:], op0=ALU.mult, op1=ALU.mult),
                        reads=[B_hTt[(c, tt)], brs, B_const], writes=[bst])
                    P.dma(sp, dso, outT[final_seq, c * 128:(c + 1) * 128, tsl], st[:], reads=[bst])

    def perm512(c, ut, r):
        if r == 1:
            return hnT[:, c, ut * 512:(ut + 1) * 512]
        if r == 4:
            return hnT[:, c, ut:S:4]
        return hnT[:, c, :].rearrange("p (a q) -> p q a", q=16)[:, 4 * ut:4 * ut + 4, :]

    def perm128(c, uc, r):
        if r == 1:
            return hnT[:, c, uc * 128:(uc + 1) * 128]
        if r == 4:
            sub, a0 = uc // 4, (uc % 4) * 128
            return hnT[:, c, sub + 4 * a0:sub + 4 * (a0 + 127) + 1:4]
        return hnT[:, c, uc:S:16]

    def nat128(qt, r):
        if r == 1:
            return slice(qt * 128, (qt + 1) * 128)
        if r == 4:
            sub, a0 = qt // 4, (qt % 4) * 128
            return slice(sub + 4 * a0, sub + 4 * (a0 + 127) + 1, 4)
        return slice(qt, S, 16)

    def ps_view(ps, r):
        if r == 16:
            return ps[0][:, :].rearrange("p (q a) -> p q a", q=4)
        return ps[0][:, :]

    cp_flip = [0]

    def copy_ps(out_ap, in_ap, reads, writes, eng=None):
        if eng is None:
            cp_flip[0] ^= 1
            eng = act if cp_flip[0] else dve
        if eng is act:
            P.emit(act, lambda: nc.scalar.copy(out=out_ap, in_=in_ap), reads=reads, writes=writes)
        else:
            P.emit(dve, lambda: nc.vector.tensor_copy(out=out_ap, in_=in_ap), reads=reads, writes=writes)

    def ck(name):
        if stop_after == name:
            raise _Stop()

    try:
      for s in range(NS):
          P.dma(sp, ds_x, [hT[:, c, :] for c in range(KC)], [xT[s, c * 128:(c + 1) * 128, :] for c in range(KC)],
                writes=B_hT_all)
          for l in range(L):
              with ExitStack() as ph:
                  def sbp(name, shape, dt):
                      return ph.enter_context(nc.sbuf_tensor(uname(), list(shape), dt))

                  OT = sbp("OT", [128, 10, S], BF16)
                  B_OT = [Buf("OT%d" % i) for i in range(10)]
                  ck("load")
                  with ExitStack() as ph0:
                      sq_ring = Ring([(ph0.enter_context(nc.sbuf_tensor(uname(), [128, 512], BF16)), Buf("sq%d" % i))
                                      for i in range(2)])
                      rs_ring = Ring([(ph0.enter_context(nc.sbuf_tensor(uname(), [128, 512], F32)), Buf("rs%d" % i))
                                      for i in range(2)])
                      rmsnorm(V_MIX + l * 8, sq_ring, rs_ring)
                      P.barrier()
                  ck("norm")

                  with ExitStack() as ph2:
                      def sb2(shape, dt):
                          return ph2.enter_context(nc.sbuf_tensor(uname(), list(shape), dt))

                      QA0, Q0B, K2 = sb2([128, S], BF16), sb2([128, S], BF16), sb2([128, S], BF16)
                      VA0, V0B = sb2([128, 16, 128], BF16), sb2([128, 16, 128], BF16)
                      tab = sb2([128, 2, 3, 128], F32)
                      B_tab = Buf("tab")
                      P.dma(sp, P.dsem("tab"), tab[:].rearrange("p a b c -> p (a b c)"), tabs_d[:, :], writes=[B_tab])
                      acc = sb2([128, 2, S], F32)
                      B_QA, B_QB, B_K, B_VA, B_VB, B_acc = (Buf(n) for n in ("QA", "QB", "K", "VA", "VB", "acc"))
                      wa_ring = Ring([(sb2([128, KC, 384], BF16), Buf("wa%d" % i), P.dsem("wa%d" % i)) for i in range(1)])
                      wq_ring = Ring([(sb2([128, KC, 128], BF16), Buf("wq%d" % i), P.dsem("wq%d" % i)) for i in range(1)])
                      wkv_ring = Ring([(sb2([128, KC, 192], BF16), Buf("wkv%d" % i), P.dsem("wkv%d" % i)) for i in range(1)])
                      tS_ring = Ring([(sb2([128, 384], F32), Buf("tS%d" % i)) for i in range(2)])
                      PT_ring = Ring([(sb2([128, 384], BF16), Buf("PT%d" % i)) for i in range(4)])
                      dn_ring = Ring([(sb2([128, 128], F32), Buf("dn%d" % i)) for i in range(2)])
                      P.emit(dve, lambda: nc.vector.memset(QA0[64:128, :], 0.0), writes=[B_QA])
                      P.emit(dve, lambda: nc.vector.memset(Q0B[0:64, :], 0.0), writes=[B_QB])
                      P.emit(dve, lambda: nc.vector.memset(VA0[:, :, 64:128], 0.0), writes=[B_VA])
                      P.emit(dve, lambda: nc.vector.memset(V0B[:, :, 0:64], 0.0), writes=[B_VB])
                      ck("ph2alloc")

                      def perm_views(dst, src_ps, ut, r):
                          if r == 1:
                              return dst[:, ut * 512:(ut + 1) * 512], src_ps
                          n = 512 // r
                          o = dst.rearrange("p (c a) -> p c a", c=r)[:, :, ut * n:(ut + 1) * n]
                          i = src_ps.rearrange("p (a c) -> p c a", c=r)
                          return o, i

                      def proj_q(wt, bw, col0, r):
                          for ut in range(4):
                              ps = next_ps()
                              for c in range(KC):
                                  mm(ps, ps[0][:, :], wt[:, c, col0:col0 + 128], hnT[:, c, ut * 512:(ut + 1) * 512],
                                     [bw, B_hnT], last=(c == KC - 1))
                              o, i = perm_views(QA0[0:64, :], ps[0][0:64, :], ut, r)
                              copy_ps(o, i, [ps[1]], [B_QA], act)
                              o, i = perm_views(Q0B[64:128, :], ps[0][64:128, :], ut, r)
                              copy_ps(o, i, [ps[1]], [B_QB], dve)

                      def proj_k(wt, bw, col0, r):
                          for ut in range(4):
                              ps = next_ps()
                              for c in range(KC):
                                  mm(ps, ps[0][:, :], wt[:, c, col0:col0 + 128], hnT[:, c, ut * 512:(ut + 1) * 512],
                                     [bw, B_hnT], last=(c == KC - 1))
                              o, i = perm_views(K2[:, :], ps[0][:, :], ut, r)
                              copy_ps(o, i, [ps[1]], [B_K])

                      def attention(r, tabi, scA, scB, epilogue):
                          tps = (S // r) // 128

                          def stage1(qt):
                              jj = qt % tps
                              chunks = []
                              if jj > 0:
                                  chunks.append((qt - 1, 0))
                              chunks.append((qt, 1))
                              if jj < tps - 1:
                                  chunks.append((qt + 1, 2))
                              lo, hi = chunks[0][1] * 128, (chunks[-1][1] + 1) * 128
                              qsl = slice(qt * 128, (qt + 1) * 128)
                              pts = []
                              for (QX, BQ, sc) in ((QA0, B_QA, scA), (Q0B, B_QB, scB)):
                                  ps = next_ps()
                                  for i, (kc, ti) in enumerate(chunks):
                                      mm(ps, ps[0][:, ti * 128:(ti + 1) * 128], K2[:, kc * 128:(kc + 1) * 128],
                                         QX[:, qsl], [B_K, BQ], last=(i == len(chunks) - 1))
                                  tS, btS = tS_ring.next()
                                  P.emit(dve, lambda: nc.vector.scalar_tensor_tensor(
                                      out=tS[:, lo:hi], in0=tab[:, tabi].rearrange("p a b -> p (a b)")[:, lo:hi],
                                      scalar=sc, in1=ps[0][:, lo:hi], op0=ALU.mult, op1=ALU.add),
                                      reads=[ps[1], B_tab], writes=[btS])
                                  PT, bPT = PT_ring.next()
                                  P.emit(act, lambda: nc.scalar.activation(out=PT[:, lo:hi], in_=tS[:, lo:hi],
                                                                           func=AF.Exp, scale=0.125),
                                         reads=[btS], writes=[bPT])
                                  pts.append((PT, bPT))
                              return chunks, pts

                          def stage2(qt, chunks, pts):
                              pso = next_ps()
                              n = 0
                              tot = 4 * len(chunks)
                              for (PT, bPT), VX, BV, oX in ((pts[0], VA0, B_VA, onesA0), (pts[1], V0B, B_VB, ones0B)):
                                  for (kc, ti) in chunks:
                                      n += 1
                                      mm(pso, pso[0][:, 0:128], VX[:, kc, :], PT[:, ti * 128:(ti + 1) * 128],
                                         [BV, bPT], last=False)
                                      n += 1
                                      mm(pso, pso[0][:, 128:256], oX[:], PT[:, ti * 128:(ti + 1) * 128],
                                         [B_ones, bPT], last=(n == tot))
                              epilogue(pso, qt)

                          cur = stage1(0)
                          for qt in range(16):
                              nxt = stage1(qt + 1) if qt + 1 < 16 else None
                              stage2(qt, *cur)
                              cur = nxt

                      for jp in range(2):
                          for g in range(3):
                              r = DIL_R[g]
                              wt, bw, dsw = wa_ring.next()
                              P.dma(pool, dsw, wt[:], wa_d[l, jp * 3 + g].rearrange("(k p) c -> p k c", p=128),
                                    writes=[bw])
                              ck("wadma")
                              proj_q(wt, bw, 0, r)
                              ck("projq")
                              proj_k(wt, bw, 128, r)
                              ck("proj%d" % g)
                              for u0 in range(0, 16, 4):
                                  ps = next_ps()
                                  for uu in range(4):
                                      for c in range(KC):
                                          mm(ps, ps[0][:, uu * 128:(uu + 1) * 128], perm128(c, u0 + uu, r),
                                             wt[:, c, 256:384], [bw, B_hnT], last=(uu == 3 and c == KC - 1),
                                             start=(c == 0), stop=(c == KC - 1))
                                  ck("vmm")
                                  pv = ps[0][:, :].rearrange("p (n c) -> p n c", c=128)
                                  copy_ps(VA0[:, u0:u0 + 4, 0:64], pv[:, :, 0:64], [ps[1]], [B_VA], act)
                                  ck("vcpa")
                                  copy_ps(V0B[:, u0:u0 + 4, 64:128], pv[:, :, 64:128], [ps[1]], [B_VB], dve)
                              sA = -8.0 * SL_DIL[g * 4 + 2 * jp] * r
                              sB = -8.0 * SL_DIL[g * 4 + 2 * jp + 1] * r

                              def epi_dil(pso, qt, g=g, r=r):
                                  nat = nat128(qt, r)
                                  pv = pso[0][:, 0:256].rearrange("p (t q) -> p t q", t=2)
                                  if g == 0:
                                      copy_ps(acc[:, :, nat], pv, [pso[1]], [B_acc], act)
                                  else:
                                      P.emit(dve, lambda: nc.vector.tensor_tensor(out=acc[:, :, nat], in0=pv,
                                                                                  in1=acc[:, :, nat], op=ALU.add),
                                             reads=[pso[1], B_acc], writes=[B_acc])

                              ck("vproj%d" % g)
                              attention(r, 1, sA, sB, epi_dil)
                              ck("att%d" % g)
                          P.emit(dve, lambda: nc.vector.reciprocal(acc[:, 1, :], acc[:, 1, :]), reads=[B_acc],
                                 writes=[B_acc])
                          P.emit(dve, lambda: nc.vector.tensor_tensor(out=OT[:, jp, :], in0=acc[:, 0, :],
                                                                      in1=acc[:, 1, :], op=ALU.mult),
                                 reads=[B_acc], writes=[B_OT[jp]])

                      for m in range(8):
                          g = m // 2
                          if m % 2 == 0:
                              wkv, bkv, dskv = wkv_ring.next()
                              P.dma(pool, dskv, wkv[:], wkvw_d[l, g].rearrange("(k p) c -> p k c", p=128),
                                    writes=[bkv])
                              proj_k(wkv, bkv, 0, 1)
                              for u0 in range(0, 16, 8):
                                  ps = next_ps()
                                  for uu in range(8):
                                      for c in range(KC):
                                          mm(ps, ps[0][:, uu * 64:(uu + 1) * 64], perm128(c, u0 + uu, 1),
                                             wkv[:, c, 128:192], [bkv, B_hnT], last=(uu == 7 and c == KC - 1),
                                             start=(c == 0), stop=(c == KC - 1))
                                  pv = ps[0][:, :].rearrange("p (n c) -> p n c", c=64)
                                  copy_ps(VA0[:, u0:u0 + 8, 0:64], pv, [ps[1]], [B_VA], act)
                                  copy_ps(V0B[:, u0:u0 + 8, 64:128], pv, [ps[1]], [B_VB], dve)
                          wq, bq, dsq = wq_ring.next()
                          P.dma(pool, dsq, wq[:], wqw_d[l, m].rearrange("(k p) c -> p k c", p=128), writes=[bq])
                          proj_q(wq, bq, 0, 1)

                          def epi_win(pso, qt, m=m):
                              dn, bdn = dn_ring.next()
                              P.emit(dve, lambda: nc.vector.tensor_scalar(
                                  out=dn[:], in0=pso[0][:, 128:256], scalar1=esink[:, l * 8 + m:l * 8 + m + 1],
                                  scalar2=None, op0=ALU.add), reads=[pso[1], B_esink], writes=[bdn])
                              P.emit(dve, lambda: nc.vector.reciprocal(dn[:], dn[:]), reads=[bdn], writes=[bdn])
                              P.emit(dve, lambda: nc.vector.tensor_tensor(
                                  out=OT[:, 2 + m, qt * 128:(qt + 1) * 128], in0=pso[0][:, 0:128], in1=dn[:],
                                  op=ALU.mult), reads=[pso[1], bdn], writes=[B_OT[2 + m]])

                          attention(1, 0, -8.0 * SL_WIN[2 * m], -8.0 * SL_WIN[2 * m + 1], epi_win)
                          ck("win%d" % m)
                  P.barrier()

                  with ExitStack() as ph3:
                      def sb3(shape, dt):
                          return ph3.enter_context(nc.sbuf_tensor(uname(), list(shape), dt))

                      mgf = sb3([128, KC, S], BF16)
                      B_mg = {(j_, t_): Buf("mg%d_%d" % (j_, t_)) for j_ in range(8) for t_ in range(4)}
                      tw_ring = Ring([(sb3([128, KC, 256], BF16), sb3([128, KC, 128], BF16), sb3([128, 2, 128], BF16),
                                       Buf("tw%d" % i), P.dsem("tw%d" % i)) for i in range(2)])
                      wo_ring = Ring([(sb3([128, KC, 128], BF16), Buf("wo%d" % i), P.dsem("wo%d" % i)) for i in range(2)])
                      g_ring = Ring([(sb3([128, 512], F32), Buf("g%d" % i)) for i in range(4)])
                      for j in range(8):
                          wtj, wbbj, wbaj, btw, dstw = tw_ring.next()
                          P.dma(pool, dstw, [wtj[:], wbbj[:], wbaj[:]],
                                [wtail_d[l, j].rearrange("(k p) c -> p k c", p=128),
                                 wbb_d[l, j].rearrange("(k p) c -> p k c", p=128),
                                 wba_d[l, j].rearrange("(k p) c -> p k c", p=128)], writes=[btw])
                          for tt in range(4):
                              tsl = slice(tt * 512, (tt + 1) * 512)
                              gts = []
                              for b in range(2):
                                  ps = next_ps()
                                  for c in range(KC):
                                      mm(ps, ps[0][:, :], wtj[:, c, b * 128:(b + 1) * 128], hnT[:, c, tsl],
                                         [btw, B_hnT], last=(c == KC - 1))
                                  gt, bgt = g_ring.next()
                                  col = V_BG + l * 16 + b * 8 + j
                                  P.emit(act, lambda: nc.scalar.activation(out=gt[:], in_=ps[0][:, :], func=AF.Sigmoid,
                                                                           bias=vec[:, col:col + 1], scale=1.0),
                                         reads=[ps[1], B_const], writes=[bgt])
                                  gts.append((gt, bgt))
                              psa = next_ps()
                              for p_ in range(2):
                                  mm(psa, psa[0][:, :], wbaj[:, p_, :], OT[:, p_, tsl], [btw, B_OT[p_]], last=(p_ == 1))
                              P.emit(dve, lambda: nc.vector.tensor_tensor(out=gts[0][0][:], in0=psa[0][:, :],
                                                                          in1=gts[0][0][:], op=ALU.mult),
                                     reads=[psa[1], gts[0][1]], writes=[gts[0][1]])
                              psb = next_ps()
                              for mm_ in range(8):
                                  mm(psb, psb[0][:, :], wbbj[:, mm_, :], OT[:, 2 + mm_, tsl], [btw, B_OT[2 + mm_]],
                                     last=(mm_ == 7))
                              P.emit(dve, lambda: nc.vector.tensor_tensor(out=gts[1][0][:], in0=psb[0][:, :],
                                                                          in1=gts[1][0][:], op=ALU.mult),
                                     reads=[psb[1], gts[1][1]], writes=[gts[1][1]])
                              P.emit(dve, lambda: nc.vector.tensor_tensor(out=mgf[:, j, tsl], in0=gts[0][0][:],
                                                                          in1=gts[1][0][:], op=ALU.add),
                                     reads=[gts[0][1], gts[1][1]], writes=[B_mg[(j, tt)]])
                      for jo in range(8):
                          woj, bwo, dswo = wo_ring.next()
                          P.dma(pool, dswo, woj[:],
                                wout_d[l][:, jo * 128:(jo + 1) * 128].rearrange("(k p) c -> p k c", p=128), writes=[bwo])
                          for tt in range(4):
                              tsl = slice(tt * 512, (tt + 1) * 512)
                              ps = next_ps()
                              for c in range(KC):
                                  mm(ps, ps[0][:, :], woj[:, c, :], mgf[:, c, tsl], [bwo, B_mg[(c, tt)]],
                                     last=(c == KC - 1))
                              P.emit(dve, lambda: nc.vector.tensor_tensor(out=hT[:, jo, tsl], in0=ps[0][:, :],
                                                                          in1=hT[:, jo, tsl], op=ALU.add),
                                     reads=[ps[1], B_hTt[(jo, tt)]], writes=[B_hTt[(jo, tt)]])
                  P.barrier()
              ck("mixer")

              with ExitStack() as ph:
                  def sbm(shape, dt):
                      return ph.enter_context(nc.sbuf_tensor(uname(), list(shape), dt))

                  hn2 = sbm([128, 16, D], BF16)
                  posmT = sbm([128, 256], F32)
                  gmHL = sbm([128, 2, 256], BF16)
                  B_hn2, B_posmT, B_gmT, B_gmHL = Buf("hn2"), Buf("posmT"), Buf("gmT"), Buf("gmHL")
                  with ExitStack() as ph1:
                      def sb1(shape, dt):
                          return ph1.enter_context(nc.sbuf_tensor(uname(), list(shape), dt))

                      sq_ring = Ring([(sb1([128, 512], BF16), Buf("sq%d" % i)) for i in range(2)])
                      rs_ring = Ring([(sb1([128, 512], F32), Buf("rs%d" % i)) for i in range(2)])
                      rmsnorm(V_FFN + l * 8, sq_ring, rs_ring)
                      affT = sb1([NE, S], F32)
                      gmT = sb1([128, 256], F32)
                      work = sb1([NE, S], F32)
                      t3 = sb1([NE, S], F32)
                      mx8 = sb1([NE, 8], F32)
                      B_aff, B_work, B_t3, B_mx = Buf("aff"), Buf("work"), Buf("t3"), Buf("mx8")
                      ex_ring = Ring([(sb1([NE, 512], F32), Buf("ex%d" % i)) for i in range(2)])
                      for tt in range(4):
                          tsl = slice(tt * 512, (tt + 1) * 512)
                          ps = next_ps()
                          for c in range(KC):
                              mm(ps, ps[0][0:NE, :], wr_sb[:, l, c, :], hnT[:, c, tsl], [B_wr, B_hnT],
                                 last=(c == KC - 1))
                          ex, bex = ex_ring.next()
                          P.emit(act, lambda: nc.scalar.activation(out=ex[:], in_=ps[0][0:NE, :], func=AF.Exp),
                                 reads=[ps[1]], writes=[bex])
                          ps2 = next_ps()
                          mm(ps2, ps2[0][0:NE, :], ones16[:], ex[:], [bex, B_ones], last=True)
                          P.emit(dve, lambda: nc.vector.reciprocal(affT[:, tsl], ps2[0][0:NE, :]), reads=[ps2[1]],
                                 writes=[B_aff])
                          P.emit(dve, lambda: nc.vector.tensor_tensor(out=affT[:, tsl], in0=ex[:], in1=affT[:, tsl],
                                                                      op=ALU.mult), reads=[bex, B_aff], writes=[B_aff])
                      for tc in range(16):
                          for half in range(2):
                              ps = next_ps()
                              for j in range(4):
                                  c = half * 4 + j
                                  mm(ps, ps[0][:, j * 128:(j + 1) * 128], hnT[:, c, tc * 128:(tc + 1) * 128], identB[:],
                                     [B_hnT, B_c2], last=(j == 3), start=True, stop=True)
                              copy_ps(hn2[:, tc, half * 512:(half + 1) * 512], ps[0][:, :], [ps[1]], [B_hn2], act)
                      src = affT
                      bsrc = B_aff
                      for it in range(CAP // 8):
                          P.emit(dve, lambda: nc.vector.max(out=mx8[:], in_=src[:]), reads=[bsrc], writes=[B_mx])
                          P.emit(dve, lambda: nc.vector.match_replace(out=work[:], in_to_replace=mx8[:],
                                                                      in_values=src[:], imm_value=0.0),
                                 reads=[B_mx, bsrc], writes=[B_work])
                          src, bsrc = work, B_work
                      P.emit(dve, lambda: nc.vector.tensor_tensor(out=work[:], in0=affT[:], in1=work[:],
                                                                  op=ALU.subtract),
                             reads=[B_aff, B_work], writes=[B_work])
                      P.emit(dve, lambda: nc.vector.tensor_single_scalar(out=affT[:], in_=work[:], scalar=0.0,
                                                                         op=ALU.is_gt),
                             reads=[B_work], writes=[B_aff])
                      P.emit(dve, lambda: nc.vector.tensor_tensor_scan(out=t3[:], data0=affT[:], data1=affT[:],
                                                                       initial=0.0, op0=ALU.add, op1=ALU.max),
                             reads=[B_aff], writes=[B_t3])
                      P.emit(dve, lambda: nc.vector.tensor_tensor(out=t3[:], in0=t3[:], in1=affT[:], op=ALU.mult),
                             reads=[B_t3, B_aff], writes=[B_t3])
                      for (srcT, bsrcT, dstT, bdstT) in ((t3, B_t3, posmT, B_posmT), (work, B_work, gmT, B_gmT)):
                          ps = next_ps()
                          for tc in range(16):
                              mm(ps, ps[0][:, tc * 16:(tc + 1) * 16], srcT[:, tc * 128:(tc + 1) * 128],
                                 identF[0:NE, 0:NE], [bsrcT, B_c2], last=(tc == 15), start=True, stop=True)
                          copy_ps(dstT[:, :], ps[0][:, 0:256], [ps[1]], [bdstT], act)
                      P.emit(dve, lambda: nc.vector.tensor_copy(out=gmHL[:, 0, :], in_=gmT[:, :]), reads=[B_gmT],
                             writes=[B_gmHL])
                      P.emit(dve, lambda: nc.vector.tensor_tensor(out=gmT[:, :], in0=gmT[:, :], in1=gmHL[:, 0, :],
                                                                  op=ALU.subtract),
                             reads=[B_gmT, B_gmHL], writes=[B_gmT])
                      P.emit(dve, lambda: nc.vector.tensor_copy(out=gmHL[:, 1, :], in_=gmT[:, :]), reads=[B_gmT],
                             writes=[B_gmHL])
                  P.barrier()
                  ck("route")
                  PTg = hnT[:, 0:2, :]
                  Pm = hnT[:, 2:4, :].rearrange("p a (b c) -> p (a b) c", c=256)
                  xeT = hnT[:, 4, :].rearrange("p (a c) -> p a c", c=256)
                  hm = hnT[:, 5, :].rearrange("p (a c) -> p a c", c=256)
                  yeb = hnT[:, 6, :].rearrange("p (k d) -> p k d", k=2)
                  B_xe, B_hm = Buf("xe"), Buf("hm")
                  PTgs = [(PTg, Buf("PT0")), (sbm([128, 2, S], BF16), Buf("PT1"))]
                  yebs = [(yeb, Buf("ye0")), (sbm([128, 2, D], BF16), Buf("ye1"))]
                  Pbufs = [(Pm, Buf("P0")), (sbm([128, 16, 256], BF16), Buf("P1"))]

                  def build_P(e_):
                      Pm_, B_P_ = Pbufs[e_ % 2]
                      for tc in range(16):
                          P.emit(dve, lambda: nc.vector.tensor_scalar(
                              out=Pm_[:, tc, :], in0=iota1[:, :], scalar1=posmT[:, tc * 16 + e_:tc * 16 + e_ + 1],
                              scalar2=None, op0=ALU.is_equal), reads=[B_posmT, B_c2], writes=[B_P_])

                  we_ring = Ring([(sbm([128, KC, D], BF16), Buf("we%d" % i), P.dsem("we%d" % i)) for i in range(3)])
                  s_ring = Ring([(sbm([128, 256], F32), Buf("s%d" % i)) for i in range(2)])
                  gc_ring = Ring([(sbm([128, 2], F32), Buf("gc%d" % i)) for i in range(2)])
                  for e in range(NE):
                      ws = []
                      for wd_ in (weg_d, weu_d, wed_d):
                          wt, bw, dsw = we_ring.next()
                          P.dma(pool, dsw, [wt[:, 0:4, :], wt[:, 4:8, :]],
                                [wd_[l, e, 0:512, :].rearrange("(k p) c -> p k c", p=128),
                                 wd_[l, e, 512:1024, :].rearrange("(k p) c -> p k c", p=128)], writes=[bw])
                          ws.append((wt, bw))
                      (wg, bwg), (wu, bwu), (wdn, bwd) = ws
                      Pm, B_P = Pbufs[e % 2]
                      PTg, B_PT = PTgs[e % 2]
                      yeb, B_ye = yebs[e % 2]
                      if e == 0:
                          build_P(0)
                      for k in range(2):
                          for tt in range(4):
                              ps = next_ps()
                              for j in range(4):
                                  tc = tt * 4 + j
                                  mm(ps, ps[0][:, j * 128:(j + 1) * 128], Pm[:, tc, k * 128:(k + 1) * 128], identB[:],
                                     [B_P, B_c2], last=(j == 3), start=True, stop=True)
                              copy_ps(PTg[:, k, tt * 512:(tt + 1) * 512], ps[0][:, :], [ps[1]], [B_PT])
                      psg = next_ps()
                      for k in range(2):
                          for tc in range(16):
                              mm(psg, psg[0][:, 2 * k:2 * k + 2], Pm[:, tc, k * 128:(k + 1) * 128],
                                 gmHL[:, :, tc * 16 + e], [B_P, B_gmHL], last=(k == 1 and tc == 15), start=(tc == 0), stop=(tc == 15))
                      gc, bgc = gc_ring.next()
                      P.emit(dve, lambda: nc.vector.reduce_sum(
                          out=gc[:, :], in_=psg[0][:, 0:4].rearrange("p (k h) -> p k h", h=2), axis=AX.X),
                          reads=[psg[1]], writes=[bgc])
                      for half in range(4):
                          ps = next_ps()
                          for j in range(2):
                              c = half * 2 + j
                              for tc in range(16):
                                  mm(ps, ps[0][:, j * 256:(j + 1) * 256], hn2[:, tc, c * 128:(c + 1) * 128], Pm[:, tc, :],
                                     [B_hn2, B_P], last=(j == 1 and tc == 15), start=(tc == 0), stop=(tc == 15))
                          copy_ps(xeT[:, half * 2:half * 2 + 2, :], ps[0][:, :].rearrange("p (a c) -> p a c", c=256),
                                  [ps[1]], [B_xe], act)
                      if e + 1 < NE:
                          build_P(e + 1)
                      for f in range(8):
                          ps = next_ps()
                          for c in range(KC):
                              mm(ps, ps[0][:, 0:256], wg[:, c, f * 128:(f + 1) * 128], xeT[:, c, :], [bwg, B_xe],
                                 start=(c == 0), stop=(c == KC - 1))
                          for c in range(KC):
                              mm(ps, ps[0][:, 256:512], wu[:, c, f * 128:(f + 1) * 128], xeT[:, c, :], [bwu, B_xe],
                                 last=(c == KC - 1), start=(c == 0))
                          st, bst = s_ring.next()
                          P.emit(act, lambda: nc.scalar.activation(out=st[:], in_=ps[0][:, 0:256], func=AF.Silu),
                                 reads=[ps[1]], writes=[bst])
                          P.emit(dve, lambda: nc.vector.tensor_tensor(out=hm[:, f, :], in0=ps[0][:, 256:512], in1=st[:],
                                                                      op=ALU.mult),
                                 reads=[ps[1], bst], writes=[B_hm])
                      for k in range(2):
                          for dh in range(2):
                              ps = next_ps()
                              for f in range(8):
                                  mm(ps, ps[0][:, :], hm[:, f, k * 128:(k + 1) * 128], wdn[:, f, dh * 512:(dh + 1) * 512],
                                     [bwd, B_hm], last=(f == 7))
                              P.emit(act, lambda: nc.scalar.activation(out=yeb[:, k, dh * 512:(dh + 1) * 512],
                                                                       in_=ps[0][:, :], func=AF.Copy,
                                                                       scale=gc[:, k:k + 1]),
                                     reads=[ps[1], bgc], writes=[B_ye])
                      if e % 2 == 1:
                          for jo in range(8):
                              for tt in range(4):
                                  tsl = slice(tt * 512, (tt + 1) * 512)
                                  ps = next_ps()
                                  for q_ in range(2):
                                      ptq, bptq = PTgs[q_]
                                      yeq, byeq = yebs[q_]
                                      for k in range(2):
                                          mm(ps, ps[0][:, :], yeq[:, k, jo * 128:(jo + 1) * 128], ptq[:, k, tsl],
                                             [byeq, bptq], last=(q_ == 1 and k == 1))
                                  P.emit(dve, lambda: nc.vector.tensor_tensor(out=hT[:, jo, tsl], in0=ps[0][:, :],
                                                                              in1=hT[:, jo, tsl], op=ALU.add),
                                         reads=[ps[1], B_hTt[(jo, tt)]], writes=[B_hTt[(jo, tt)]])
                  P.barrier()

          with ExitStack() as ph:
              def sbf(shape, dt):
                  return ph.enter_context(nc.sbuf_tensor(uname(), list(shape), dt))

              sq_ring = Ring([(sbf([128, 512], BF16), Buf("sq%d" % i)) for i in range(2)])
              rs_ring = Ring([(sbf([128, 512], F32), Buf("rs%d" % i)) for i in range(2)])
              stage_ring = Ring([(sbf([128, 512], F32), Buf("stg%d" % i), P.dsem("out%d" % i)) for i in range(3)])
              rmsnorm(V_FIN, sq_ring, rs_ring, final_seq=s, stage_ring=stage_ring)
              P.barrier()

    except _Stop:
        P.barrier()
        for c in range(KC):
            P.dma(sp, ds_out, outT[0, c * 128:(c + 1) * 128, :], hT[:, c, :], reads=B_hT_all)

    for d in P.dsems:
        if d.val and sp.waited.get(d.key, 0) < d.val:
            sp.h.wait_ge(d.sem, d.val)
    nc._keep_es = es if False else None; globals().setdefault("_KEEP", []).append(es)
    return nc


def _tables():
    kk = np.arange(128)[:, None]
    qq = np.arange(128)[None, :]
    tabs = np.zeros((128, 2, 3, 128), np.float32)
    for ti in range(3):
        rel = np.abs(kk + (ti - 1) * 128 - qq).astype(np.float32)
        tabs[:, 0, ti, :] = np.where(rel <= 128, rel, 1e6)
        tabs[:, 1, ti, :] = np.where(rel <= 64, rel, 1e6)
    cst2 = np.zeros((128, 384), np.float32)
    cst2[:, 0:128] = np.eye(128, dtype=np.float32)
    cst2[:, 128:384] = np.arange(1, 257, dtype=np.float32)[None, :]
    return tabs.reshape(128, -1), cst2


def prep_weights(norm_mix, w_in, w_branch_a, w_branch_b, b_gate, sink_logit, w_out, norm_ffn, w_router,
                 w_expert_gate, w_expert_up, w_expert_down, norm_final, L=DEPTH):
    f = np.float32
    w_in = np.asarray(w_in, f)
    wa = np.empty((L, 6, D, 384), f)
    for jp in range(2):
        for g in range(3):
            c0 = g * 256 + jp * 128
            u = jp * 3 + g
            wa[:, u, :, 0:128] = w_in[:L, :, c0:c0 + 128]
            wa[:, u, :, 128:256] = w_in[:L, :, 768 + c0:768 + c0 + 128]
            wa[:, u, :, 256:384] = w_in[:L, :, 1536 + c0:1536 + c0 + 128]
    q0, k0, v0, g0 = 2304, 3328, 3584, 3840
    wqw = np.ascontiguousarray(w_in[:L, :, q0:q0 + 1024].reshape(L, D, 8, 128).transpose(0, 2, 1, 3))
    wkvw = np.empty((L, 4, D, 192), f)
    for g in range(4):
        wkvw[:, g, :, 0:64] = w_in[:L, :, k0 + g * 64:k0 + (g + 1) * 64]
        wkvw[:, g, :, 64:128] = w_in[:L, :, k0 + g * 64:k0 + (g + 1) * 64]
        wkvw[:, g, :, 128:192] = w_in[:L, :, v0 + g * 64:v0 + (g + 1) * 64]
    wtail = np.empty((L, 8, D, 256), f)
    for j in range(8):
        wtail[:, j, :, 0:128] = w_in[:L, :, g0 + j * 128:g0 + (j + 1) * 128]
        wtail[:, j, :, 128:256] = w_in[:L, :, g0 + 1024 + j * 128:g0 + 1024 + (j + 1) * 128]
    wba = np.ascontiguousarray(np.asarray(w_branch_a, f)[:L].reshape(L, 256, 8, 128).transpose(0, 2, 1, 3))
    wbb = np.ascontiguousarray(np.asarray(w_branch_b, f)[:L].reshape(L, D, 8, 128).transpose(0, 2, 1, 3))

    def pm(v):
        v = np.asarray(v, f)
        return v.reshape(v.shape[:-1] + (8, 128))

    NV = L * 40 + 8
    vecs = np.zeros((128, NV), f)
    vecs[:, 0:L * 8] = pm(norm_mix)[:L].transpose(2, 0, 1).reshape(128, L * 8)
    vecs[:, L * 8:L * 16] = pm(norm_ffn)[:L].transpose(2, 0, 1).reshape(128, L * 8)
    bg = np.asarray(b_gate, f)[:L].reshape(L, 2, 8, 128)
    vecs[:, L * 16:L * 32] = bg.transpose(3, 0, 1, 2).reshape(128, L * 16)
    vecs[:, L * 32:L * 32 + 8] = pm(norm_final).transpose(1, 0)
    sk = np.asarray(sink_logit, f)[:L].reshape(L, 8, 2)
    sp = np.repeat(sk.transpose(2, 0, 1)[:, None], 64, axis=1).reshape(128, L * 8)
    vecs[:, L * 32 + 8:] = sp
    tabs, cst2 = _tables()
    return {
        "wa": wa, "wqw": wqw, "wkvw": wkvw, "wtail": wtail, "wba": wba, "wbb": wbb,
        "wout": np.ascontiguousarray(np.asarray(w_out, f)[:L]),
        "wr": np.ascontiguousarray(np.asarray(w_router, f)[:L]),
        "weg": np.ascontiguousarray(np.asarray(w_expert_gate, f)[:L]),
        "weu": np.ascontiguousarray(np.asarray(w_expert_up, f)[:L]),
        "wed": np.ascontiguousarray(np.asarray(w_expert_down, f)[:L]),
        "vecs": vecs, "tabs": tabs, "cst2": cst2,
    }


def kernel(x, norm_mix, w_in, w_branch_a, w_branch_b, b_gate, sink_logit, w_out, norm_ffn, w_router,
           w_expert_gate, w_expert_up, w_expert_down, norm_final):
    x = np.asarray(x, np.float32)
    wts = prep_weights(norm_mix, w_in, w_branch_a, w_branch_b, b_gate, sink_logit, w_out, norm_ffn, w_router,
                       w_expert_gate, w_expert_up, w_expert_down, norm_final)
    nc = build_program(DEPTH, NSEQ)
    in_maps = []
    for c in range(NCORES):
        m = dict(wts)
        m["xT"] = np.ascontiguousarray(x[c * NSEQ:(c + 1) * NSEQ].transpose(0, 2, 1))
        in_maps.append(m)
    res = run_bass_kernel_spmd(nc, in_maps, core_ids=list(range(NCORES)))
    out = np.empty((NCORES * NSEQ, S, D), np.float32)
    for c in range(NCORES):
        out[c * NSEQ:(c + 1) * NSEQ] = res.results[c]["outT"].transpose(0, 2, 1)
    return out
```
